# Optimizing a Trainium2 kernel written in Bass

```python
import math
import jax
import jax.numpy as jnp
from jax import lax
import numpy as np

D_MODEL = 1024
BATCH = 8
SEQ = 4096
DEPTH = 4

CTX_LEN = 256
GRID_W = 64
EPS = 1e-6
ROPE_BASE = 10000.0
CHUNK = 128
Q_BLOCK = 128
CONV_W = 5

MLA_HEADS = 8
MLA_NOPE = 64
MLA_ROPE = 32
MLA_V = 64
MLA_Q_LORA = 384
MLA_KV_LORA = 256
MLA_SCALE = (MLA_NOPE + MLA_ROPE) ** -0.5
MLA_OUT_W = MLA_HEADS * MLA_V

ML_HEADS = 4
ML_QK = 64
ML_V = 128
ML_QK_W = ML_HEADS * ML_QK
ML_V_W = ML_HEADS * ML_V

SSD_HEADS = 16
SSD_P = 64
SSD_N = 128
SSD_GROUPS = 4
SSD_HPG = SSD_HEADS // SSD_GROUPS
SSD_INNER = SSD_HEADS * SSD_P
SSD_BC_W = SSD_GROUPS * SSD_N

N_BRANCH = 3

N_EXPERTS = 32
TOP_K = 4
D_FF = 1024
SWIGLU_LIMIT = 7.0
SWIGLU_ALPHA = 1.702
MOE_BLOCK = 128

IN_SIZES = (MLA_Q_LORA, MLA_KV_LORA, MLA_ROPE,
            2 * ML_QK_W, ML_V_W, ML_V_W, 2 * 2 * ML_HEADS,
            SSD_INNER, SSD_INNER + 2 * SSD_BC_W, 2 * SSD_HEADS,
            N_BRANCH * D_MODEL)
D_IN = sum(IN_SIZES)

kernel_name = 'hybrid_mla_mlstm_ssd_moe_dit'


def rmsnorm(x, g):
    xf = x.astype(jnp.float32)
    y = xf * lax.rsqrt(jnp.mean(xf * xf, axis=-1, keepdims=True) + EPS)
    return (y * g.astype(jnp.float32)).astype(x.dtype)


def modulate(x, g, shift, scale):
    return rmsnorm(x, g) * (1 + scale) + shift


def split_cols(u):
    idx = [int(s) for s in np.cumsum(IN_SIZES)[:-1]]
    return jnp.split(u, idx, axis=-1)


def axial_rope(n_tok):
    rows = n_tok // GRID_W
    row = jnp.broadcast_to(jnp.arange(rows)[:, None], (rows, GRID_W)).reshape(-1)
    col = jnp.broadcast_to(jnp.arange(GRID_W)[None, :], (rows, GRID_W)).reshape(-1)
    n_freq = MLA_ROPE // 4
    inv = ROPE_BASE ** (-jnp.arange(n_freq, dtype=jnp.float32) / n_freq)
    ang = jnp.concatenate([row[:, None] * inv, col[:, None] * inv], axis=-1)
    return jnp.cos(ang), jnp.sin(ang)


def apply_rope(x, cos, sin):
    xf = x.astype(jnp.float32)
    x1, x2 = jnp.split(xf, 2, axis=-1)
    cs, sn = cos[:, None, :], sin[:, None, :]
    return jnp.concatenate([x1 * cs - x2 * sn, x2 * cs + x1 * sn], axis=-1).astype(x.dtype)


def dwconv_centred(x, w, b):
    pad = CONV_W // 2
    y = lax.conv_general_dilated(x, w[:, None, :].astype(x.dtype), window_strides=(1,),
                                 padding=((pad, pad),), dimension_numbers=('NWC', 'WIO', 'NWC'),
                                 feature_group_count=x.shape[-1])
    return y + b.astype(x.dtype)


def mla_queries(u_q, qnorm_g, w_uq, rope):
    b, t, _ = u_q.shape
    q = (rmsnorm(u_q, qnorm_g) @ w_uq).reshape(b, t, MLA_HEADS, MLA_NOPE + MLA_ROPE)
    if rope is not None:
        q = jnp.concatenate([q[..., :MLA_NOPE], apply_rope(q[..., MLA_NOPE:], *rope)], axis=-1)
    return q


def mla_keys_values(u_kv, u_kr, kvnorm_g, w_ukv, rope):
    b, t, _ = u_kv.shape
    kv = (rmsnorm(u_kv, kvnorm_g) @ w_ukv).reshape(b, t, MLA_HEADS, MLA_NOPE + MLA_V)
    k_rope = u_kr[:, :, None, :]
    if rope is not None:
        k_rope = apply_rope(k_rope, *rope)
    k = jnp.concatenate([kv[..., :MLA_NOPE], jnp.broadcast_to(k_rope, (b, t, MLA_HEADS, MLA_ROPE))], axis=-1)
    return k, kv[..., MLA_NOPE:]


def softmax_attend(q, k, v):
    s = jnp.einsum('bqhd,bkhd->bhqk', q, k).astype(jnp.float32) * MLA_SCALE
    p = jax.nn.softmax(s, axis=-1).astype(v.dtype)
    return jnp.einsum('bhqk,bkhd->bqhd', p, v)


def blocked_attend(q, k, v):
    b, t, h, d = q.shape
    nb = t // Q_BLOCK
    qb = jnp.moveaxis(q.reshape(b, nb, Q_BLOCK, h, d), 1, 0)
    out = lax.map(lambda qi: softmax_attend(qi, k, v), qb)
    return jnp.moveaxis(out, 0, 1).reshape(b, t, h, v.shape[-1])


def mlstm_scan(q, k, v, log_i, log_f, state, emit):
    b, h, t, _ = q.shape
    nc = t // CHUNK

    def chunks(a):
        return jnp.moveaxis(a.reshape(b, h, nc, CHUNK, *a.shape[3:]), 2, 0)

    tril = jnp.tril(jnp.ones((CHUNK, CHUNK), dtype=bool))

    def step(carry, inp):
        c_mat, n_vec, m = carry
        qc, kc, vc, ic, fc = inp
        fcum = jnp.cumsum(fc, axis=-1)
        last = fcum[..., -1]
        src = last[..., None] - fcum + ic
        m_new = jnp.maximum(last + m, jnp.max(src, axis=-1))
        w_src = jnp.exp(src - m_new[..., None])
        w_old = jnp.exp(last + m - m_new)
        c_new = w_old[..., None, None] * c_mat + jnp.einsum('bhs,bhsv,bhsd->bhvd', w_src, vc, kc)
        n_new = w_old[..., None] * n_vec + jnp.einsum('bhs,bhsd->bhd', w_src, kc)
        out = None
        if emit:
            dmat = jnp.where(tril, fcum[..., :, None] - fcum[..., None, :] + ic[..., None, :], -jnp.inf)
            g = fcum + m[..., None]
            m_row = jnp.maximum(g, jnp.max(dmat, axis=-1))
            s = jnp.einsum('bhtd,bhsd->bhts', qc, kc) * jnp.exp(dmat - m_row[..., None])
            w_prev = jnp.exp(g - m_row)
            num = jnp.einsum('bhts,bhsv->bhtv', s, vc) + w_prev[..., None] * jnp.einsum('bhvd,bhtd->bhtv', c_mat, qc)
            den = jnp.sum(s, axis=-1) + w_prev * jnp.einsum('bhd,bhtd->bht', n_vec, qc)
            out = num / jnp.maximum(jnp.abs(den), jnp.exp(-m_row))[..., None]
        return (c_new, n_new, m_new), out

    state, hs = lax.scan(step, state, tuple(chunks(a) for a in (q, k, v, log_i, log_f)))
    if not emit:
        return None, state
    return jnp.moveaxis(hs, 0, 2).reshape(b, h, t, -1), state


def mlstm_dir(p, d, state, reverse, emit):
    arrs = (p['ml_q'], p['ml_k'], p['ml_v'], p['ml_i'][:, d], p['ml_f'][:, d])
    if reverse:
        arrs = tuple(jnp.flip(a, axis=2) for a in arrs)
    h, state = mlstm_scan(*arrs, state, emit)
    if reverse and emit:
        h = jnp.flip(h, axis=2)
    return h, state


def ssd_scan(x, dt, bm, cm, a, state, emit):
    b, t = x.shape[:2]
    nc = t // CHUNK

    def chunks(arr):
        return jnp.moveaxis(arr.reshape(b, nc, CHUNK, *arr.shape[2:]), 1, 0)

    tril = jnp.tril(jnp.ones((CHUNK, CHUNK), dtype=bool))[None, :, :, None, None]

    def step(hst, inp):
        xc, dtc, bc, cc = inp
        acum = jnp.cumsum(dtc * a, axis=1)
        last = acum[:, -1]
        w_src = jnp.exp(last[:, None] - acum) * dtc
        h_new = jnp.exp(last)[..., None, None] * hst + jnp.einsum('bsgn,bsgh,bsghp->bghnp', bc, w_src, xc)
        out = None
        if emit:
            seg = jnp.exp(jnp.where(tril, acum[:, :, None] - acum[:, None, :], -jnp.inf))
            cb = jnp.einsum('btgn,bsgn->btsg', cc, bc)
            y = jnp.einsum('btsgh,bsghp->btghp', seg * cb[..., None] * dtc[:, None], xc)
            out = y + jnp.einsum('btgn,bghnp->btghp', cc, hst) * jnp.exp(acum)[..., None]
        return h_new, out

    state, ys = lax.scan(step, state, tuple(chunks(arr) for arr in (x, dt, bm, cm)))
    if not emit:
        return None, state
    return jnp.moveaxis(ys, 0, 1).reshape(x.shape), state


def ssd_dir(p, a, d, state, reverse, emit):
    arrs = (p['ssd_x'], p['ssd_dt'][:, :, d], p['ssd_b'], p['ssd_c'])
    if reverse:
        arrs = tuple(jnp.flip(arr, axis=1) for arr in arrs)
    y, state = ssd_scan(*arrs, a, state, emit)
    if reverse and emit:
        y = jnp.flip(y, axis=1)
    return y, state


def stream_prep(hn, lp, rope, queries):
    b, t, _ = hn.shape
    f32 = jnp.float32
    (u_q, u_kv, u_kr, u_qk, u_v, u_o, u_if, u_z, u_xbc, u_dt, u_g) = split_cols(hn @ lp['w_in'])

    def heads(a, n):
        return jnp.moveaxis(a.reshape(b, t, n, -1), 2, 1).astype(f32)

    mla_k, mla_v = mla_keys_values(u_kv, u_kr, lp['mla_kvnorm_g'], lp['mla_w_ukv'], rope)
    qk = jax.nn.silu(dwconv_centred(u_qk, lp['ml_conv_w'], lp['ml_conv_b']))
    ml_q, ml_k = jnp.split(qk, 2, axis=-1)
    gates = u_if.reshape(b, t, 2, 2, ML_HEADS).astype(f32) + lp['ml_gate_b']
    gates = jnp.moveaxis(gates, 1, -1)
    xbc = jax.nn.silu(dwconv_centred(u_xbc, lp['ssd_conv_w'], lp['ssd_conv_b']))
    s_x, s_b, s_c = jnp.split(xbc, [SSD_INNER, SSD_INNER + SSD_BC_W], axis=-1)
    dt = jax.nn.softplus(u_dt.reshape(b, t, 2, SSD_GROUPS, SSD_HPG).astype(f32) + lp['ssd_dt_bias'])
    return dict(
        mla_q=mla_queries(u_q, lp['mla_qnorm_g'], lp['mla_w_uq'], rope) if queries else None,
        mla_k=mla_k, mla_v=mla_v,
        ml_q=heads(ml_q, ML_HEADS), ml_k=heads(ml_k, ML_HEADS) * (ML_QK ** -0.5), ml_v=heads(u_v, ML_HEADS),
        ml_i=gates[:, :, 0], ml_f=jax.nn.log_sigmoid(gates[:, :, 1]), ml_o=u_o,
        ssd_x=s_x.reshape(b, t, SSD_GROUPS, SSD_HPG, SSD_P).astype(f32),
        ssd_b=s_b.reshape(b, t, SSD_GROUPS, SSD_N).astype(f32),
        ssd_c=s_c.reshape(b, t, SSD_GROUPS, SSD_N).astype(f32),
        ssd_dt=dt, ssd_z=u_z, gates=u_g)


def stream_out(att, h_ml, y_ssd, p, lp):
    b, t = p['gates'].shape[:2]
    dtype = p['gates'].dtype
    a_out = att.reshape(b, t, MLA_OUT_W)
    m_out = rmsnorm(jnp.moveaxis(h_ml, 1, 2), lp['ml_norm_g']).reshape(b, t, ML_V_W).astype(dtype) * jax.nn.sigmoid(p['ml_o'])
    y = (y_ssd + lp['ssd_d'][..., None] * p['ssd_x']).reshape(b, t, SSD_GROUPS, SSD_HPG * SSD_P)
    y = y * jax.nn.silu(p['ssd_z'].astype(jnp.float32).reshape(b, t, SSD_GROUPS, SSD_HPG * SSD_P))
    s_out = rmsnorm(y, lp['ssd_norm_g']).reshape(b, t, SSD_INNER).astype(dtype)
    g = jax.nn.sigmoid(p['gates']).reshape(b, t, N_BRANCH, D_MODEL)
    merged = (g[:, :, 0] * (a_out @ lp['w_br_mla']) + g[:, :, 1] * (m_out @ lp['w_br_ml'])
              + g[:, :, 2] * (s_out @ lp['w_br_ssd']))
    return merged @ lp['w_out']


def mixer_layer(pc, pl, lp, emit_ctx):
    b = pl['ssd_x'].shape[0]
    f32 = jnp.float32
    k_all = jnp.concatenate([pc['mla_k'], pl['mla_k']], axis=1)
    v_all = jnp.concatenate([pc['mla_v'], pl['mla_v']], axis=1)
    att_l = blocked_attend(pl['mla_q'], k_all, v_all)
    ml_zero = (jnp.zeros((b, ML_HEADS, ML_V, ML_QK), f32), jnp.zeros((b, ML_HEADS, ML_QK), f32),
               jnp.zeros((b, ML_HEADS), f32))
    ssd_zero = jnp.zeros((b, SSD_GROUPS, SSD_HPG, SSD_N, SSD_P), f32)
    ml_l, ml_c, ssd_l, ssd_c = [], [], [], []
    for d in range(2):
        rev = d == 1
        hc, st = mlstm_dir(pc, d, ml_zero, rev, emit_ctx)
        hl, _ = mlstm_dir(pl, d, st, rev, True)
        yc, sst = ssd_dir(pc, lp['ssd_a'][d], d, ssd_zero, rev, emit_ctx)
        yl, _ = ssd_dir(pl, lp['ssd_a'][d], d, sst, rev, True)
        ml_l.append(hl)
        ml_c.append(hc)
        ssd_l.append(yl)
        ssd_c.append(yc)
    out_l = stream_out(att_l, ml_l[0] + ml_l[1], ssd_l[0] + ssd_l[1], pl, lp)
    if not emit_ctx:
        return out_l, None
    att_c = softmax_attend(pc['mla_q'], pc['mla_k'], pc['mla_v'])
    out_c = stream_out(att_c, ml_c[0] + ml_c[1], ssd_c[0] + ssd_c[1], pc, lp)
    return out_l, out_c


def clamped_swiglu(gu):
    glu, lin = jnp.split(gu, 2, axis=-1)
    glu = jnp.minimum(glu, SWIGLU_LIMIT)
    lin = jnp.clip(lin, -SWIGLU_LIMIT, SWIGLU_LIMIT)
    return glu * jax.nn.sigmoid(SWIGLU_ALPHA * glu) * (lin + 1)


def moe_ffn(h, w_router, b_router, w_up, b_up, w_down, b_down):
    t, d = h.shape
    logits = (h @ w_router).astype(jnp.float32) + b_router
    top_v, top_i = lax.top_k(logits, TOP_K)
    gate = jax.nn.softmax(top_v, axis=-1)
    flat_e = top_i.reshape(-1)
    flat_tok = jnp.repeat(jnp.arange(t), TOP_K)
    order = jnp.argsort(flat_e)
    se = flat_e[order]
    counts = jnp.bincount(flat_e, length=N_EXPERTS)
    start = jnp.cumsum(counts) - counts
    padded = (counts + MOE_BLOCK - 1) // MOE_BLOCK * MOE_BLOCK
    pad_end = jnp.cumsum(padded)
    pad_start = pad_end - padded
    dest = pad_start[se] + jnp.arange(t * TOP_K) - start[se]
    n_rows = -(-(t * TOP_K + N_EXPERTS * (MOE_BLOCK - 1)) // MOE_BLOCK) * MOE_BLOCK
    n_blocks = n_rows // MOE_BLOCK
    row_tok = jnp.full((n_rows,), t, dtype=flat_tok.dtype).at[dest].set(flat_tok[order])
    row_w = jnp.zeros((n_rows,), gate.dtype).at[dest].set(gate.reshape(-1)[order])
    block_e = jnp.minimum(jnp.searchsorted(pad_end, jnp.arange(n_blocks) * MOE_BLOCK, side='right'), N_EXPERTS - 1)
    h_pad = jnp.concatenate([h, jnp.zeros((1, d), h.dtype)], axis=0)

    def run_block(args):
        tok, e = args
        gu = h_pad[tok] @ w_up[e] + b_up[e]
        return clamped_swiglu(gu) @ w_down[e] + b_down[e]

    out = lax.map(run_block, (row_tok.reshape(n_blocks, MOE_BLOCK), block_e)).reshape(n_rows, d)
    out = out * row_w[:, None].astype(out.dtype)
    return jax.ops.segment_sum(out, row_tok, num_segments=t + 1)[:t]


def setup_inputs(seed: int = 0) -> dict:
    key = jax.random.key(seed)
    ks = iter(jax.random.split(key, 40))
    L, D = DEPTH, D_MODEL

    def nrm(shape, scale):
        return jax.random.normal(next(ks), shape, jnp.float32) * scale

    x = nrm((BATCH, SEQ, D), 1.0)
    c = nrm((BATCH, D), 1.0)
    ctx = nrm((BATCH, CTX_LEN, D), 1.0)
    c_ctx = nrm((D,), 1.0)
    w_ada = nrm((L, D, 6 * D), 0.5 * D ** -0.5)
    b_ada = nrm((L, 6 * D), 0.02)
    norm1_g = 1.0 + nrm((L, D), 0.02)
    w_in = nrm((L, D, D_IN), D ** -0.5)
    mla_qnorm_g = 1.0 + nrm((L, MLA_Q_LORA), 0.02)
    mla_w_uq = nrm((L, MLA_Q_LORA, MLA_HEADS * (MLA_NOPE + MLA_ROPE)), MLA_Q_LORA ** -0.5)
    mla_kvnorm_g = 1.0 + nrm((L, MLA_KV_LORA), 0.02)
    mla_w_ukv = nrm((L, MLA_KV_LORA, MLA_HEADS * (MLA_NOPE + MLA_V)), MLA_KV_LORA ** -0.5)
    ml_conv_w = nrm((L, CONV_W, 2 * ML_QK_W), CONV_W ** -0.5)
    ml_conv_b = nrm((L, 2 * ML_QK_W), 0.02)
    i_bias = nrm((L, 2, ML_HEADS), 0.1)
    f_bias = jnp.linspace(3.0, 6.0, ML_HEADS, dtype=jnp.float32) + nrm((L, 2, ML_HEADS), 0.1)
    ml_gate_b = jnp.stack([i_bias, f_bias], axis=2)
    ml_norm_g = 1.0 + nrm((L, ML_HEADS, ML_V), 0.02)
    ssd_conv_w = nrm((L, CONV_W, SSD_INNER + 2 * SSD_BC_W), CONV_W ** -0.5)
    ssd_conv_b = nrm((L, SSD_INNER + 2 * SSD_BC_W), 0.02)
    dt0 = jnp.exp(jax.random.uniform(next(ks), (L, 2, SSD_HEADS), jnp.float32,
                                     minval=math.log(1e-3), maxval=math.log(1e-1)))
    ssd_dt_bias = dt0 + jnp.log(-jnp.expm1(-dt0))
    ssd_a_log = jnp.log(jax.random.uniform(next(ks), (L, 2, SSD_HEADS), jnp.float32, minval=1.0, maxval=16.0))
    ssd_d = 1.0 + nrm((L, SSD_HEADS), 0.1)
    ssd_norm_g = 1.0 + nrm((L, SSD_GROUPS, SSD_HPG * SSD_P), 0.02)
    w_br_mla = nrm((L, MLA_OUT_W, D), MLA_OUT_W ** -0.5)
    w_br_ml = nrm((L, ML_V_W, D), ML_V_W ** -0.5)
    w_br_ssd = nrm((L, SSD_INNER, D), SSD_INNER ** -0.5)
    w_out = nrm((L, D, D), D ** -0.5)
    norm2_g = 1.0 + nrm((L, D), 0.02)
    w_router = nrm((L, D, N_EXPERTS), D ** -0.5)
    b_router = nrm((L, N_EXPERTS), 0.01)
    w_up = nrm((L, N_EXPERTS, D, 2 * D_FF), D ** -0.5)
    b_up = nrm((L, N_EXPERTS, 2 * D_FF), 0.02)
    w_down = nrm((L, N_EXPERTS, D_FF, D), D_FF ** -0.5)
    b_down = nrm((L, N_EXPERTS, D), 0.02)
    final_g = 1.0 + nrm((D,), 0.02)
    return {'x': x, 'c': c, 'ctx': ctx, 'c_ctx': c_ctx, 'w_ada': w_ada, 'b_ada': b_ada, 'norm1_g': norm1_g,
            'w_in': w_in, 'mla_qnorm_g': mla_qnorm_g, 'mla_w_uq': mla_w_uq, 'mla_kvnorm_g': mla_kvnorm_g,
            'mla_w_ukv': mla_w_ukv, 'ml_conv_w': ml_conv_w, 'ml_conv_b': ml_conv_b, 'ml_gate_b': ml_gate_b,
            'ml_norm_g': ml_norm_g, 'ssd_conv_w': ssd_conv_w, 'ssd_conv_b': ssd_conv_b, 'ssd_dt_bias': ssd_dt_bias,
            'ssd_a_log': ssd_a_log, 'ssd_d': ssd_d, 'ssd_norm_g': ssd_norm_g, 'w_br_mla': w_br_mla,
            'w_br_ml': w_br_ml, 'w_br_ssd': w_br_ssd, 'w_out': w_out, 'norm2_g': norm2_g, 'w_router': w_router,
            'b_router': b_router, 'w_up': w_up, 'b_up': b_up, 'w_down': w_down, 'b_down': b_down,
            'final_g': final_g}


def reference(x, c, ctx, c_ctx, w_ada, b_ada, norm1_g, w_in, mla_qnorm_g, mla_w_uq, mla_kvnorm_g, mla_w_ukv,
              ml_conv_w, ml_conv_b, ml_gate_b, ml_norm_g, ssd_conv_w, ssd_conv_b, ssd_dt_bias, ssd_a_log, ssd_d,
              ssd_norm_g, w_br_mla, w_br_ml, w_br_ssd, w_out, norm2_g, w_router, b_router, w_up, b_up, w_down,
              b_down, final_g):
    f32 = jnp.float32
    n_lat, d_model = x.shape[1], x.shape[2]
    rope = axial_rope(n_lat)
    xl, xc = x, ctx
    for l in range(DEPTH):
        last = l == DEPTH - 1
        lp = dict(w_in=w_in[l], mla_qnorm_g=mla_qnorm_g[l], mla_w_uq=mla_w_uq[l], mla_kvnorm_g=mla_kvnorm_g[l],
                  mla_w_ukv=mla_w_ukv[l], ml_conv_w=ml_conv_w[l], ml_conv_b=ml_conv_b[l], ml_gate_b=ml_gate_b[l],
                  ml_norm_g=ml_norm_g[l], ssd_conv_w=ssd_conv_w[l], ssd_conv_b=ssd_conv_b[l],
                  ssd_dt_bias=ssd_dt_bias[l].astype(f32).reshape(2, SSD_GROUPS, SSD_HPG),
                  ssd_a=-jnp.exp(ssd_a_log[l].astype(f32)).reshape(2, SSD_GROUPS, SSD_HPG),
                  ssd_d=ssd_d[l].reshape(SSD_GROUPS, SSD_HPG), ssd_norm_g=ssd_norm_g[l],
                  w_br_mla=w_br_mla[l], w_br_ml=w_br_ml[l], w_br_ssd=w_br_ssd[l], w_out=w_out[l])
        mod_l = (jax.nn.silu(c) @ w_ada[l] + b_ada[l])[:, None, :]
        mod_c = (jax.nn.silu(c_ctx) @ w_ada[l] + b_ada[l])[None, None, :]
        sh1_l, sc1_l, g1_l, sh2_l, sc2_l, g2_l = jnp.split(mod_l, 6, axis=-1)
        sh1_c, sc1_c, g1_c, sh2_c, sc2_c, g2_c = jnp.split(mod_c, 6, axis=-1)
        pl = stream_prep(modulate(xl, norm1_g[l], sh1_l, sc1_l), lp, rope, True)
        pc = stream_prep(modulate(xc, norm1_g[l], sh1_c, sc1_c), lp, None, not last)
        out_l, out_c = mixer_layer(pc, pl, lp, not last)
        xl = xl + g1_l * out_l
        moe_args = (w_router[l], b_router[l], w_up[l], b_up[l], w_down[l], b_down[l])
        hl2 = modulate(xl, norm2_g[l], sh2_l, sc2_l).reshape(-1, d_model)
        if last:
            xl = xl + g2_l * moe_ffn(hl2, *moe_args).reshape(xl.shape)
        else:
            xc = xc + g1_c * out_c
            hc2 = modulate(xc, norm2_g[l], sh2_c, sc2_c).reshape(-1, d_model)
            n_c = hc2.shape[0]
            f = moe_ffn(jnp.concatenate([hc2, hl2], axis=0), *moe_args)
            xc = xc + g2_c * f[:n_c].reshape(xc.shape)
            xl = xl + g2_l * f[n_c:].reshape(xl.shape)
    return rmsnorm(xl, final_g)
```

```python
import numpy as np
from contextlib import ExitStack
import concourse.bass as bass
import concourse.mybir as mybir
from concourse.bass_utils import run_bass_kernel_spmd

F32 = mybir.dt.float32
BF16 = mybir.dt.bfloat16
AF = mybir.ActivationFunctionType
ALU = mybir.AluOpType
AX = mybir.AxisListType

SEM_LIMIT = 30000
N_DMA_SEMS = 12

L = 4
D = 1024
CTX = 256
SEQ = 4096
T = CTX + SEQ
NCH = T // 128
TP = T + 8
EPS = 1e-6
NE = 32
MLA_SCALE = 96 ** -0.5
NEG = -30000.0


def colof(t):
    return t + 2 if t < CTX else t + 6


TILES = [(0, 256)] + [(CTX + 512 * i, 512) for i in range(8)]


class Tok:
    __slots__ = ("sem", "val", "eng")

    def __init__(self, sem, val, eng):
        self.sem = sem
        self.val = val
        self.eng = eng


class Prog:
    ENGS = ("pe", "dve", "act", "pool", "sp")

    def __init__(self, nc):
        self.nc = nc
        self.es = ExitStack()
        self.ops = {e: [] for e in self.ENGS}
        self.cur_sem = {}
        self.cnt = {}
        self.nsem = 0
        for e in self.ENGS:
            self._new_sem(e)
        self.last_tok = {e: None for e in self.ENGS}
        self.known = {e: {} for e in self.ENGS}
        self.last_write = {}
        self.readers = {}
        self.dma_sems = {}
        self.dma_idx = {}
        self.dma_last = {}
        self.dma_cnt = {}
        for q in ("sp", "pool", "act"):
            self.dma_sems[q] = [self._sem("d%s%d" % (q, i)) for i in range(N_DMA_SEMS)]
            self.dma_idx[q] = 0
            self.dma_last[q] = [None] * N_DMA_SEMS
            self.dma_cnt[q] = [0] * N_DMA_SEMS
        self.n_ops = 0

    def _sem(self, name):
        self.nsem += 1
        return self.es.enter_context(self.nc.semaphore("s%d_%s" % (self.nsem, name)))

    def _new_sem(self, e):
        self.cur_sem[e] = self._sem(e)
        self.cnt[e] = 0

    def sb(self, name, shape, dtype, stack=None):
        self.n_tiles = getattr(self, "n_tiles", 0) + 1
        return (stack or self.es).enter_context(self.nc.sbuf_tensor("%s_u%d" % (name, self.n_tiles), list(shape), dtype))

    def ps(self, name, shape, dtype=F32, stack=None):
        return (stack or self.es).enter_context(self.nc.psum_tensor(name, list(shape), dtype))

    def _need(self, eng, tok, waits):
        if tok is None:
            return
        k = self.known[eng]
        if k.get(tok.sem, 0) >= tok.val:
            return
        k[tok.sem] = tok.val
        waits.append((tok.sem, tok.val))

    @staticmethod
    def _k(k):
        if isinstance(k, (str, int)):
            return k
        if isinstance(k, tuple):
            return tuple(Prog._k(x) for x in k)
        return k.name

    def op(self, eng, fn, reads=(), writes=(), acc=False, dma=False):
        reads = [self._k(k) for k in reads]
        writes = [self._k(k) for k in writes]
        waits = []
        for k in reads:
            self._need(eng, self.last_write.get(k), waits)
            kn = k[0] if isinstance(k, tuple) else k
            if isinstance(kn, str) and kn.startswith("ps"):
                for r in self.readers.get(k, ()):
                    if r.eng != eng:
                        self._need(eng, r, waits)
        for k in writes:
            w = self.last_write.get(k)
            if w is not None and not (acc and w.eng == "pe" and eng == "pe"):
                self._need(eng, w, waits)
            for r in self.readers.get(k, ()):
                self._need(eng, r, waits)
        if dma:
            q = eng
            i = self.dma_idx[q]
            self.dma_idx[q] = (i + 1) % N_DMA_SEMS
            self._need(eng, self.dma_last[q][i], waits)
            self.dma_cnt[q][i] += 16
            tok = Tok(self.dma_sems[q][i], self.dma_cnt[q][i], "dma")
            self.dma_last[q][i] = tok
            inc = 16
        else:
            if self.cnt[eng] >= SEM_LIMIT:
                self._new_sem(eng)
            self.cnt[eng] += 1
            tok = Tok(self.cur_sem[eng], self.cnt[eng], eng)
            self.last_tok[eng] = tok
            inc = 1
        for k in writes:
            self.last_write[k] = tok
            self.readers[k] = []
        for k in reads:
            lst = self.readers.setdefault(k, [])
            if tok.eng != "dma":
                lst[:] = [r for r in lst if r.eng != tok.eng]
            lst.append(tok)
        self.ops[eng].append((waits, fn, tok.sem, inc))
        self.n_ops += 1
        return tok

    def barrier(self):
        toks = [self.last_tok[e] for e in self.ENGS if self.last_tok[e] is not None]
        for q in self.dma_last:
            toks += [t for t in self.dma_last[q] if t is not None]
        for e in self.ENGS:
            waits = []
            for t in toks:
                self._need(e, t, waits)
            if waits:
                self.ops[e].append((waits, None, None, 0))
        self.last_write = {}
        self.readers = {}

    def emit(self):
        self.barrier()
        nc = self.nc
        ops = self.ops
        with nc.Block() as block:
            def run(eng_obj, lst):
                for waits, fn, sem, inc in lst:
                    for s, v in waits:
                        eng_obj.wait_ge(s, v)
                    if fn is not None:
                        fn(eng_obj).then_inc(sem, inc)

            @block.tensor
            def _(e):
                run(e, ops["pe"])

            @block.vector
            def _(e):
                run(e, ops["dve"])

            @block.scalar
            def _(e):
                run(e, ops["act"])

            @block.gpsimd
            def _(e):
                run(e, ops["pool"])

            @block.sync
            def _(e):
                run(e, ops["sp"])

    def close(self):
        self.es.close()


class Rot:
    def __init__(self, items):
        self.items = items
        self.i = 0

    def next(self):
        x = self.items[self.i % len(self.items)]
        self.i += 1
        return x


IN_SIZES = (384, 256, 32, 512, 512, 512, 16, 1024, 2048, 32, 3072)
IN_OFF = np.concatenate([[0], np.cumsum(IN_SIZES)]).astype(int)


def fm_vec(v, nch):
    return np.ascontiguousarray(np.swapaxes(v.reshape(v.shape[:-1] + (nch, 128)), -1, -2))


def host_prepare(inp):
    f32 = np.float32
    w_in = inp["w_in"]
    sl = lambda i: w_in[:, :, IN_OFF[i]:IN_OFF[i + 1]]
    sh = {}
    sh["w_ada"] = inp["w_ada"]
    sh["b_ada_fm"] = fm_vec(inp["b_ada"], 48)
    sh["n1g_fm"] = fm_vec(inp["norm1_g"], 8)
    sh["n2g_fm"] = fm_vec(inp["norm2_g"], 8)
    sh["fing_fm"] = fm_vec(inp["final_g"], 8)
    sh["w_q"] = np.ascontiguousarray(sl(0))
    sh["w_kv"] = np.ascontiguousarray(sl(1))
    kr = sl(2)
    w_kr = np.zeros((L, D, 256), f32)
    w_kr[:, :, 64:96] = kr
    w_kr[:, :, 128 + 64:128 + 80] = kr[:, :, 16:32]
    w_kr[:, :, 128 + 80:128 + 96] = kr[:, :, 0:16]
    sh["w_kr"] = w_kr
    sh["w_mqk"] = np.ascontiguousarray(sl(3))
    sh["w_mv"] = np.ascontiguousarray(sl(4))
    sh["w_mo"] = np.ascontiguousarray(sl(5))
    sh["w_mif"] = np.ascontiguousarray(sl(6))
    sh["w_sz"] = np.ascontiguousarray(sl(7))
    xbc = sl(8)
    sh["w_sx"] = np.ascontiguousarray(xbc[:, :, 0:1024])
    sh["w_sB"] = np.ascontiguousarray(xbc[:, :, 1024:1536])
    sh["w_sC"] = np.ascontiguousarray(xbc[:, :, 1536:2048])
    sh["w_sdt"] = np.ascontiguousarray(sl(9))
    sh["w_g"] = np.ascontiguousarray(sl(10))
    uq = inp["mla_w_uq"].reshape(L, 384, 8, 96)
    w_uq = np.zeros((L, 384, 2, 8, 128), f32)
    w_uq[:, :, 0, :, 0:96] = uq
    w_uq[:, :, 1, :, 64:80] = uq[..., 80:96]
    w_uq[:, :, 1, :, 80:96] = uq[..., 64:80]
    sh["w_uq"] = w_uq.reshape(L, 384, 2048)
    ukv = inp["mla_w_ukv"].reshape(L, 256, 8, 128)
    sh["w_uk"] = np.ascontiguousarray(ukv[..., 0:64]).reshape(L, 256, 512)
    sh["w_uv"] = np.ascontiguousarray(ukv[..., 64:128]).reshape(L, 256, 512)
    sh["qn_fm"] = fm_vec(inp["mla_qnorm_g"], 3)
    sh["kvn_fm"] = fm_vec(inp["mla_kvnorm_g"], 2)
    sh["ml_cw"] = inp["ml_conv_w"]
    sh["ml_cb_fm"] = fm_vec(inp["ml_conv_b"], 4)
    sh["ml_gb"] = inp["ml_gate_b"].reshape(L, 1, 16)
    sh["ml_ng"] = inp["ml_norm_g"].reshape(L, 1, 512)
    scw = inp["ssd_conv_w"]
    scb = inp["ssd_conv_b"]
    sh["s_cw"] = scw
    sh["s_cb"] = scb.reshape(L, 1, 2048)
    sh["s_cbB_fm"] = fm_vec(scb[:, 1024:1536], 4)
    sh["s_cbC_fm"] = fm_vec(scb[:, 1536:2048], 4)
    sh["s_dtb"] = inp["ssd_dt_bias"].reshape(L, 1, 32)
    sh["s_alog"] = inp["ssd_a_log"].reshape(L, 1, 32)
    sh["s_d"] = inp["ssd_d"].reshape(L, 1, 16)
    sh["s_ng"] = inp["ssd_norm_g"].reshape(L, 1, 1024)
    sh["w_bra"] = inp["w_br_mla"]
    sh["w_brm"] = inp["w_br_ml"]
    sh["w_brs"] = inp["w_br_ssd"]
    sh["w_out"] = inp["w_out"]
    sh["w_rt"] = inp["w_router"]
    sh["b_rt"] = inp["b_router"].reshape(L, 1, 32)
    sh["w_up"] = inp["w_up"]
    sh["w_dn"] = inp["w_down"]
    sh["b_up_fm"] = fm_vec(inp["b_up"], 16)
    sh["b_dn"] = inp["b_down"]
    r = np.arange(128)
    sh["c_ident"] = np.eye(128, dtype=f32)
    sh["c_ones"] = np.ones((128, 128), f32)
    sh["c_tri_le"] = (r[:, None] <= r[None, :]).astype(f32)
    sh["c_tri_ge"] = (r[:, None] >= r[None, :]).astype(f32)
    sh["c_tri_gt"] = (r[:, None] > r[None, :]).astype(f32)
    sh["c_tri_lt"] = (r[:, None] < r[None, :]).astype(f32)
    sel = np.zeros((32, 32, 128), f32)
    for e in range(32):
        sel[e, e, :] = 1.0
    sh["c_sel"] = sel
    rows = SEQ // 64
    row = np.repeat(np.arange(rows), 64)
    col = np.tile(np.arange(64), rows)
    inv = (10000.0 ** (-np.arange(8, dtype=np.float32) / 8)).astype(np.float32)
    ang = np.concatenate([row[:, None] * inv, col[:, None] * inv], axis=-1).astype(np.float32)
    cs = np.cos(ang).T
    sn = np.sin(ang).T
    rc = np.ones((32, T), f32)
    rs = np.zeros((32, T), f32)
    rc[0:16, CTX:] = cs
    rc[16:32, CTX:] = cs
    rs[0:16, CTX:] = -sn
    rs[16:32, CTX:] = sn
    rope = np.zeros((128, 2, T), f32)
    rope[64:96, 0] = rc
    rope[64:96, 1] = rs
    sh["c_rope"] = rope
    return sh


def core_inputs(inp, b):
    xin = np.concatenate([inp["ctx"][b], inp["x"][b]], axis=0).astype(np.float32)
    cvec = np.stack([fm_vec(inp["c"][b], 8), fm_vec(inp["c_ctx"], 8)], axis=-1).astype(np.float32)
    return {"xin": np.ascontiguousarray(xin), "cvec": np.ascontiguousarray(cvec)}


SHARED_SPECS = None


class K:
    pass


def dma(P, q, out, in_, reads=(), writes=()):
    return P.op(q, lambda e: e.dma_start(out=out, in_=in_), reads=reads, writes=writes, dma=True)


def build_program(shared_shapes, n_layers=L, stop_after=None, debug=False, only=None, scr_inputs=()):
    nc = bass.Bass("TRN2", target_bir_lowering=False)
    P = Prog(nc)
    k = K()
    k.nc, k.P = nc, P
    k.debug = debug
    k.din = {}
    for name, shp in shared_shapes.items():
        k.din[name] = nc.dram_tensor(name, list(shp), F32, kind="ExternalInput").ap()
    k.din["xin"] = nc.dram_tensor("xin", [T, D], F32, kind="ExternalInput").ap()
    k.din["cvec"] = nc.dram_tensor("cvec", [128, 8, 2], F32, kind="ExternalInput").ap()
    k.out = nc.dram_tensor("out", [SEQ, D], F32, kind="ExternalOutput").ap()
    skind = "ExternalOutput" if debug else "Internal"
    k.scr = {}

    def scr(name, shape, dt):
        kd = "ExternalInput" if name in scr_inputs else skind
        k.scr[name] = nc.dram_tensor("scr_" + name, list(shape), dt, kind=kd).ap()

    scr("xres", [D, T], F32)
    scr("uq", [384, T], BF16)
    scr("ukv", [256, T], BF16)
    scr("kr", [256, T], BF16)
    scr("mqk", [512, T], BF16)
    scr("mv", [T, 512], BF16)
    scr("mo", [T, 512], BF16)
    scr("mif", [T, 16], F32)
    scr("sz", [T, 1024], BF16)
    scr("sx", [T, 1024], BF16)
    scr("sBf", [512, T], BF16)
    scr("sBt", [T, 512], BF16)
    scr("sCf", [512, T], BF16)
    scr("sdt", [T, 32], F32)
    scr("gg", [3072, T], BF16)
    scr("att", [512, T], BF16)
    scr("mout", [512, T], BF16)
    scr("sout", [1024, T], BF16)
    scr("hacc", [T, 512], F32)
    scr("yacc", [T, 1024], F32)
    scr("hn2", [D, T], BF16)
    scr("gfm", [32, T], F32)

    C = {}
    k.C = C

    def cload(name, src, shape, dt=F32, q="sp"):
        t = P.sb("k_" + name, shape, dt)
        dma(P, q, t[:], src, writes=[t])
        C[name] = t
        return t

    cload("ident_f", k.din["c_ident"][:, :], [128, 128])
    cload("ones_f", k.din["c_ones"][:, :], [128, 128])
    cload("ident", k.din["c_ident"][:, :], [128, 128], BF16, q="pool")
    cload("ones", k.din["c_ones"][:, :], [128, 128], BF16, q="pool")
    cload("tri_le", k.din["c_tri_le"][:, :], [128, 128])
    cload("tri_ge", k.din["c_tri_ge"][:, :], [128, 128])
    cload("tri_gt", k.din["c_tri_gt"][:, :], [128, 128])
    cload("tri_lt", k.din["c_tri_lt"][:, :], [128, 128])
    for nm in ("tri_le", "tri_ge", "tri_gt", "tri_lt"):
        cload(nm + "_b", k.din["c_" + nm][:, :], [128, 128], BF16, q="pool")
    eps = P.sb("k_eps", [128, 1], F32)
    P.op("dve", lambda e: e.memset(eps[:], EPS), writes=[eps])
    C["eps"] = eps
    one1 = P.sb("k_one1", [128, 1], F32)
    P.op("dve", lambda e: e.memset(one1[:], 1.0), writes=[one1])
    C["one1"] = one1
    neg1 = P.sb("k_neg1", [128, 1], F32)
    P.op("dve", lambda e: e.memset(neg1[:], -1.0), writes=[neg1])
    C["neg1"] = neg1
    seven = P.sb("k_seven", [128, 1], F32)
    P.op("dve", lambda e: e.memset(seven[:], 7.0), writes=[seven])
    C["seven"] = seven
    big = P.sb("k_big", [128, 1], F32)
    P.op("dve", lambda e: e.memset(big[:], 1e9), writes=[big])
    C["big"] = big
    zero1 = P.sb("k_zero1", [128, 1], F32)
    P.op("dve", lambda e: e.memset(zero1[:], 0.0), writes=[zero1])
    C["zero1"] = zero1
    k.psn = [0]

    def alloc_psum(st, nf, nb=0):
        k.psn[0] += 1
        k.psum = [P.ps("psum%d_%d" % (k.psn[0], i), [128, 512], F32, st) for i in range(nf)]
        k.psbf = [P.ps("psbf%d_%d" % (k.psn[0], i), [128, 1024], BF16, st) for i in range(nb)]
    k.alloc_psum = alloc_psum
    k.modv = P.sb("g_modv", [128, 48, 2], F32)
    k.a1 = P.sb("g_a1", [128, 8, 2], F32)
    k.a2 = P.sb("g_a2", [128, 8, 2], F32)
    cv = P.sb("g_cv", [128, 8, 2], F32)
    k.csil = P.sb("g_csil", [128, 8, 2], F32)
    dma(P, "sp", cv[:], k.din["cvec"][:, :, :], writes=[cv])
    P.op("act", lambda e: e.activation(out=k.csil[:], in_=cv[:], func=AF.Silu), reads=[cv], writes=[k.csil])
    P.barrier()

    if only is not None:
        only(k, 0)
        return finish(k)
    phase0(k)
    P.barrier()
    if stop_after == "p0":
        return finish(k)
    for l in range(n_layers):
        phaseA(k, l)
        P.barrier()
        if stop_after == ("A", l):
            return finish(k)
        phaseMLA(k, l, l == n_layers - 1)
        P.barrier()
        if stop_after == ("MLA", l):
            return finish(k)
        phaseML(k, l)
        P.barrier()
        if stop_after == ("ML", l):
            return finish(k)
        phaseSSD(k, l)
        P.barrier()
        if stop_after == ("SSD", l):
            return finish(k)
        phaseOUT(k, l)
        P.barrier()
        if stop_after == ("OUT", l):
            return finish(k)
        phaseMOE(k, l)
        P.barrier()
        if stop_after == ("MOE", l):
            return finish(k)
    phaseFIN(k)
    return finish(k)


def finish(k):
    k.P.emit()
    k.P.close()
    return k.nc


def phase0(k):
    P, C = k.P, k.C
    with ExitStack() as st:
        k.alloc_psum(st, 8)
        xt = [P.sb("p0_x%d" % i, [128, D], F32, st) for i in range(2)]
        ot = [P.sb("p0_o%d" % i, [128, 8, 128], F32, st) for i in range(2)]
        xr3 = k.scr["xres"].rearrange("(c p) t -> p c t", p=128)
        for ch in range(NCH):
            x = xt[ch % 2]
            o = ot[ch % 2]
            dma(P, "sp", x[:], k.din["xin"][ch * 128:(ch + 1) * 128, :], writes=[x])
            for half in range(2):
                ps = k.psum[(ch * 2 + half) % 4]
                for j in range(4):
                    fc = half * 4 + j
                    P.op("pe", lambda e, ps=ps, j=j, fc=fc, x=x: e.transpose(ps[:, j * 128:(j + 1) * 128], x[:, fc * 128:(fc + 1) * 128], C["ident_f"][:]),
                         reads=[x], writes=[ps])
                eng = "act" if half == 0 else "dve"
                if eng == "act":
                    P.op("act", lambda e, ps=ps, o=o, half=half: e.copy(o[:, half * 4:half * 4 + 4, :], ps[:].rearrange("p (a b) -> p a b", a=4)),
                         reads=[ps], writes=[(o, half)])
                else:
                    P.op("dve", lambda e, ps=ps, o=o, half=half: e.tensor_copy(o[:, half * 4:half * 4 + 4, :], ps[:].rearrange("p (a b) -> p a b", a=4)),
                         reads=[ps], writes=[(o, half)])
            dma(P, "sp", xr3[:, :, ch * 128:(ch + 1) * 128], o[:], reads=[(o, 0), (o, 1)], writes=["xres"])


def compute_mod(k, l, st):
    P, C, din = k.P, k.C, k.din
    wa = [P.sb("md_wa%d" % i, [128, 8, 512], F32, st) for i in range(2)]
    bada = P.sb("md_b", [128, 48], F32, st)
    n1g = P.sb("md_n1", [128, 8], F32, st)
    n2g = P.sb("md_n2", [128, 8], F32, st)
    dma(P, "sp", bada[:], din["b_ada_fm"][l], writes=[bada])
    dma(P, "sp", n1g[:], din["n1g_fm"][l], writes=[n1g])
    dma(P, "sp", n2g[:], din["n2g_fm"][l], writes=[n2g])
    psm = k.psum[7]
    w3 = din["w_ada"][l].rearrange("(kc p) n -> p kc n", p=128)
    for blk in range(12):
        w = wa[blk % 2]
        dma(P, "sp", w[:], w3[:, :, blk * 512:(blk + 1) * 512], writes=[w])
        for o4 in range(4):
            oc = blk * 4 + o4
            for kc in range(8):
                P.op("pe", lambda e, w=w, o4=o4, oc=oc, kc=kc: e.matmul(psm[:, oc * 2:oc * 2 + 2], lhsT=w[:, kc, o4 * 128:(o4 + 1) * 128],
                                                                    rhs=k.csil[:, kc, :], start=(kc == 0), stop=(kc == 7)),
                     reads=[w, k.csil], writes=[psm], acc=True)
    modv, a1, a2 = k.modv, k.a1, k.a2
    P.op("dve", lambda e: e.tensor_tensor(out=modv[:], in0=psm[:, 0:96].rearrange("p (o c) -> p o c", c=2),
                                          in1=bada[:].unsqueeze(2).to_broadcast([128, 48, 2]), op=ALU.add),
         reads=[psm, bada], writes=[modv])
    for (a, ng, base) in ((a1, n1g, 8), (a2, n2g, 32)):
        P.op("dve", lambda e, a=a, base=base: e.tensor_scalar(out=a[:], in0=modv[:, base:base + 8, :], scalar1=1.0, scalar2=None, op0=ALU.add),
             reads=[modv], writes=[a])
        P.op("dve", lambda e, a=a, ng=ng: e.tensor_tensor(out=a[:], in0=a[:], in1=ng[:].unsqueeze(2).to_broadcast([128, 8, 2]), op=ALU.mult),
             reads=[a, ng], writes=[a])


def modulate_tiles(k, st, src3, a, bbase, consume):
    P, C = k.P, k.C
    xt = [P.sb("mo_x%d" % i, [128, 8, 512], F32, st) for i in range(2)]
    sq = [P.sb("mo_sq%d" % i, [128, 8, 512], BF16, st) for i in range(2)]
    rs = [P.sb("mo_rs%d" % i, [128, 512], F32, st) for i in range(2)]
    for ti, (t0, n) in enumerate(TILES):
        mc = 1 if t0 < CTX else 0
        x, q, r = xt[ti % 2], sq[ti % 2], rs[ti % 2]
        ps = k.psum[4 + ti % 2]
        dma(P, "sp", x[:, :, :n], src3[:, :, t0:t0 + n], writes=[x])
        P.op("act", lambda e, x=x, q=q, n=n: e.activation(out=q[:, :, :n], in_=x[:, :, :n], func=AF.Square), reads=[x], writes=[q])
        for c in range(8):
            P.op("pe", lambda e, ps=ps, q=q, c=c, n=n: e.matmul(ps[:, :n], lhsT=C["ones"][:], rhs=q[:, c, :n], start=(c == 0), stop=(c == 7)),
                 reads=[q], writes=[ps], acc=True)
        P.op("act", lambda e, ps=ps, r=r, n=n: e.activation(out=r[:, :n], in_=ps[:, :n], func=AF.Sqrt, bias=C["eps"][:, 0:1], scale=1.0 / D),
             reads=[ps], writes=[r])
        P.op("dve", lambda e, r=r, n=n: e.reciprocal(r[:, :n], r[:, :n]), reads=[r], writes=[r])
        P.op("dve", lambda e, x=x, r=r, n=n: e.tensor_tensor(out=x[:, :, :n], in0=x[:, :, :n], in1=r[:, :n].unsqueeze(1).to_broadcast([128, 8, n]), op=ALU.mult),
             reads=[x, r], writes=[x])
        consume(ti, t0, n, mc, x)


def phaseA(k, l):
    P, C, din, scr = k.P, k.C, k.din, k.scr
    with ExitStack() as st:
        k.alloc_psum(st, 8)
        hn = P.sb("A_hn", [128, 8, TP], BF16, st)
        for (a, b) in ((0, 2), (258, 262), (TP - 2, TP)):
            P.op("pool", lambda e, a=a, b=b: e.memset(hn[:, :, a:b], 0.0), writes=[("hnpad", a)])
        xr3 = scr["xres"].rearrange("(c p) t -> p c t", p=128)

        def consume(ti, t0, n, mc, x):
            c0 = colof(t0)
            for c in range(8):
                P.op("act", lambda e, c=c, x=x, n=n, c0=c0, mc=mc: e.activation(out=hn[:, c, c0:c0 + n], in_=x[:, c, :n], func=AF.Identity,
                                                                           bias=k.modv[:, c, mc:mc + 1], scale=k.a1[:, c, mc:mc + 1]),
                     reads=[x, k.modv, k.a1], writes=[("hn", ti, c)])

        with ExitStack() as st2:
            compute_mod(k, l, st2)
            modulate_tiles(k, st2, xr3, k.a1, 0, consume)
            P.barrier()

        wraw = [P.sb("A_wraw%d" % i, [128, 8, 512], BF16, st) for i in range(2)]
        wexp = [P.sb("A_wexp%d" % i, [128, 5, 8, 512], BF16, st) for i in range(1)] * 2
        cwb = [P.sb("A_cwb%d" % i, [128, 5, 512], F32, st) for i in range(1)] * 2
        bbc = [P.sb("A_bbc%d" % i, [128, 512], F32, st) for i in range(2)]
        bfm = [P.sb("A_bfm%d" % i, [128, 4], F32, st) for i in range(2)]
        stg_b = Rot([P.sb("A_sb%d" % i, [128, 512], BF16, st) for i in range(4)])
        stg_f = Rot([P.sb("A_sf%d" % i, [128, 512], F32, st) for i in range(4)])
        tmpf = Rot([P.sb("A_tf%d" % i, [128, 512], F32, st) for i in range(3)])
        psr = Rot(k.psum[0:4])
        gi = [0]

        def load_w(wname, c0, Cn, conv):
            i = gi[0] % 2
            gi[0] += 1
            wr = wraw[i]
            w3 = din[wname][l].rearrange("(kc p) n -> p kc n", p=128)
            dma(P, "pool", wr[:, :, :Cn], w3[:, :, c0:c0 + Cn], writes=[wr])
            if conv is None:
                return (lambda j, kc: wr[:, kc, :Cn]), [wr], 1
            cwname, cc0 = conv
            cw, we = cwb[i], wexp[i]
            for j in range(5):
                dma(P, "sp", cw[:, j, :Cn], din[cwname][l, j:j + 1, cc0:cc0 + Cn].partition_broadcast(128), writes=[(cw, j)])
            for j in range(5):
                P.op("dve", lambda e, j=j: e.tensor_tensor(out=we[:, j, :, :Cn], in0=wr[:, :, :Cn],
                                                           in1=cw[:, j, :Cn].unsqueeze(1).to_broadcast([128, 8, Cn]), op=ALU.mult),
                     reads=[wr, (cw, j)], writes=[(we, j)])
            return (lambda j, kc: we[:, j, kc, :Cn]), [(we, j) for j in range(5)], 5

        def proj_fm(wname, wc0, Cn, oname, orow0, func, bias_name=None, conv=None, post=None, odt=BF16):
            wv, wkeys, nsh = load_w(wname, wc0, Cn, conv)
            noc = Cn // 128
            bt = None
            if bias_name is not None:
                bt = bfm[gi[0] % 2]
                dma(P, "sp", bt[:, :noc], din[bias_name][l], writes=[bt])
            for ti, (t0, n) in enumerate(TILES):
                c0 = colof(t0)
                for oc in range(noc):
                    ps = psr.next()
                    cnt = 0
                    for j in range(nsh):
                        sh = (j - 2) if nsh == 5 else 0
                        for kc in range(8):
                            P.op("pe", lambda e, ps=ps, j=j, kc=kc, oc=oc, sh=sh, c0=c0, n=n, cnt=cnt: e.matmul(
                                ps[:, :n], lhsT=wv(j, kc)[:, oc * 128:(oc + 1) * 128], rhs=hn[:, kc, c0 + sh:c0 + sh + n],
                                start=(cnt == 0), stop=(cnt == nsh * 8 - 1)), reads=wkeys, writes=[ps], acc=True)
                            cnt += 1
                    sg = stg_b.next() if odt == BF16 else stg_f.next()
                    bias_ap = bt[:, oc:oc + 1] if bt is not None else C["zero1"][:, 0:1]
                    P.op("act", lambda e, sg=sg, ps=ps, n=n, bias_ap=bias_ap: e.activation(out=sg[:, :n], in_=ps[:, :n], func=func, bias=bias_ap, scale=1.0),
                         reads=[ps] + ([bt] if bt is not None else []), writes=[sg])
                    if post is not None and post(oc) is not None:
                        sc = post(oc)
                        P.op("pool", lambda e, sg=sg, n=n, sc=sc: e.tensor_scalar(out=sg[:, :n], in0=sg[:, :n], scalar1=sc, scalar2=None, op0=ALU.mult),
                             reads=[sg], writes=[sg])
                    dma(P, "sp", scr[oname][orow0 + oc * 128:orow0 + (oc + 1) * 128, t0:t0 + n], sg[:, :n], reads=[sg], writes=[(oname, "o")])

        def proj_tm(wname, wc0, Cn, oname, ocol0, func, bias=None, conv=None, odt=BF16, softplus=False):
            wv, wkeys, nsh = load_w(wname, wc0, Cn, conv)
            bt = None
            if bias is not None:
                bname, bc0 = bias
                bt = bbc[gi[0] % 2]
                dma(P, "sp", bt[:, :Cn], din[bname][l, 0:1, bc0:bc0 + Cn].partition_broadcast(128), writes=[bt])
            for ch in range(NCH):
                c0 = colof(ch * 128)
                ps = psr.next()
                cnt = 0
                for j in range(nsh):
                    sh = (j - 2) if nsh == 5 else 0
                    for kc in range(8):
                        P.op("pe", lambda e, ps=ps, j=j, kc=kc, sh=sh, c0=c0, cnt=cnt: e.matmul(
                            ps[:, :Cn], lhsT=hn[:, kc, c0 + sh:c0 + sh + 128], rhs=wv(j, kc),
                            start=(cnt == 0), stop=(cnt == nsh * 8 - 1)), reads=wkeys, writes=[ps], acc=True)
                        cnt += 1
                sg = stg_b.next() if odt == BF16 else stg_f.next()
                src, skey = ps, ps
                if bt is not None:
                    tf = tmpf.next() if (func is not None or softplus) else sg
                    P.op("dve", lambda e, tf=tf, ps=ps: e.tensor_tensor(out=tf[:, :Cn], in0=ps[:, :Cn], in1=bt[:, :Cn], op=ALU.add),
                         reads=[ps, bt], writes=[tf])
                    src, skey = tf, tf
                if softplus:
                    tf2 = tmpf.next()
                    P.op("act", lambda e, tf2=tf2, src=src: e.activation(out=tf2[:, :Cn], in_=src[:, :Cn], func=AF.Exp), reads=[skey], writes=[tf2])
                    P.op("act", lambda e, tf2=tf2, sg=sg: e.activation(out=sg[:, :Cn], in_=tf2[:, :Cn], func=AF.Ln, bias=C["one1"][:, 0:1], scale=1.0),
                         reads=[tf2], writes=[sg])
                elif func is not None:
                    P.op("act", lambda e, sg=sg, src=src: e.activation(out=sg[:, :Cn], in_=src[:, :Cn], func=func), reads=[skey], writes=[sg])
                dma(P, "sp", scr[oname][ch * 128:(ch + 1) * 128, ocol0:ocol0 + Cn], sg[:, :Cn], reads=[sg], writes=[(oname, "o")])

        ID, SILU, SIG = AF.Identity, AF.Silu, AF.Sigmoid
        proj_fm("w_q", 0, 384, "uq", 0, ID)
        proj_fm("w_kv", 0, 256, "ukv", 0, ID)
        proj_fm("w_kr", 0, 256, "kr", 0, ID)
        proj_fm("w_mqk", 0, 512, "mqk", 0, SILU, bias_name="ml_cb_fm", conv=("ml_cw", 0), post=lambda oc: 0.125 if oc >= 2 else None)
        proj_tm("w_mv", 0, 512, "mv", 0, ID)
        proj_tm("w_mo", 0, 512, "mo", 0, SIG)
        proj_tm("w_mif", 0, 16, "mif", 0, None, bias=("ml_gb", 0), odt=F32)
        proj_fm("w_sB", 0, 512, "sBf", 0, SILU, bias_name="s_cbB_fm", conv=("s_cw", 1024))
        proj_fm("w_sC", 0, 512, "sCf", 0, SILU, bias_name="s_cbC_fm", conv=("s_cw", 1536))
        proj_tm("w_sB", 0, 512, "sBt", 0, SILU, bias=("s_cb", 1024), conv=("s_cw", 1024))
        for h in range(2):
            proj_tm("w_sz", h * 512, 512, "sz", h * 512, SILU)
            proj_tm("w_sx", h * 512, 512, "sx", h * 512, SILU, bias=("s_cb", h * 512), conv=("s_cw", h * 512))
        proj_tm("w_sdt", 0, 32, "sdt", 0, None, bias=("s_dtb", 0), odt=F32, softplus=True)
        for g in range(6):
            proj_fm("w_g", g * 512, 512, "gg", g * 512, SIG)


def rms_rows(k, st, name, src, nch, dst, tag):
    P, C = k.P, k.C
    nf = nch * 128
    s3 = src.rearrange("(c p) t -> p c t", p=128)
    xt = [P.sb("%s_x%d" % (tag, i), [128, nch, 512], BF16, st) for i in range(2)]
    sq = [P.sb("%s_q%d" % (tag, i), [128, nch, 512], BF16, st) for i in range(2)]
    rs = [P.sb("%s_r%d" % (tag, i), [128, 512], F32, st) for i in range(2)]
    for ti, (t0, n) in enumerate(TILES):
        x, q, r = xt[ti % 2], sq[ti % 2], rs[ti % 2]
        ps = k.psum[5 + ti % 2]
        dma(P, "sp", x[:, :, :n], s3[:, :, t0:t0 + n], writes=[x])
        P.op("act", lambda e, x=x, q=q, n=n: e.activation(out=q[:, :, :n], in_=x[:, :, :n], func=AF.Square), reads=[x], writes=[q])
        for c in range(nch):
            P.op("pe", lambda e, ps=ps, q=q, c=c, n=n: e.matmul(ps[:, :n], lhsT=C["ones"][:], rhs=q[:, c, :n], start=(c == 0), stop=(c == nch - 1)),
                 reads=[q], writes=[ps], acc=True)
        P.op("act", lambda e, ps=ps, r=r, n=n: e.activation(out=r[:, :n], in_=ps[:, :n], func=AF.Sqrt, bias=C["eps"][:, 0:1], scale=1.0 / nf),
             reads=[ps], writes=[r])
        P.op("dve", lambda e, r=r, n=n: e.reciprocal(r[:, :n], r[:, :n]), reads=[r], writes=[r])
        P.op("dve", lambda e, x=x, r=r, n=n, t0=t0: e.tensor_tensor(out=dst[:, :, t0:t0 + n], in0=x[:, :, :n],
                                                                   in1=r[:, :n].unsqueeze(1).to_broadcast([128, nch, n]), op=ALU.mult),
             reads=[x, r], writes=[(dst, ti)])


def phaseMLA(k, l, last):
    P, C, din, scr = k.P, k.C, k.din, k.scr
    with ExitStack() as st:
        k.alloc_psum(st, 8)
        uqn = P.sb("M_uqn", [128, 3, T], BF16, st)
        ukvn = P.sb("M_ukvn", [128, 2, T], BF16, st)
        Kh = P.sb("M_Kh", [128, T], BF16, st)
        Qh = P.sb("M_Qh", [128, T], BF16, st)
        Vh = P.sb("M_Vh", [128, NCH, 128], BF16, st)
        wuq = P.sb("M_wuq", [128, 3, 2048], BF16, st)
        wuk = P.sb("M_wuk", [128, 2, 512], BF16, st)
        wuv = P.sb("M_wuv", [128, 2, 512], BF16, st)
        qn = P.sb("M_qn", [128, 3], F32, st)
        kvn = P.sb("M_kvn", [128, 2], F32, st)
        kmax = P.sb("M_kmax", [128, 1], F32, st)
        kmt = P.sb("M_kmt", [128, 1], F32, st)
        with ExitStack() as st2:
            rms_rows(k, st2, "uq", scr["uq"], 3, uqn, "Mq")
            rms_rows(k, st2, "ukv", scr["ukv"], 2, ukvn, "Mk")
            P.barrier()
        P.op("pool", lambda e: e.memset(Kh[96:128, :], 0.0), writes=["Kc0"])
        P.op("pool", lambda e: e.memset(Kh[96:97, :], 1.0), reads=["Kc0"], writes=["Kc1"])
        P.op("pool", lambda e: e.memset(Qh[96:128, :], 0.0), writes=["Qc0"])
        P.op("pool", lambda e: e.memset(Vh[:, :, 64:128], 1.0), writes=["Vc0"])
        dma(P, "sp", qn[:], din["qn_fm"][l], writes=[qn])
        dma(P, "sp", kvn[:], din["kvn_fm"][l], writes=[kvn])
        dma(P, "pool", wuq[:], din["w_uq"][l].rearrange("(kc p) n -> p kc n", p=128), writes=[wuq])
        dma(P, "pool", wuk[:], din["w_uk"][l].rearrange("(kc p) n -> p kc n", p=128), writes=[wuk])
        dma(P, "pool", wuv[:], din["w_uv"][l].rearrange("(kc p) n -> p kc n", p=128), writes=[wuv])
        for kc in range(3):
            P.op("dve", lambda e, kc=kc: e.tensor_scalar(out=wuq[:, kc, :], in0=wuq[:, kc, :], scalar1=qn[:, kc:kc + 1], scalar2=None, op0=ALU.mult),
                 reads=[wuq, qn], writes=[wuq])
        for kc in range(2):
            P.op("dve", lambda e, kc=kc: e.tensor_scalar(out=wuk[:, kc, :], in0=wuk[:, kc, :], scalar1=kvn[:, kc:kc + 1], scalar2=None, op0=ALU.mult),
                 reads=[wuk, kvn], writes=[wuk])
            P.op("dve", lambda e, kc=kc: e.tensor_scalar(out=wuv[:, kc, :], in0=wuv[:, kc, :], scalar1=kvn[:, kc:kc + 1], scalar2=None, op0=ALU.mult),
                 reads=[wuv, kvn], writes=[wuv])
        rp = [P.sb("M_rp%d" % i, [128, 2, 512], F32, st) for i in range(2)]
        kr = [P.sb("M_kr%d" % i, [128, 2, 512], BF16, st) for i in range(2)]
        t1 = [P.sb("M_t1%d" % i, [128, 512], F32, st) for i in range(2)]
        t2 = [P.sb("M_t2%d" % i, [128, 512], F32, st) for i in range(2)]
        for ti, (t0, n) in enumerate(TILES):
            r_, kr_, a_, b_ = rp[ti % 2], kr[ti % 2], t1[ti % 2], t2[ti % 2]
            dma(P, "sp", r_[64:96, :, :n], din["c_rope"][64:96, :, t0:t0 + n], writes=[r_])
            dma(P, "sp", kr_[64:96, 0, :n], scr["kr"][64:96, t0:t0 + n], writes=[(kr_, 0)])
            dma(P, "sp", kr_[64:96, 1, :n], scr["kr"][192:224, t0:t0 + n], writes=[(kr_, 1)])
            P.op("dve", lambda e, r_=r_, kr_=kr_, a_=a_, n=n: e.tensor_tensor(out=a_[64:96, :n], in0=kr_[64:96, 0, :n], in1=r_[64:96, 0, :n], op=ALU.mult),
                 reads=[r_, (kr_, 0)], writes=[a_])
            P.op("dve", lambda e, r_=r_, kr_=kr_, b_=b_, n=n: e.tensor_tensor(out=b_[64:96, :n], in0=kr_[64:96, 1, :n], in1=r_[64:96, 1, :n], op=ALU.mult),
                 reads=[r_, (kr_, 1)], writes=[b_])
            P.op("pool", lambda e, a_=a_, b_=b_, n=n, t0=t0: e.tensor_tensor(out=Kh[64:96, t0:t0 + n], in0=a_[64:96, :n], in1=b_[64:96, :n], op=ALU.add),
                 reads=[a_, b_], writes=[("Kr", ti)])
        P.barrier()

        sqb = [P.sb("M_sq%d" % i, [128, 512], BF16, st) for i in range(2)]
        Et = Rot([P.sb("M_E%d" % i, [128, 512], BF16, st) for i in range(3)])
        dn = [P.sb("M_dn%d" % i, [64, 512], F32, st) for i in range(2)]
        ao = [P.sb("M_ao%d" % i, [64, 512], BF16, st) for i in range(2)]
        mt = [P.sb("M_mt%d" % i, [128, 512], F32, st) for i in range(2)]
        ps_s = Rot(k.psum[0:3])
        ps_o = Rot(k.psum[3:5])
        ps_p = Rot(k.psum[5:7])
        ps_m = k.psum[7]
        for h in range(8):
            for ti, (t0, n) in enumerate(TILES):
                ps = ps_p.next()
                for kc in range(2):
                    P.op("pe", lambda e, ps=ps, kc=kc, n=n, t0=t0, h=h: e.matmul(ps[0:64, :n], lhsT=wuk[:, kc, h * 64:(h + 1) * 64], rhs=ukvn[:, kc, t0:t0 + n],
                                                                            start=(kc == 0), stop=(kc == 1)), reads=[wuk], writes=[ps], acc=True)
                P.op("act", lambda e, ps=ps, n=n, t0=t0: e.copy(Kh[0:64, t0:t0 + n], ps[0:64, :n]), reads=[ps], writes=[("Kn", ti)])
                q = sqb[ti % 2]
                P.op("act", lambda e, q=q, n=n, t0=t0: e.activation(out=q[0:96, :n], in_=Kh[0:96, t0:t0 + n], func=AF.Square), reads=[("Kn", ti)], writes=[q])
                P.op("pe", lambda e, q=q, n=n: e.matmul(ps_m[:, :n], lhsT=C["ones"][0:96, :], rhs=q[0:96, :n], start=True, stop=True), reads=[q], writes=[ps_m])
                if ti == 0:
                    P.op("dve", lambda e, n=n: e.reduce_max(out=kmax[:], in_=ps_m[:, :n], axis=AX.X), reads=[ps_m], writes=[kmax])
                else:
                    P.op("dve", lambda e, n=n: e.reduce_max(out=kmt[:], in_=ps_m[:, :n], axis=AX.X), reads=[ps_m], writes=[kmt])
                    P.op("dve", lambda e: e.tensor_tensor(out=kmax[:], in0=kmax[:], in1=kmt[:], op=ALU.max), reads=[kmax, kmt], writes=[kmax])
            for c0 in range(0, NCH, 8):
                nb = min(8, NCH - c0)
                ps = ps_p.next()
                for j in range(nb):
                    ch = c0 + j
                    for kc in range(2):
                        P.op("pe", lambda e, ps=ps, j=j, ch=ch, kc=kc, h=h: e.matmul(ps[:, j * 64:(j + 1) * 64], lhsT=ukvn[:, kc, ch * 128:(ch + 1) * 128],
                                                                                rhs=wuv[:, kc, h * 64:(h + 1) * 64], start=(kc == 0), stop=(kc == 1)),
                             reads=[wuv], writes=[ps], acc=True)
                P.op("act", lambda e, ps=ps, c0=c0, nb=nb: e.copy(Vh[:, c0:c0 + nb, 0:64], ps[:, :nb * 64].rearrange("p (a b) -> p a b", b=64)),
                     reads=[ps], writes=[("Vh", c0)])
            for ti, (t0, n) in enumerate(TILES):
                pr, pw = ps_p.next(), ps_p.next()
                for kc in range(3):
                    P.op("pe", lambda e, pr=pr, kc=kc, n=n, t0=t0, h=h: e.matmul(pr[:, :n], lhsT=wuq[:, kc, h * 128:(h + 1) * 128], rhs=uqn[:, kc, t0:t0 + n],
                                                                            start=(kc == 0), stop=(kc == 2)), reads=[wuq], writes=[pr], acc=True)
                for kc in range(3):
                    P.op("pe", lambda e, pw=pw, kc=kc, n=n, t0=t0, h=h: e.matmul(pw[:, :n], lhsT=wuq[:, kc, 1024 + h * 128:1024 + (h + 1) * 128], rhs=uqn[:, kc, t0:t0 + n],
                                                                            start=(kc == 0), stop=(kc == 2)), reads=[wuq], writes=[pw], acc=True)
                r_, a_, b_ = rp[ti % 2], t1[ti % 2], t2[ti % 2]
                dma(P, "sp", r_[64:96, :, :n], din["c_rope"][64:96, :, t0:t0 + n], writes=[r_])
                P.op("act", lambda e, pr=pr, n=n, t0=t0: e.copy(Qh[0:64, t0:t0 + n], pr[0:64, :n]), reads=[pr], writes=[("Qn", ti)])
                P.op("dve", lambda e, pr=pr, r_=r_, a_=a_, n=n: e.tensor_tensor(out=a_[64:96, :n], in0=pr[64:96, :n], in1=r_[64:96, 0, :n], op=ALU.mult),
                     reads=[pr, r_], writes=[a_])
                P.op("dve", lambda e, pw=pw, r_=r_, b_=b_, n=n: e.tensor_tensor(out=b_[64:96, :n], in0=pw[64:96, :n], in1=r_[64:96, 1, :n], op=ALU.mult),
                     reads=[pw, r_], writes=[b_])
                P.op("pool", lambda e, a_=a_, b_=b_, n=n, t0=t0: e.tensor_tensor(out=Qh[64:96, t0:t0 + n], in0=a_[64:96, :n], in1=b_[64:96, :n], op=ALU.add),
                     reads=[a_, b_], writes=[("Qr", ti)])
                q = sqb[ti % 2]
                P.op("act", lambda e, q=q, n=n, t0=t0: e.activation(out=q[0:96, :n], in_=Qh[0:96, t0:t0 + n], func=AF.Square),
                     reads=[("Qn", ti), ("Qr", ti)], writes=[q])
                P.op("pe", lambda e, q=q, n=n: e.matmul(ps_m[:, :n], lhsT=C["ones"][0:96, :], rhs=q[0:96, :n], start=True, stop=True), reads=[q], writes=[ps_m])
                m_ = mt[ti % 2]
                P.op("act", lambda e, m_=m_, n=n: e.activation(out=m_[96:97, :n], in_=ps_m[96:97, :n], func=AF.Sqrt, bias=C["zero1"][96:97, 0:1], scale=kmax[96:97, 0:1]),
                     reads=[ps_m, kmax], writes=[m_])
                P.op("dve", lambda e, m_=m_, n=n, t0=t0: e.tensor_scalar(out=Qh[96:97, t0:t0 + n], in0=m_[96:97, :n], scalar1=-1.0, scalar2=None, op0=ALU.mult),
                     reads=[m_], writes=[("Qm", ti)])
            for ti, (t0, n) in enumerate(TILES):
                chunks = [0, 1] if t0 < CTX else list(range(NCH))
                po = ps_o.next()
                for ci, kc in enumerate(chunks):
                    ps = ps_s.next()
                    kti = 0 if kc < 2 else 1 + (kc - 2) // 4
                    P.op("pe", lambda e, ps=ps, kc=kc, n=n, t0=t0: e.matmul(ps[:, :n], lhsT=Kh[:, kc * 128:(kc + 1) * 128], rhs=Qh[:, t0:t0 + n], start=True, stop=True),
                         reads=[("Kn", kti), ("Qn", ti), ("Qr", ti), ("Qm", ti)], writes=[ps])
                    E = Et.next()
                    P.op("act", lambda e, ps=ps, E=E, n=n: e.activation(out=E[:, :n], in_=ps[:, :n], func=AF.Exp, scale=MLA_SCALE), reads=[ps], writes=[E])
                    P.op("pe", lambda e, po=po, E=E, kc=kc, n=n, ci=ci, nc_=len(chunks): e.matmul(po[:, :n], lhsT=Vh[:, kc, :], rhs=E[:, :n], start=(ci == 0), stop=(ci == nc_ - 1)),
                         reads=[E, ("Vh", (kc // 8) * 8)], writes=[po], acc=True)
                d_, a_ = dn[ti % 2], ao[ti % 2]
                P.op("act", lambda e, po=po, d_=d_, n=n: e.copy(d_[:, :n], po[64:128, :n]), reads=[po], writes=[d_])
                P.op("dve", lambda e, d_=d_, n=n: e.reciprocal(d_[:, :n], d_[:, :n]), reads=[d_], writes=[d_])
                P.op("dve", lambda e, po=po, d_=d_, a_=a_, n=n: e.tensor_tensor(out=a_[:, :n], in0=po[0:64, :n], in1=d_[:, :n], op=ALU.mult),
                     reads=[po, d_], writes=[a_])
                dma(P, "sp", scr["att"][h * 64:(h + 1) * 64, t0:t0 + n], a_[:, :n], reads=[a_], writes=["att_o"])


def scan_order(rev):
    return list(range(NCH)) if not rev else [1, 0] + list(range(NCH - 1, 1, -1))


def phaseML(k, l):
    P, C, din, scr = k.P, k.C, k.din, k.scr
    with ExitStack() as st:
        k.alloc_psum(st, 7, 1)
        NG = NCH * 8
        gt = P.sb("L_gt", [128, NCH, 16], F32, st)
        lf = P.sb("L_lf", [128, NCH, 2, 4], F32, st)
        aex = P.sb("L_aex", [128, NG], F32, st)
        esrc = P.sb("L_esrc", [128, NG], F32, st)
        iosc = P.sb("L_iosc", [128, NG], F32, st)
        etot = P.sb("L_etot", [128, NG], F32, st)
        tmpg = P.sb("L_tmpg", [128, NCH, 2, 4], F32, st)
        ngb = P.sb("L_ngb", [128, 512], F32, st)
        dma(P, "sp", gt[:], scr["mif"].rearrange("(c p) g -> p c g", p=128), writes=[gt])
        dma(P, "sp", ngb[:], din["ml_ng"][l, 0:1, :].partition_broadcast(128), writes=[ngb])
        gt5 = gt[:].rearrange("p c (d i h) -> p c d i h", d=2, i=2)
        P.op("act", lambda e: e.activation(out=tmpg[:], in_=gt5[:, :, :, 1, :], func=AF.Exp, scale=-1.0), reads=[gt], writes=[tmpg])
        P.op("act", lambda e: e.activation(out=tmpg[:], in_=tmpg[:], func=AF.Ln, bias=C["one1"][:, 0:1], scale=1.0), reads=[tmpg], writes=[tmpg])
        P.op("dve", lambda e: e.tensor_scalar(out=lf[:], in0=tmpg[:], scalar1=-1.0, scalar2=None, op0=ALU.mult), reads=[tmpg], writes=[lf])
        psA, psT = k.psum[5], k.psum[6]
        for c in range(NCH):
            P.op("pe", lambda e, c=c: e.matmul(psA[:, c * 8:c * 8 + 4], lhsT=C["tri_gt"][:], rhs=lf[:, c, 0, :], start=True, stop=True), reads=[lf], writes=[psA], acc=True)
            P.op("pe", lambda e, c=c: e.matmul(psA[:, c * 8 + 4:c * 8 + 8], lhsT=C["tri_lt"][:], rhs=lf[:, c, 1, :], start=True, stop=True), reads=[lf], writes=[psA], acc=True)
        P.op("pe", lambda e: e.matmul(psT[:, :NG], lhsT=C["ones_f"][:], rhs=lf[:].rearrange("p c d h -> p (c d h)"), start=True, stop=True), reads=[lf], writes=[psT])
        P.op("dve", lambda e: e.tensor_copy(aex[:], psA[:, :NG]), reads=[psA], writes=[aex])
        P.op("act", lambda e: e.activation(out=iosc[:], in_=aex[:], func=AF.Exp), reads=[aex], writes=[iosc])
        P.op("act", lambda e: e.activation(out=etot[:], in_=psT[:, :NG], func=AF.Exp), reads=[psT], writes=[etot])
        P.op("dve", lambda e: e.tensor_tensor(out=esrc[:].rearrange("p (c d h) -> p c d h", d=2, h=4), in0=aex[:].rearrange("p (c d h) -> p c d h", d=2, h=4),
                                              in1=gt5[:, :, :, 0, :], op=ALU.add), reads=[aex, gt], writes=[esrc])
        P.op("act", lambda e: e.activation(out=esrc[:], in_=esrc[:], func=AF.Exp), reads=[esrc], writes=[esrc])
        P.barrier()

        gidx = lambda c, d, h: c * 8 + d * 4 + h
        qk = [P.sb("L_qk%d" % i, [128, 4, 128], BF16, st) for i in range(3)]
        va = [P.sb("L_va%d" % i, [128, 4, 129], BF16, st) for i in range(3)]
        for v_ in va:
            P.op("pool", lambda e, v_=v_: e.memset(v_[:, :, 128:129], 1.0), writes=[(v_, "one")])
        ktm = [P.sb("L_ktm%d" % i, [128, 2, 128], BF16, st) for i in range(2)]
        SM = Rot([P.sb("L_SM%d" % i, [128, 128], BF16, st) for i in range(4)])
        vp = Rot([P.sb("L_vp%d" % i, [128, 129], BF16, st) for i in range(4)])
        Cf = P.sb("L_Cf", [128, 4, 129], F32, st)
        Cb = P.sb("L_Cb", [128, 4, 129], BF16, st)
        ctmp = Rot([P.sb("L_ct%d" % i, [128, 129], F32, st) for i in range(4)])
        mx = Rot([P.sb("L_mx%d" % i, [128, 1], F32, st) for i in range(8)])
        hch = [P.sb("L_h%d" % i, [128, 512], F32, st) for i in range(2)]
        hpv = [P.sb("L_hp%d" % i, [128, 512], F32, st) for i in range(2)]
        mog = [P.sb("L_mo%d" % i, [128, 512], BF16, st) for i in range(2)]
        sq = P.sb("L_sq", [128, 512], F32, st)
        ss = P.sb("L_ss", [128, 4], F32, st)
        mtm = P.sb("L_mtm", [128, 512], BF16, st)
        mfm = [P.sb("L_mfm%d" % i, [128, 4, 128], BF16, st) for i in range(2)]
        ps_s = Rot(k.psum[0:2])
        ps_p = Rot(k.psum[2:4])
        ps_c = Rot(k.psum[4:6])
        pst = k.psbf[0]
        mqk3 = scr["mqk"].rearrange("(c p) t -> p c t", p=128)
        mout3 = scr["mout"].rearrange("(c p) t -> p c t", p=128)
        for d in range(2):
            order = scan_order(d == 1)
            mask = C["tri_le"] if d == 0 else C["tri_ge"]
            if d == 1:
                P.barrier()
            P.op("dve", lambda e: e.memset(Cf[:], 0.0), writes=[(Cf, h) for h in range(4)])
            P.op("pool", lambda e: e.memset(Cb[:], 0.0), writes=[(Cb, h) for h in range(4)])
            for oi, c in enumerate(order):
                q_, v_, kt_ = qk[oi % 3], va[oi % 3], ktm[oi % 2]
                dma(P, "sp", q_[:], mqk3[:, :, c * 128:(c + 1) * 128], writes=[q_])
                dma(P, "sp", v_[:, :, 0:128], scr["mv"][c * 128:(c + 1) * 128, :].rearrange("p (h v) -> p h v", h=4), writes=[v_])
                for j in range(2):
                    P.op("pe", lambda e, q_=q_, j=j: e.transpose(pst[:, j * 128:(j + 1) * 128], q_[:, 2 + j, :], C["ident"][:]), reads=[q_], writes=[pst])
                P.op("act", lambda e, kt_=kt_: e.copy(kt_[:], pst[:, 0:256].rearrange("p (a b) -> p a b", a=2)), reads=[pst], writes=[kt_])
                h_ = hch[oi % 2]
                if d == 1:
                    hp_, mo_ = hpv[oi % 2], mog[oi % 2]
                    dma(P, "sp", hp_[:], scr["hacc"][c * 128:(c + 1) * 128, :], writes=[hp_])
                    dma(P, "sp", mo_[:], scr["mo"][c * 128:(c + 1) * 128, :], writes=[mo_])
                for h in range(4):
                    pb = (h % 2) * 64
                    g = gidx(c, d, h)
                    pss, psp, psc = ps_s.next(), ps_p.next(), ps_c.next()
                    P.op("pe", lambda e, pss=pss, q_=q_, h=h, pb=pb: e.matmul(pss[:, 0:128], lhsT=q_[pb:pb + 64, 2 + h // 2, :], rhs=q_[pb:pb + 64, h // 2, :], start=True, stop=True),
                         reads=[q_], writes=[pss])
                    sm = SM.next()
                    P.op("dve", lambda e, pss=pss, sm=sm, mask=mask: e.tensor_tensor(out=sm[:], in0=pss[:, 0:128], in1=mask[:], op=ALU.mult), reads=[pss], writes=[sm])
                    vp_ = vp.next()
                    P.op("pool", lambda e, vp_=vp_, v_=v_, h=h, g=g: e.tensor_scalar(out=vp_[:], in0=v_[:, h, :], scalar1=esrc[:, g:g + 1], scalar2=None, op0=ALU.mult),
                         reads=[v_, (v_, "one")], writes=[vp_])
                    P.op("pe", lambda e, psp=psp, sm=sm, vp_=vp_: e.matmul(psp[:, 0:129], lhsT=sm[:], rhs=vp_[:], start=True, stop=False), reads=[sm, vp_], writes=[psp], acc=True)
                    P.op("pe", lambda e, psp=psp, q_=q_, h=h, pb=pb: e.matmul(psp[:, 0:129], lhsT=q_[pb:pb + 64, h // 2, :], rhs=Cb[pb:pb + 64, h, :], start=False, stop=True),
                         reads=[q_, (Cb, h)], writes=[psp], acc=True)
                    m_ = mx.next()
                    P.op("dve", lambda e, m_=m_, psp=psp, g=g: e.tensor_scalar(out=m_[:], in0=psp[:, 128:129], scalar1=C["neg1"][:, 0:1], scalar2=iosc[:, g:g + 1], op0=ALU.mult, op1=ALU.max),
                         reads=[psp], writes=[m_])
                    P.op("dve", lambda e, m_=m_, psp=psp: e.tensor_tensor(out=m_[:], in0=psp[:, 128:129], in1=m_[:], op=ALU.max),
                         reads=[psp, m_], writes=[m_])
                    P.op("dve", lambda e, m_=m_: e.reciprocal(m_[:], m_[:]), reads=[m_], writes=[m_])
                    if d == 0:
                        P.op("act", lambda e, h_=h_, psp=psp, m_=m_, h=h: e.activation(out=h_[:, h * 128:(h + 1) * 128], in_=psp[:, 0:128], func=AF.Identity,
                                                                                 bias=C["zero1"][:, 0:1], scale=m_[:, 0:1]), reads=[psp, m_], writes=[(h_, h)])
                    else:
                        P.op("dve", lambda e, h_=h_, psp=psp, m_=m_, h=h, hp_=hp_: e.scalar_tensor_tensor(out=h_[:, h * 128:(h + 1) * 128], in0=psp[:, 0:128], scalar=m_[:, 0:1],
                                                                                                   in1=hp_[:, h * 128:(h + 1) * 128], op0=ALU.mult, op1=ALU.add),
                             reads=[psp, m_, hp_], writes=[(h_, h)])
                    if oi + 1 < len(order):
                        gn = gidx(order[oi + 1], d, h)
                        P.op("pe", lambda e, psc=psc, kt_=kt_, h=h, vp_=vp_: e.matmul(psc[:, 0:129], lhsT=kt_[:, h // 2, :], rhs=vp_[:], start=True, stop=True),
                             reads=[kt_, vp_], writes=[psc])
                        ct = ctmp.next()
                        P.op("dve", lambda e, ct=ct, psc=psc, h=h, pb=pb: e.tensor_tensor(out=ct[pb:pb + 64, :], in0=psc[pb:pb + 64, 0:129], in1=Cf[pb:pb + 64, h, :], op=ALU.add),
                             reads=[psc, (Cf, h)], writes=[ct])
                        P.op("act", lambda e, ct=ct, h=h, pb=pb, gn=gn: e.activation(out=Cf[pb:pb + 64, h, :], in_=ct[pb:pb + 64, :], func=AF.Identity,
                                                                               bias=C["zero1"][pb:pb + 64, 0:1], scale=etot[pb:pb + 64, gn:gn + 1]), reads=[ct], writes=[(Cf, h)])
                        P.op("act", lambda e, ct=ct, h=h, pb=pb, gn=gn: e.activation(out=Cb[pb:pb + 64, h, :], in_=ct[pb:pb + 64, :], func=AF.Identity,
                                                                               bias=C["zero1"][pb:pb + 64, 0:1], scale=etot[pb:pb + 64, gn:gn + 1]), reads=[ct], writes=[(Cb, h)])
                hkeys = [(h_, h) for h in range(4)]
                if d == 0:
                    dma(P, "sp", scr["hacc"][c * 128:(c + 1) * 128, :], h_[:], reads=hkeys, writes=["hacc_o"])
                else:
                    P.op("act", lambda e, h_=h_: e.activation(out=sq[:], in_=h_[:], func=AF.Square), reads=hkeys, writes=[sq])
                    P.op("dve", lambda e: e.reduce_sum(out=ss[:], in_=sq[:].rearrange("p (h v) -> p h v", h=4), axis=AX.X), reads=[sq], writes=[ss])
                    P.op("act", lambda e: e.activation(out=ss[:], in_=ss[:], func=AF.Sqrt, bias=C["eps"][:, 0:1], scale=1.0 / 128), reads=[ss], writes=[ss])
                    P.op("dve", lambda e: e.reciprocal(ss[:], ss[:]), reads=[ss], writes=[ss])
                    P.op("dve", lambda e, h_=h_: e.tensor_tensor(out=sq[:].rearrange("p (h v) -> p h v", h=4), in0=h_[:].rearrange("p (h v) -> p h v", h=4),
                                                                 in1=ss[:].unsqueeze(2).to_broadcast([128, 4, 128]), op=ALU.mult), reads=hkeys + [ss], writes=[sq])
                    P.op("pool", lambda e: e.tensor_tensor(out=sq[:], in0=sq[:], in1=ngb[:], op=ALU.mult), reads=[sq], writes=[sq])
                    P.op("dve", lambda e, mo_=mo_: e.tensor_tensor(out=mtm[:], in0=sq[:], in1=mo_[:], op=ALU.mult), reads=[sq, mo_], writes=[mtm])
                    for j in range(4):
                        P.op("pe", lambda e, j=j: e.transpose(pst[:, 512 + j * 128:512 + (j + 1) * 128], mtm[:, j * 128:(j + 1) * 128], C["ident"][:]), reads=[mtm], writes=[(pst, "m")])
                    mf = mfm[oi % 2]
                    P.op("act", lambda e, mf=mf: e.copy(mf[:], pst[:, 512:1024].rearrange("p (a b) -> p a b", a=4)), reads=[(pst, "m")], writes=[mf])
                    dma(P, "sp", mout3[:, :, c * 128:(c + 1) * 128], mf[:], reads=[mf], writes=["mout_o"])


SSD_DEBUG_LIMIT = None
SSD_PRE_STOP = None


def phaseSSD(k, l):
    P, C, din, scr = k.P, k.C, k.din, k.scr
    with ExitStack() as st:
        k.alloc_psum(st, 7, 1)
        NW = NCH * 32
        dtt = P.sb("S_dtt", [128, 2, NCH, 16], F32, st)
        dta = P.sb("S_dta", [128, 2, NCH, 16], F32, st)
        ainc = P.sb("S_ainc", [128, 2, NCH, 16], F32, st)
        nain = P.sb("S_nain", [128, 2, NCH, 16], F32, st)
        eain = P.sb("S_eain", [128, 2, NCH, 16], F32, st)
        wsrc = P.sb("S_wsrc", [128, 2, NCH, 16], F32, st)
        etot = P.sb("S_etot", [128, 2, NCH, 16], F32, st)
        abc = P.sb("S_abc", [128, 32], F32, st)
        dbc = P.sb("S_dbc", [128, 16], F32, st)
        sngb = P.sb("S_sngb", [128, 1024], F32, st)
        negm = [P.sb("S_negm%d" % i, [128, 4, 128], BF16, st) for i in range(2)]
        dsp = [P.sb("S_dsp%d" % i, [128, 2, NCH, 16], BF16, st) for i in range(3)]
        dr1 = P.sb("S_dr1", [128, 2, NCH, 16], F32, st)
        for d_ in range(2):
            dma(P, "sp", dtt[:, d_], scr["sdt"][:, d_ * 16:(d_ + 1) * 16].rearrange("(c p) h -> p c h", p=128), writes=[(dtt, d_)])
        dma(P, "sp", abc[:], din["s_alog"][l, 0:1, :].partition_broadcast(128), writes=[abc])
        dma(P, "sp", dbc[:], din["s_d"][l, 0:1, :].partition_broadcast(128), writes=[dbc])
        dma(P, "sp", sngb[:], din["s_ng"][l, 0:1, :].partition_broadcast(128), writes=[sngb])
        if SSD_PRE_STOP == 1:
            return
        P.op("act", lambda e: e.activation(out=abc[:], in_=abc[:], func=AF.Exp), reads=[abc], writes=[abc])
        P.op("dve", lambda e: e.tensor_scalar(out=abc[:], in0=abc[:], scalar1=-1.0, scalar2=None, op0=ALU.mult), reads=[abc], writes=[abc])
        for d_ in range(2):
            P.op("dve", lambda e, d_=d_: e.tensor_tensor(out=dta[:, d_], in0=dtt[:, d_], in1=abc[:, d_ * 16:(d_ + 1) * 16].unsqueeze(1).to_broadcast([128, NCH, 16]), op=ALU.mult), reads=[(dtt, d_), abc], writes=[(dta, d_)])
        P.op("dve", lambda e: e.tensor_copy(dsp[0][:], dta[:]), reads=[(dta, 0), (dta, 1)], writes=[dsp[0]])
        P.op("dve", lambda e: e.tensor_tensor(out=dr1[:], in0=dta[:], in1=dsp[0][:], op=ALU.subtract), reads=[(dta, 0), (dta, 1), dsp[0]], writes=[dr1])
        P.op("dve", lambda e: e.tensor_copy(dsp[1][:], dr1[:]), reads=[dr1], writes=[dsp[1]])
        P.op("dve", lambda e: e.tensor_tensor(out=dsp[2][:], in0=dr1[:], in1=dsp[1][:], op=ALU.subtract), reads=[dr1, dsp[1]], writes=[dsp[2]])
        P.op("dve", lambda e: e.tensor_scalar(out=negm[0][:], in0=C["tri_gt"][:].unsqueeze(1).to_broadcast([128, 4, 128]), scalar1=NEG, scalar2=None, op0=ALU.mult), writes=[negm[0]])
        P.op("dve", lambda e: e.tensor_scalar(out=negm[1][:], in0=C["tri_lt"][:].unsqueeze(1).to_broadcast([128, 4, 128]), scalar1=NEG, scalar2=None, op0=ALU.mult), writes=[negm[1]])
        if SSD_PRE_STOP == 2:
            return
        def cums(tri0, tri1, post):
            for d_ in range(2):
                for hf_ in range(2):
                    ps = k.psum[d_ * 2 + hf_]
                    ca = hf_ * 17
                    for si in range(3):
                        P.op("pe", lambda e, ps=ps, d_=d_, ca=ca, si=si: e.matmul(ps[:, :272], lhsT=C[(tri0, tri1)[d_]][:], rhs=dsp[si][:, d_, ca:ca + 17, :].rearrange("p c h -> p (c h)"), start=(si == 0), stop=(si == 2)),
                             reads=[dsp[si]], writes=[ps], acc=True)
                    post(ps, d_, ca)

        def post_inc(ps, d_, ca):
            v = lambda t: t[:, d_, ca:ca + 17, :].rearrange("p c h -> p (c h)")
            P.op("dve", lambda e: e.tensor_copy(v(ainc), ps[:, :272]), reads=[ps], writes=[(ainc, d_, ca)])
            P.op("act", lambda e: e.activation(out=v(eain), in_=ps[:, :272], func=AF.Exp), reads=[ps], writes=[(eain, d_, ca)])
            P.op("dve", lambda e: e.tensor_scalar(out=v(nain), in0=v(ainc), scalar1=-1.0, scalar2=None, op0=ALU.mult), reads=[(ainc, d_, ca)], writes=[(nain, d_, ca)])

        def post_exc(ps, d_, ca):
            v = lambda t: t[:, d_, ca:ca + 17, :].rearrange("p c h -> p (c h)")
            P.op("act", lambda e: e.activation(out=v(wsrc), in_=ps[:, :272], func=AF.Exp), reads=[ps], writes=[(wsrc, d_, ca)])
            P.op("dve", lambda e: e.tensor_tensor(out=v(wsrc), in0=v(wsrc), in1=v(dtt), op=ALU.mult), reads=[(wsrc, d_, ca)], writes=[(wsrc, d_, ca)])

        def post_tot(ps, d_, ca):
            v = lambda t: t[:, d_, ca:ca + 17, :].rearrange("p c h -> p (c h)")
            P.op("act", lambda e: e.activation(out=v(etot), in_=ps[:, :272], func=AF.Exp), reads=[ps], writes=[(etot, d_, ca)])

        cums("tri_le_b", "tri_ge_b", post_inc)
        P.barrier()
        if SSD_PRE_STOP == 3:
            return
        cums("tri_gt_b", "tri_lt_b", post_exc)
        P.barrier()
        if SSD_PRE_STOP == 4:
            return
        cums("ones", "ones", post_tot)
        P.barrier()
        if SSD_PRE_STOP == 5:
            return

        xt = [P.sb("S_x%d" % i, [128, 1024], BF16, st) for i in range(2)]
        Bt = [P.sb("S_Bt%d" % i, [128, 512], BF16, st) for i in range(2)]
        Bf = [P.sb("S_Bf%d" % i, [128, 4, 128], BF16, st) for i in range(2)]
        Cf = [P.sb("S_Cf%d" % i, [128, 4, 128], BF16, st) for i in range(2)]
        X = [P.sb("S_X%d" % i, [128, 2, 16, 128], BF16, st) for i in range(2)]
        cbm = [P.sb("S_cbm%d" % i, [128, 4, 128], F32, st) for i in range(2)]
        Eh = [P.sb("S_E%d" % i, [128, 8, 128], F32, st) for i in range(2)]
        MT = [P.sb("S_MT%d" % i, [128, 8, 128], BF16, st) for i in range(2)]
        xw = [P.sb("S_xw%d" % i, [128, 1024], BF16, st) for i in range(2)]
        Hf = P.sb("S_Hf", [128, 1024], F32, st)
        Hb = P.sb("S_Hb", [128, 1024], BF16, st)
        ht = P.sb("S_ht", [128, 1024], F32, st)
        ych = [P.sb("S_y%d" % i, [128, 1024], F32, st) for i in range(2)]
        ypv = [P.sb("S_yp%d" % i, [128, 1024], F32, st) for i in range(2)]
        szt = [P.sb("S_sz%d" % i, [128, 1024], BF16, st) for i in range(2)]
        y2 = P.sb("S_y2", [128, 1024], F32, st)
        sq = P.sb("S_sq", [128, 1024], F32, st)
        ss = P.sb("S_ss", [128, 4], F32, st)
        stm = P.sb("S_stm", [128, 1024], BF16, st)
        sfm = [P.sb("S_sfm%d" % i, [128, 8, 128], BF16, st) for i in range(2)]
        ps_cb = k.psum[0]
        ps_A = Rot([(k.psum[1], k.psum[2]), (k.psum[3], k.psum[4])])
        ps_y, ps_i, ps_h = k.psum[5], k.psum[6], k.psum[0]
        pst = k.psbf[0]
        sBf3 = scr["sBf"].rearrange("(g p) t -> p g t", p=128)
        sCf3 = scr["sCf"].rearrange("(g p) t -> p g t", p=128)
        sout3 = scr["sout"].rearrange("(c p) t -> p c t", p=128)
        for d in range(2):
            order = scan_order(d == 1)
            mask = C["tri_le"] if d == 0 else C["tri_ge"]
            maskb = C["tri_le_b"] if d == 0 else C["tri_ge_b"]
            if d == 1:
                P.barrier()
            P.op("dve", lambda e: e.memset(Hf[:], 0.0), writes=[Hf])
            P.op("pool", lambda e: e.memset(Hb[:], 0.0), writes=[Hb])
            for oi, c in enumerate(order):
                if SSD_DEBUG_LIMIT is not None and (d * NCH + oi) >= SSD_DEBUG_LIMIT:
                    break
                i2 = oi % 2
                x_, bt_, bf_, cf_, X_, cb_, xw_, y_ = xt[i2], Bt[i2], Bf[i2], Cf[i2], X[i2], cbm[i2], xw[i2], ych[i2]
                dma(P, "sp", x_[:], scr["sx"][c * 128:(c + 1) * 128, :], writes=[x_])
                dma(P, "sp", bt_[:], scr["sBt"][c * 128:(c + 1) * 128, :], writes=[bt_])
                dma(P, "sp", bf_[:], sBf3[:, :, c * 128:(c + 1) * 128], writes=[bf_])
                dma(P, "sp", cf_[:], sCf3[:, :, c * 128:(c + 1) * 128], writes=[cf_])
                if d == 1:
                    yp_, sz_ = ypv[i2], szt[i2]
                    dma(P, "sp", yp_[:], scr["yacc"][c * 128:(c + 1) * 128, :], writes=[yp_])
                    dma(P, "sp", sz_[:], scr["sz"][c * 128:(c + 1) * 128, :], writes=[sz_])
                go = d * 16
                for g in range(4):
                    P.op("pe", lambda e, g=g, bf_=bf_, cf_=cf_: e.matmul(ps_cb[:, g * 128:(g + 1) * 128], lhsT=bf_[:, g, :], rhs=cf_[:, g, :], start=True, stop=True),
                         reads=[bf_, cf_], writes=[ps_cb], acc=True)
                P.op("dve", lambda e, cb_=cb_, mask=mask: e.tensor_tensor(out=cb_[:], in0=ps_cb[:].rearrange("p (g t) -> p g t", g=4), in1=mask[:].unsqueeze(1).to_broadcast([128, 4, 128]), op=ALU.mult),
                     reads=[ps_cb], writes=[cb_])
                for si in range(2):
                    P.op("dve", lambda e, X_=X_, c=c, d=d, maskb=maskb, si=si: e.tensor_tensor(out=X_[:, si], in0=maskb[:].unsqueeze(1).to_broadcast([128, 16, 128]),
                                                                                        in1=dsp[si][:, d, c, :].unsqueeze(2).to_broadcast([128, 16, 128]), op=ALU.mult), writes=[(X_, si)])
                for hf in range(2):
                    pA = ps_A.next()
                    E_, M_ = Eh[hf], MT[hf]
                    for j in range(2):
                        gg = hf * 2 + j
                        for si in range(2):
                            P.op("pe", lambda e, pA=pA, j=j, gg=gg, X_=X_, si=si: e.matmul(pA[j][:, :], lhsT=C["ones"][:], rhs=X_[:, si, gg * 4:(gg + 1) * 4, :].rearrange("p h t -> p (h t)"), start=(si == 0), stop=False),
                                 reads=[(X_, si)], writes=[pA[j]], acc=True)
                        P.op("pe", lambda e, pA=pA, j=j, d=d: e.matmul(pA[j][:, :], lhsT=C["ident"][:], rhs=negm[d][:].rearrange("p h t -> p (h t)"), start=False, stop=True),
                             reads=[negm[d]], writes=[pA[j]], acc=True)
                    for hh in range(8):
                        h = hf * 8 + hh
                        P.op("act", lambda e, pA=pA, hh=hh, h=h, E_=E_, c=c, d=d: e.activation(out=E_[:, hh, :], in_=pA[hh // 4][:, (hh % 4) * 128:(hh % 4 + 1) * 128], func=AF.Exp,
                                                                                         bias=nain[:, d, c, h:h + 1], scale=1.0), reads=[pA[hh // 4]], writes=[(E_, hh)])
                        P.op("dve", lambda e, hh=hh, h=h, E_=E_, M_=M_, cb_=cb_, c=c, d=d: e.scalar_tensor_tensor(out=M_[:, hh, :], in0=E_[:, hh, :], scalar=dtt[:, d, c, h:h + 1],
                                                                                                        in1=cb_[:, h // 4, :], op0=ALU.mult, op1=ALU.mult),
                             reads=[(E_, hh), cb_], writes=[(M_, hh)])
                    for hh in range(8):
                        h = hf * 8 + hh
                        P.op("pe", lambda e, hh=hh, h=h, M_=M_, x_=x_: e.matmul(ps_y[:, hh * 64:(hh + 1) * 64], lhsT=M_[:, hh, :], rhs=x_[:, h * 64:(h + 1) * 64], start=True, stop=True),
                             reads=[(M_, hh), x_], writes=[ps_y], acc=True)
                    for j in range(2):
                        gg = hf * 2 + j
                        P.op("pe", lambda e, j=j, gg=gg, cf_=cf_: e.matmul(ps_i[:, j * 256:(j + 1) * 256], lhsT=cf_[:, gg, :], rhs=Hb[:, gg * 256:(gg + 1) * 256], start=True, stop=True),
                             reads=[cf_, Hb], writes=[ps_i], acc=True)
                    ysl = y_[:, hf * 512:(hf + 1) * 512]
                    P.op("dve", lambda e, ysl=ysl, c=c, d=d, hf=hf: e.tensor_tensor(out=ysl.rearrange("p (h q) -> p h q", h=8), in0=ps_i[:].rearrange("p (h q) -> p h q", h=8),
                                                                                 in1=eain[:, d, c, hf * 8:hf * 8 + 8].unsqueeze(2).to_broadcast([128, 8, 64]), op=ALU.mult),
                         reads=[ps_i], writes=[(y_, hf)])
                    P.op("dve", lambda e, ysl=ysl: e.tensor_tensor(out=ysl, in0=ps_y[:], in1=ysl, op=ALU.add), reads=[ps_y, (y_, hf)], writes=[(y_, hf)])
                    if d == 1:
                        P.op("pool", lambda e, ysl=ysl, yp_=yp_, hf=hf: e.tensor_tensor(out=ysl, in0=ysl, in1=yp_[:, hf * 512:(hf + 1) * 512], op=ALU.add),
                             reads=[(y_, hf), yp_], writes=[(y_, hf)])
                if oi + 1 < len(order):
                    P.op("dve", lambda e, xw_=xw_, x_=x_, c=c, d=d: e.tensor_tensor(out=xw_[:].rearrange("p (h q) -> p h q", h=16), in0=x_[:].rearrange("p (h q) -> p h q", h=16),
                                                                                  in1=wsrc[:, d, c, :].unsqueeze(2).to_broadcast([128, 16, 64]), op=ALU.mult),
                         reads=[x_], writes=[xw_])
                    P.op("dve", lambda e, c=c, d=d: e.tensor_tensor(out=ht[:].rearrange("p (h q) -> p h q", h=16), in0=Hf[:].rearrange("p (h q) -> p h q", h=16),
                                                                      in1=etot[:, d, c, :].unsqueeze(2).to_broadcast([128, 16, 64]), op=ALU.mult),
                         reads=[Hf], writes=[ht])
                    for hf in range(2):
                        for j in range(2):
                            gg = hf * 2 + j
                            P.op("pe", lambda e, j=j, gg=gg, bt_=bt_, xw_=xw_: e.matmul(ps_h[:, j * 256:(j + 1) * 256], lhsT=bt_[:, gg * 128:(gg + 1) * 128], rhs=xw_[:, gg * 256:(gg + 1) * 256], start=True, stop=True),
                                 reads=[bt_, xw_], writes=[ps_h], acc=True)
                        P.op("dve", lambda e, hf=hf: e.tensor_tensor(out=Hf[:, hf * 512:(hf + 1) * 512], in0=ps_h[:], in1=ht[:, hf * 512:(hf + 1) * 512], op=ALU.add),
                             reads=[ps_h, ht], writes=[Hf])
                    P.op("act", lambda e: e.copy(Hb[:], Hf[:]), reads=[Hf], writes=[Hb])
                ykeys = [(y_, 0), (y_, 1)]
                if d == 0:
                    dma(P, "sp", scr["yacc"][c * 128:(c + 1) * 128, :], y_[:], reads=ykeys, writes=["yacc_o"])
                else:
                    P.op("dve", lambda e, x_=x_: e.tensor_tensor(out=y2[:].rearrange("p (h q) -> p h q", h=16), in0=x_[:].rearrange("p (h q) -> p h q", h=16),
                                                                  in1=dbc[:].unsqueeze(2).to_broadcast([128, 16, 64]), op=ALU.mult), reads=[x_], writes=[y2])
                    P.op("dve", lambda e, y_=y_: e.tensor_tensor(out=y2[:], in0=y2[:], in1=y_[:], op=ALU.add), reads=[y2] + ykeys, writes=[y2])
                    P.op("dve", lambda e, sz_=sz_: e.tensor_tensor(out=y2[:], in0=y2[:], in1=sz_[:], op=ALU.mult), reads=[y2, sz_], writes=[y2])
                    P.op("act", lambda e: e.activation(out=sq[:], in_=y2[:], func=AF.Square), reads=[y2], writes=[sq])
                    P.op("dve", lambda e: e.reduce_sum(out=ss[:], in_=sq[:].rearrange("p (g v) -> p g v", g=4), axis=AX.X), reads=[sq], writes=[ss])
                    P.op("act", lambda e: e.activation(out=ss[:], in_=ss[:], func=AF.Sqrt, bias=C["eps"][:, 0:1], scale=1.0 / 256), reads=[ss], writes=[ss])
                    P.op("dve", lambda e: e.reciprocal(ss[:], ss[:]), reads=[ss], writes=[ss])
                    P.op("dve", lambda e: e.tensor_tensor(out=sq[:].rearrange("p (g v) -> p g v", g=4), in0=y2[:].rearrange("p (g v) -> p g v", g=4),
                                                          in1=ss[:].unsqueeze(2).to_broadcast([128, 4, 256]), op=ALU.mult), reads=[y2, ss], writes=[sq])
                    P.op("pool", lambda e: e.tensor_tensor(out=stm[:], in0=sq[:], in1=sngb[:], op=ALU.mult), reads=[sq], writes=[stm])
                    sf = sfm[i2]
                    for j in range(8):
                        P.op("pe", lambda e, j=j: e.transpose(pst[:, j * 128:(j + 1) * 128], stm[:, j * 128:(j + 1) * 128], C["ident"][:]), reads=[stm], writes=[pst])
                    P.op("act", lambda e, sf=sf: e.copy(sf[:], pst[:].rearrange("p (a b) -> p a b", a=8)), reads=[pst], writes=[sf])
                    dma(P, "sp", sout3[:, :, c * 128:(c + 1) * 128], sf[:], reads=[sf], writes=["sout_o"])


def phaseOUT(k, l):
    P, C, din, scr = k.P, k.C, k.din, k.scr
    with ExitStack() as st:
        k.alloc_psum(st, 8)
        wa = P.sb("O_wa", [64, 8, 1024], BF16, st)
        wm = P.sb("O_wm", [128, 4, 1024], BF16, st)
        ws = P.sb("O_ws", [128, 8, 1024], BF16, st)
        wo = P.sb("O_wo", [128, 8, 1024], BF16, st)
        wrf = P.sb("O_wrf", [128, 8, 32], F32, st)
        wrh = P.sb("O_wrh", [128, 8, 32], BF16, st)
        wrl = P.sb("O_wrl", [128, 8, 32], BF16, st)
        brt = P.sb("O_brt", [128, 32], F32, st)
        dma(P, "pool", wa[:], din["w_bra"][l].rearrange("(h p) n -> p h n", p=64), writes=[wa])
        dma(P, "pool", wm[:], din["w_brm"][l].rearrange("(c p) n -> p c n", p=128), writes=[wm])
        dma(P, "pool", ws[:], din["w_brs"][l].rearrange("(c p) n -> p c n", p=128), writes=[ws])
        dma(P, "pool", wo[:], din["w_out"][l].rearrange("(c p) n -> p c n", p=128), writes=[wo])
        dma(P, "sp", wrf[:], din["w_rt"][l].rearrange("(c p) n -> p c n", p=128), writes=[wrf])
        dma(P, "sp", brt[:], din["b_rt"][l, 0:1, :].partition_broadcast(128), writes=[brt])
        P.op("dve", lambda e: e.tensor_copy(wrh[:], wrf[:]), reads=[wrf], writes=[wrh])
        P.op("dve", lambda e: e.tensor_tensor(out=wrl[:], in0=wrf[:], in1=wrh[:], op=ALU.subtract), reads=[wrf, wrh], writes=[wrl])
        att = P.sb("O_att", [64, 8, 512], BF16, st)
        mo_ = P.sb("O_mo", [128, 4, 512], BF16, st)
        so_ = P.sb("O_so", [128, 8, 512], BF16, st)
        gg_ = P.sb("O_gg", [128, 24, 512], BF16, st)
        xr = P.sb("O_xr", [128, 8, 512], F32, st)
        mg = P.sb("O_mg", [128, 8, 512], BF16, st)
        tt = [[P.sb("O_t%d%d" % (i, j), [128, 512], F32, st) for j in range(3)] for i in range(2)]
        sq = P.sb("O_sq", [128, 8, 512], BF16, st)
        rs = P.sb("O_rs", [128, 512], F32, st)
        hfc = [P.sb("O_hf%d" % i, [128, 512], F32, st) for i in range(2)]
        h2b = P.sb("O_h2b", [128, 8, 512], BF16, st)
        hlo = P.sb("O_hlo", [128, 8, 512], BF16, st)
        lg = P.sb("O_lg", [128, 4, 32], F32, st)
        mk = P.sb("O_mk", [128, 4, 32], F32, st)
        ex = P.sb("O_ex", [128, 4, 32], F32, st)
        mx8 = P.sb("O_mx8", [128, 4, 8], F32, st)
        nmx = P.sb("O_nmx", [128, 4], F32, st)
        sm = P.sb("O_sm", [128, 4], F32, st)
        gfs = P.sb("O_gfs", [32, 512], F32, st)
        att3 = scr["att"].rearrange("(h p) t -> p h t", p=64)
        mo3 = scr["mout"].rearrange("(c p) t -> p c t", p=128)
        so3 = scr["sout"].rearrange("(c p) t -> p c t", p=128)
        gg3 = scr["gg"].rearrange("(c p) t -> p c t", p=128)
        xr3 = scr["xres"].rearrange("(c p) t -> p c t", p=128)
        h23 = scr["hn2"].rearrange("(c p) t -> p c t", p=128)
        modv, a2 = k.modv, k.a2
        for ti, (t0, n) in enumerate(TILES):
            mc = 1 if t0 < CTX else 0
            nj = n // 128
            dma(P, "sp", att[:, :, :n], att3[:, :, t0:t0 + n], writes=[att])
            dma(P, "sp", mo_[:, :, :n], mo3[:, :, t0:t0 + n], writes=[mo_])
            dma(P, "sp", so_[:, :, :n], so3[:, :, t0:t0 + n], writes=[so_])
            for g3 in range(3):
                dma(P, "sp", gg_[:, g3 * 8:(g3 + 1) * 8, :n], gg3[:, g3 * 8:(g3 + 1) * 8, t0:t0 + n], writes=[(gg_, g3)])
            dma(P, "sp", xr[:, :, :n], xr3[:, :, t0:t0 + n], writes=[xr] + [(xr, "n", oc) for oc in range(8)])
            for oc in range(8):
                pp = k.psum[(oc % 2) * 3:(oc % 2) * 3 + 3]
                tq = tt[oc % 2]
                for h in range(8):
                    P.op("pe", lambda e, p0=pp[0], h=h, oc=oc, n=n: e.matmul(p0[:, :n], lhsT=wa[:, h, oc * 128:(oc + 1) * 128], rhs=att[:, h, :n], start=(h == 0), stop=(h == 7)),
                         reads=[wa, att], writes=[pp[0]], acc=True)
                for c in range(4):
                    P.op("pe", lambda e, p1=pp[1], c=c, oc=oc, n=n: e.matmul(p1[:, :n], lhsT=wm[:, c, oc * 128:(oc + 1) * 128], rhs=mo_[:, c, :n], start=(c == 0), stop=(c == 3)),
                         reads=[wm, mo_], writes=[pp[1]], acc=True)
                for c in range(8):
                    P.op("pe", lambda e, p2=pp[2], c=c, oc=oc, n=n: e.matmul(p2[:, :n], lhsT=ws[:, c, oc * 128:(oc + 1) * 128], rhs=so_[:, c, :n], start=(c == 0), stop=(c == 7)),
                         reads=[ws, so_], writes=[pp[2]], acc=True)
                for b in range(3):
                    P.op("dve", lambda e, b=b, pb=pp[b], tq=tq, oc=oc, n=n: e.tensor_tensor(out=tq[b][:, :n], in0=pb[:, :n], in1=gg_[:, b * 8 + oc, :n], op=ALU.mult),
                         reads=[pp[b], (gg_, b)], writes=[tq[b]])
                P.op("pool", lambda e, tq=tq, n=n: e.tensor_tensor(out=tq[0][:, :n], in0=tq[0][:, :n], in1=tq[1][:, :n], op=ALU.add), reads=[tq[0], tq[1]], writes=[tq[0]])
                P.op("pool", lambda e, tq=tq, n=n, oc=oc: e.tensor_tensor(out=mg[:, oc, :n], in0=tq[0][:, :n], in1=tq[2][:, :n], op=ALU.add), reads=[tq[0], tq[2]], writes=[(mg, oc)])
            for oc in range(8):
                ps = k.psum[6 + oc % 2]
                for c in range(8):
                    P.op("pe", lambda e, ps=ps, c=c, oc=oc, n=n: e.matmul(ps[:, :n], lhsT=wo[:, c, oc * 128:(oc + 1) * 128], rhs=mg[:, c, :n], start=(c == 0), stop=(c == 7)),
                         reads=[wo] + [(mg, c2) for c2 in range(8)], writes=[ps], acc=True)
                P.op("dve", lambda e, ps=ps, oc=oc, n=n, mc=mc: e.scalar_tensor_tensor(out=xr[:, oc, :n], in0=ps[:, :n], scalar=modv[:, 16 + oc, mc:mc + 1], in1=xr[:, oc, :n], op0=ALU.mult, op1=ALU.add),
                     reads=[ps, xr], writes=[(xr, "n", oc)])
            xkeys = [(xr, "n", oc) for oc in range(8)]
            dma(P, "sp", xr3[:, :, t0:t0 + n], xr[:, :, :n], reads=xkeys, writes=["xres_o"])
            P.op("act", lambda e, n=n: e.activation(out=sq[:, :, :n], in_=xr[:, :, :n], func=AF.Square), reads=xkeys, writes=[sq])
            ps = k.psum[6]
            for c in range(8):
                P.op("pe", lambda e, ps=ps, c=c, n=n: e.matmul(ps[:, :n], lhsT=C["ones"][:], rhs=sq[:, c, :n], start=(c == 0), stop=(c == 7)), reads=[sq], writes=[ps], acc=True)
            P.op("act", lambda e, ps=ps, n=n: e.activation(out=rs[:, :n], in_=ps[:, :n], func=AF.Sqrt, bias=C["eps"][:, 0:1], scale=1.0 / D), reads=[ps], writes=[rs])
            P.op("dve", lambda e, n=n: e.reciprocal(rs[:, :n], rs[:, :n]), reads=[rs], writes=[rs])
            for c in range(8):
                hf = hfc[c % 2]
                P.op("dve", lambda e, hf=hf, c=c, n=n: e.tensor_tensor(out=hf[:, :n], in0=xr[:, c, :n], in1=rs[:, :n], op=ALU.mult), reads=xkeys + [rs, "xres_o"], writes=[hf])
                P.op("act", lambda e, hf=hf, c=c, n=n, mc=mc: e.activation(out=hf[:, :n], in_=hf[:, :n], func=AF.Identity, bias=modv[:, 24 + c, mc:mc + 1], scale=a2[:, c, mc:mc + 1]),
                     reads=[hf], writes=[hf])
                P.op("dve", lambda e, hf=hf, c=c, n=n: e.tensor_copy(h2b[:, c, :n], hf[:, :n]), reads=[hf], writes=[(h2b, c)])
                P.op("dve", lambda e, hf=hf, c=c, n=n: e.tensor_tensor(out=hlo[:, c, :n], in0=hf[:, :n], in1=h2b[:, c, :n], op=ALU.subtract), reads=[hf, (h2b, c)], writes=[(hlo, c)])
            hkeys = [(h2b, c) for c in range(8)]
            dma(P, "sp", h23[:, :, t0:t0 + n], h2b[:, :, :n], reads=hkeys, writes=["hn2_o"])
            pr = k.psum[7]
            for j in range(nj):
                cnt = 0
                for (ha, wb_) in ((h2b, wrh), (h2b, wrl), (hlo, wrh)):
                    for c in range(8):
                        P.op("pe", lambda e, j=j, c=c, ha=ha, wb_=wb_, cnt=cnt: e.matmul(pr[:, j * 32:(j + 1) * 32], lhsT=ha[:, c, j * 128:(j + 1) * 128], rhs=wb_[:, c, :], start=(cnt == 0), stop=(cnt == 23)),
                             reads=hkeys + [(hlo, c2) for c2 in range(8)] + [wrh, wrl], writes=[pr], acc=True)
                        cnt += 1
            P.op("dve", lambda e, nj=nj: e.tensor_tensor(out=lg[:, :nj, :], in0=pr[:, :nj * 32].rearrange("p (j x) -> p j x", x=32), in1=brt[:].unsqueeze(1).to_broadcast([128, nj, 32]), op=ALU.add),
                 reads=[pr, brt], writes=[lg])
            for j in range(nj):
                P.op("dve", lambda e, j=j: e.max(out=mx8[:, j, :], in_=lg[:, j, :]), reads=[lg], writes=[(mx8, j)])
            mkeys = [(mx8, j) for j in range(nj)]
            P.op("dve", lambda e, nj=nj: e.tensor_scalar(out=nmx[:, :nj], in0=mx8[:, :nj, 0], scalar1=-1.0, scalar2=None, op0=ALU.mult), reads=mkeys, writes=[nmx])
            for j in range(nj):
                P.op("dve", lambda e, j=j: e.tensor_scalar(out=mk[:, j, :], in0=lg[:, j, :], scalar1=mx8[:, j, 3:4], scalar2=C["big"][:, 0:1], op0=ALU.subtract, op1=ALU.mult), reads=[lg] + mkeys, writes=[(mk, j)])
                P.op("dve", lambda e, j=j: e.tensor_scalar(out=mk[:, j, :], in0=mk[:, j, :], scalar1=1.0, scalar2=0.0, op0=ALU.add, op1=ALU.max), reads=[(mk, j)], writes=[(mk, j)])
                P.op("dve", lambda e, j=j: e.tensor_scalar(out=mk[:, j, :], in0=mk[:, j, :], scalar1=1.0, scalar2=None, op0=ALU.min), reads=[(mk, j)], writes=[(mk, j)])
                P.op("act", lambda e, j=j: e.activation(out=ex[:, j, :], in_=lg[:, j, :], func=AF.Exp, bias=nmx[:, j:j + 1], scale=1.0), reads=[lg, nmx], writes=[(ex, j)])
                P.op("dve", lambda e, j=j: e.tensor_tensor(out=ex[:, j, :], in0=ex[:, j, :], in1=mk[:, j, :], op=ALU.mult), reads=[(ex, j), (mk, j)], writes=[(ex, j)])
            ekeys = [(ex, j) for j in range(nj)]
            P.op("dve", lambda e, nj=nj: e.reduce_sum(out=sm[:, :nj], in_=ex[:, :nj, :], axis=AX.X), reads=ekeys, writes=[sm])
            P.op("dve", lambda e, nj=nj: e.reciprocal(sm[:, :nj], sm[:, :nj]), reads=[sm], writes=[sm])
            P.op("dve", lambda e, nj=nj: e.tensor_tensor(out=ex[:, :nj, :], in0=ex[:, :nj, :], in1=sm[:, :nj].unsqueeze(2).to_broadcast([128, nj, 32]), op=ALU.mult), reads=ekeys + [sm], writes=ekeys)
            pg = k.psum[6]
            for j in range(nj):
                P.op("pe", lambda e, j=j: e.transpose(pg[0:32, j * 128:(j + 1) * 128], ex[:, j, :], C["ident_f"][:]), reads=ekeys, writes=[pg], acc=True)
            P.op("act", lambda e, n=n: e.copy(gfs[:, :n], pg[0:32, :n]), reads=[pg], writes=[gfs])
            dma(P, "sp", scr["gfm"][:, t0:t0 + n], gfs[:, :n], reads=[gfs], writes=["gfm_o"])


MOE_BLOCKS = [
    [(0, 256), (256, 384), (640, 512)],
    [(1152, 384), (1536, 384), (1920, 384)],
    [(2304, 512), (2816, 512)],
    [(3328, 512), (3840, 512)],
]
MOE_EXPERTS = NE


def phaseMOE(k, l):
    P, C, din, scr = k.P, k.C, k.din, k.scr
    with ExitStack() as st:
        k.alloc_psum(st, 8)
        NB = 1152
        h2 = P.sb("E_h2", [128, 8, NB], BF16, st)
        Gt = P.sb("E_G", [32, NB], F32, st)
        yacc = P.sb("E_y", [128, 8, NB], F32, st)
        wup = [P.sb("E_wu%d" % i, [128, 8, 2048], BF16, st) for i in range(2)]
        wdn = [P.sb("E_wd%d" % i, [128, 8, 1024], BF16, st) for i in range(2)]
        bup = [P.sb("E_bu%d" % i, [128, 16], F32, st) for i in range(2)]
        bdn = P.sb("E_bdn", [32, 1024], F32, st)
        gb = [P.sb("E_gb%d" % i, [128, 512], F32, st) for i in range(2)]
        glu = [P.sb("E_gl%d" % i, [128, 512], F32, st) for i in range(2)]
        sig = [P.sb("E_sg%d" % i, [128, 512], F32, st) for i in range(2)]
        lin = [P.sb("E_ln%d" % i, [128, 512], F32, st) for i in range(2)]
        av = P.sb("E_a", [128, 8, 512], BF16, st)
        xc = [P.sb("E_xc%d" % i, [128, 512], F32, st) for i in range(2)]
        h23 = scr["hn2"].rearrange("(c p) t -> p c t", p=128)
        xr3 = scr["xres"].rearrange("(c p) t -> p c t", p=128)
        dma(P, "sp", bdn[:], din["b_dn"][l], writes=[bdn])
        ps_gl = [k.psum[0], k.psum[1]]
        ps_ln = [k.psum[2], k.psum[3]]
        ps_y = [k.psum[4], k.psum[5]]
        ps_b = k.psum[6]
        wi = 0
        for blk in MOE_BLOCKS:
            b0 = blk[0][0]
            nb = sum(n for _, n in blk)
            dma(P, "sp", h2[:, :, :nb], h23[:, :, b0:b0 + nb], writes=[h2])
            dma(P, "sp", Gt[:, :nb], scr["gfm"][:, b0:b0 + nb], writes=[Gt])
            for (t0, n) in blk:
                o = t0 - b0
                for oc in range(8):
                    P.op("pe", lambda e, oc=oc, o=o, n=n: e.matmul(ps_b[:, :n], lhsT=bdn[:, oc * 128:(oc + 1) * 128], rhs=Gt[:, o:o + n], start=True, stop=True),
                         reads=[bdn, Gt], writes=[ps_b])
                    P.op("act", lambda e, oc=oc, o=o, n=n: e.copy(yacc[:, oc, o:o + n], ps_b[:, :n]), reads=[ps_b], writes=[(yacc, oc, t0)])
            for ex in range(MOE_EXPERTS):
                wu, wd, bu = wup[wi % 2], wdn[wi % 2], bup[wi % 2]
                wi += 1
                wu3 = din["w_up"][l, ex].rearrange("(kc p) n -> p kc n", p=128)
                wd3 = din["w_dn"][l, ex].rearrange("(kc p) n -> p kc n", p=128)
                for q4 in range(4):
                    dma(P, "pool", wu[:, q4 * 2:q4 * 2 + 2, :], wu3[:, q4 * 2:q4 * 2 + 2, :], writes=[wu] if q4 == 0 else [(wu, q4)])
                for q2 in range(2):
                    dma(P, "pool", wd[:, q2 * 4:q2 * 4 + 4, :], wd3[:, q2 * 4:q2 * 4 + 4, :], writes=[wd] if q2 == 0 else [(wd, q2)])
                dma(P, "sp", bu[:], din["b_up_fm"][l, ex], writes=[bu])
                wukeys = [wu] + [(wu, q) for q in range(1, 4)]
                wdkeys = [wd, (wd, 1)]
                for ti, (t0, n) in enumerate(blk):
                    o = t0 - b0
                    g_ = gb[ti % 2]
                    dma(P, "sp", g_[:, :n], scr["gfm"][ex:ex + 1, t0:t0 + n].partition_broadcast(128), writes=[g_])
                    for fc in range(8):
                        i2 = fc % 2
                        pg, pl = ps_gl[i2], ps_ln[i2]
                        for kc in range(8):
                            P.op("pe", lambda e, pg=pg, kc=kc, fc=fc, o=o, n=n, wu=wu: e.matmul(pg[:, :n], lhsT=wu[:, kc, fc * 128:(fc + 1) * 128], rhs=h2[:, kc, o:o + n], start=(kc == 0), stop=(kc == 7)),
                                 reads=wukeys + [h2], writes=[pg], acc=True)
                        for kc in range(8):
                            P.op("pe", lambda e, pl=pl, kc=kc, fc=fc, o=o, n=n, wu=wu: e.matmul(pl[:, :n], lhsT=wu[:, kc, 1024 + fc * 128:1024 + (fc + 1) * 128], rhs=h2[:, kc, o:o + n], start=(kc == 0), stop=(kc == 7)),
                                 reads=wukeys + [h2], writes=[pl], acc=True)
                        gl, sg, ln = glu[i2], sig[i2], lin[i2]
                        P.op("dve", lambda e, gl=gl, pg=pg, fc=fc, n=n, bu=bu: e.tensor_scalar(out=gl[:, :n], in0=pg[:, :n], scalar1=bu[:, fc:fc + 1], scalar2=C["seven"][:, 0:1], op0=ALU.add, op1=ALU.min),
                             reads=[pg, bu], writes=[gl])
                        P.op("act", lambda e, gl=gl, sg=sg, n=n: e.activation(out=sg[:, :n], in_=gl[:, :n], func=AF.Sigmoid, scale=1.702), reads=[gl], writes=[sg])
                        P.op("dve", lambda e, ln=ln, pl=pl, fc=fc, n=n, bu=bu: e.tensor_scalar(out=ln[:, :n], in0=pl[:, :n], scalar1=bu[:, 8 + fc:9 + fc], scalar2=C["seven"][:, 0:1], op0=ALU.add, op1=ALU.min),
                             reads=[pl, bu], writes=[ln])
                        P.op("pool", lambda e, ln=ln, n=n: e.tensor_scalar(out=ln[:, :n], in0=ln[:, :n], scalar1=-7.0, scalar2=1.0, op0=ALU.max, op1=ALU.add), reads=[ln], writes=[ln])
                        P.op("pool", lambda e, gl=gl, sg=sg, n=n: e.tensor_tensor(out=sg[:, :n], in0=gl[:, :n], in1=sg[:, :n], op=ALU.mult), reads=[gl, sg], writes=[sg])
                        P.op("pool", lambda e, ln=ln, sg=sg, n=n: e.tensor_tensor(out=sg[:, :n], in0=sg[:, :n], in1=ln[:, :n], op=ALU.mult), reads=[ln, sg], writes=[sg])
                        P.op("dve", lambda e, sg=sg, g_=g_, fc=fc, n=n: e.tensor_tensor(out=av[:, fc, :n], in0=sg[:, :n], in1=g_[:, :n], op=ALU.mult), reads=[sg, g_], writes=[(av, fc)])
                    akeys = [(av, fc) for fc in range(8)]
                    for oc in range(8):
                        py = ps_y[oc % 2]
                        for fc in range(8):
                            P.op("pe", lambda e, py=py, fc=fc, oc=oc, n=n, wd=wd: e.matmul(py[:, :n], lhsT=wd[:, fc, oc * 128:(oc + 1) * 128], rhs=av[:, fc, :n], start=(fc == 0), stop=(fc == 7)),
                                 reads=wdkeys + akeys, writes=[py], acc=True)
                        P.op("dve", lambda e, py=py, oc=oc, o=o, n=n: e.tensor_tensor(out=yacc[:, oc, o:o + n], in0=py[:, :n], in1=yacc[:, oc, o:o + n], op=ALU.add),
                             reads=[py, (yacc, oc, t0)], writes=[(yacc, oc, t0)])
            for (t0, n) in blk:
                o = t0 - b0
                mc = 1 if t0 < CTX else 0
                for oc in range(8):
                    x_ = xc[oc % 2]
                    dma(P, "sp", x_[:, :n], xr3[:, oc, t0:t0 + n], writes=[x_])
                    P.op("dve", lambda e, x_=x_, oc=oc, o=o, n=n, mc=mc: e.scalar_tensor_tensor(out=x_[:, :n], in0=yacc[:, oc, o:o + n], scalar=k.modv[:, 40 + oc, mc:mc + 1], in1=x_[:, :n], op0=ALU.mult, op1=ALU.add),
                         reads=[x_, (yacc, oc, t0)], writes=[x_])
                    dma(P, "sp", xr3[:, oc, t0:t0 + n], x_[:, :n], reads=[x_], writes=["xres_o"])


def phaseFIN(k):
    P, C, din, scr = k.P, k.C, k.din, k.scr
    with ExitStack() as st:
        k.alloc_psum(st, 8)
        fg = P.sb("F_g", [128, 8], F32, st)
        dma(P, "sp", fg[:], din["fing_fm"][:, :], writes=[fg])
        xt = [P.sb("F_x%d" % i, [128, 8, 512], F32, st) for i in range(2)]
        sq = [P.sb("F_sq%d" % i, [128, 8, 512], BF16, st) for i in range(2)]
        rs = [P.sb("F_rs%d" % i, [128, 512], F32, st) for i in range(2)]
        ot = [P.sb("F_o%d" % i, [128, 1024], F32, st) for i in range(2)]
        xr3 = scr["xres"].rearrange("(c p) t -> p c t", p=128)
        oi = 0
        for ti, (t0, n) in enumerate(TILES):
            if t0 < CTX:
                continue
            x, q, r = xt[ti % 2], sq[ti % 2], rs[ti % 2]
            ps = k.psum[ti % 2]
            dma(P, "sp", x[:, :, :n], xr3[:, :, t0:t0 + n], writes=[x])
            P.op("act", lambda e, x=x, q=q, n=n: e.activation(out=q[:, :, :n], in_=x[:, :, :n], func=AF.Square), reads=[x], writes=[q])
            for c in range(8):
                P.op("pe", lambda e, ps=ps, q=q, c=c, n=n: e.matmul(ps[:, :n], lhsT=C["ones"][:], rhs=q[:, c, :n], start=(c == 0), stop=(c == 7)), reads=[q], writes=[ps], acc=True)
            P.op("act", lambda e, ps=ps, r=r, n=n: e.activation(out=r[:, :n], in_=ps[:, :n], func=AF.Sqrt, bias=C["eps"][:, 0:1], scale=1.0 / D), reads=[ps], writes=[r])
            P.op("dve", lambda e, r=r, n=n: e.reciprocal(r[:, :n], r[:, :n]), reads=[r], writes=[r])
            P.op("dve", lambda e, x=x, r=r, n=n: e.tensor_tensor(out=x[:, :, :n], in0=x[:, :, :n], in1=r[:, :n].unsqueeze(1).to_broadcast([128, 8, n]), op=ALU.mult), reads=[x, r], writes=[x])
            P.op("dve", lambda e, x=x, n=n: e.tensor_tensor(out=x[:, :, :n], in0=x[:, :, :n], in1=fg[:].unsqueeze(2).to_broadcast([128, 8, n]), op=ALU.mult), reads=[x, fg], writes=[x])
            for j in range(n // 128):
                o_ = ot[oi % 2]
                oi += 1
                for half in range(2):
                    pt = k.psum[2 + (oi * 2 + half) % 4]
                    for f4 in range(4):
                        fc = half * 4 + f4
                        P.op("pe", lambda e, pt=pt, f4=f4, fc=fc, j=j, x=x: e.transpose(pt[:, f4 * 128:(f4 + 1) * 128], x[:, fc, j * 128:(j + 1) * 128], C["ident_f"][:]), reads=[x], writes=[pt], acc=True)
                    if half == 0:
                        P.op("act", lambda e, pt=pt, o_=o_: e.copy(o_[:, 0:512], pt[:]), reads=[pt], writes=[(o_, 0)])
                    else:
                        P.op("dve", lambda e, pt=pt, o_=o_: e.tensor_copy(o_[:, 512:1024], pt[:]), reads=[pt], writes=[(o_, 1)])
                r0 = t0 - CTX + j * 128
                dma(P, "sp", k.out[r0:r0 + 128, :], o_[:], reads=[(o_, 0), (o_, 1)], writes=["out_o"])


_PROG_CACHE = {}


def kernel(**inputs):
    inp = {k_: np.asarray(v) for k_, v in inputs.items()}
    sh = host_prepare(inp)
    sh = {k_: np.ascontiguousarray(v, dtype=np.float32) for k_, v in sh.items()}
    if "nc" not in _PROG_CACHE:
        _PROG_CACHE["nc"] = build_program({k_: v.shape for k_, v in sh.items()})
    nc = _PROG_CACHE["nc"]
    in_maps = []
    for b in range(8):
        m = dict(sh)
        m.update(core_inputs(inp, b))
        in_maps.append(m)
    res = run_bass_kernel_spmd(nc, in_maps, core_ids=list(range(8)))
    out = np.stack([np.asarray(r["out"], dtype=np.float32) for r in res.results], axis=0)
    return out
```

```python
import numpy as np
from contextlib import ExitStack
import concourse.bass as bass
import concourse.mybir as mybir
from concourse.bass_utils import run_bass_kernel_spmd

F32 = mybir.dt.float32
BF16 = mybir.dt.bfloat16
AF = mybir.ActivationFunctionType
ALU = mybir.AluOpType
AX = mybir.AxisListType

SEM_LIMIT = 30000
N_DMA_SEMS = 12

L = 4
D = 1024
CTX = 256
SEQ = 4096
T = CTX + SEQ
NCH = T // 128
TP = T + 8
EPS = 1e-6
NE = 32
MLA_SCALE = 96 ** -0.5
NEG = -30000.0


def colof(t):
    return t + 2 if t < CTX else t + 6


TILES = [(0, 256)] + [(CTX + 512 * i, 512) for i in range(8)]


class Tok:
    __slots__ = ("sem", "val", "eng")

    def __init__(self, sem, val, eng):
        self.sem = sem
        self.val = val
        self.eng = eng


class Prog:
    ENGS = ("pe", "dve", "act", "pool", "sp")

    def __init__(self, nc):
        self.nc = nc
        self.es = ExitStack()
        self.ops = {e: [] for e in self.ENGS}
        self.cur_sem = {}
        self.cnt = {}
        self.nsem = 0
        for e in self.ENGS:
            self._new_sem(e)
        self.last_tok = {e: None for e in self.ENGS}
        self.known = {e: {} for e in self.ENGS}
        self.last_write = {}
        self.readers = {}
        self.dma_sems = {}
        self.dma_idx = {}
        self.dma_last = {}
        self.dma_cnt = {}
        for q in ("sp", "pool", "act"):
            self.dma_sems[q] = [self._sem("d%s%d" % (q, i)) for i in range(N_DMA_SEMS)]
            self.dma_idx[q] = 0
            self.dma_last[q] = [None] * N_DMA_SEMS
            self.dma_cnt[q] = [0] * N_DMA_SEMS
        self.n_ops = 0

    def _sem(self, name):
        self.nsem += 1
        return self.es.enter_context(self.nc.semaphore("s%d_%s" % (self.nsem, name)))

    def _new_sem(self, e):
        self.cur_sem[e] = self._sem(e)
        self.cnt[e] = 0

    def sb(self, name, shape, dtype, stack=None):
        self.n_tiles = getattr(self, "n_tiles", 0) + 1
        return (stack or self.es).enter_context(self.nc.sbuf_tensor("%s_u%d" % (name, self.n_tiles), list(shape), dtype))

    def ps(self, name, shape, dtype=F32, stack=None):
        return (stack or self.es).enter_context(self.nc.psum_tensor(name, list(shape), dtype))

    def _need(self, eng, tok, waits):
        if tok is None:
            return
        k = self.known[eng]
        if k.get(tok.sem, 0) >= tok.val:
            return
        k[tok.sem] = tok.val
        waits.append((tok.sem, tok.val))

    @staticmethod
    def _k(k):
        if isinstance(k, (str, int)):
            return k
        if isinstance(k, tuple):
            return tuple(Prog._k(x) for x in k)
        return k.name

    def op(self, eng, fn, reads=(), writes=(), acc=False, dma=False):
        reads = [self._k(k) for k in reads]
        writes = [self._k(k) for k in writes]
        waits = []
        for k in reads:
            self._need(eng, self.last_write.get(k), waits)
            kn = k[0] if isinstance(k, tuple) else k
            if isinstance(kn, str) and kn.startswith("ps"):
                for r in self.readers.get(k, ()):
                    if r.eng != eng:
                        self._need(eng, r, waits)
        for k in writes:
            w = self.last_write.get(k)
            if w is not None and not (acc and w.eng == "pe" and eng == "pe"):
                self._need(eng, w, waits)
            for r in self.readers.get(k, ()):
                self._need(eng, r, waits)
        if dma:
            q = eng
            i = self.dma_idx[q]
            self.dma_idx[q] = (i + 1) % N_DMA_SEMS
            self._need(eng, self.dma_last[q][i], waits)
            self.dma_cnt[q][i] += 16
            tok = Tok(self.dma_sems[q][i], self.dma_cnt[q][i], "dma")
            self.dma_last[q][i] = tok
            inc = 16
        else:
            if self.cnt[eng] >= SEM_LIMIT:
                self._new_sem(eng)
            self.cnt[eng] += 1
            tok = Tok(self.cur_sem[eng], self.cnt[eng], eng)
            self.last_tok[eng] = tok
            inc = 1
        for k in writes:
            self.last_write[k] = tok
            self.readers[k] = []
        for k in reads:
            lst = self.readers.setdefault(k, [])
            if tok.eng != "dma":
                lst[:] = [r for r in lst if r.eng != tok.eng]
            lst.append(tok)
        self.ops[eng].append((waits, fn, tok.sem, inc))
        self.n_ops += 1
        return tok

    def barrier(self):
        toks = [self.last_tok[e] for e in self.ENGS if self.last_tok[e] is not None]
        for q in self.dma_last:
            toks += [t for t in self.dma_last[q] if t is not None]
        for e in self.ENGS:
            waits = []
            for t in toks:
                self._need(e, t, waits)
            if waits:
                self.ops[e].append((waits, None, None, 0))
        self.last_write = {}
        self.readers = {}

    def emit(self):
        self.barrier()
        nc = self.nc
        ops = self.ops
        with nc.Block() as block:
            def run(eng_obj, lst):
                for waits, fn, sem, inc in lst:
                    for s, v in waits:
                        eng_obj.wait_ge(s, v)
                    if fn is not None:
                        fn(eng_obj).then_inc(sem, inc)

            @block.tensor
            def _(e):
                run(e, ops["pe"])

            @block.vector
            def _(e):
                run(e, ops["dve"])

            @block.scalar
            def _(e):
                run(e, ops["act"])

            @block.gpsimd
            def _(e):
                run(e, ops["pool"])

            @block.sync
            def _(e):
                run(e, ops["sp"])

    def close(self):
        self.es.close()


class Rot:
    def __init__(self, items):
        self.items = items
        self.i = 0

    def next(self):
        x = self.items[self.i % len(self.items)]
        self.i += 1
        return x


IN_SIZES = (384, 256, 32, 512, 512, 512, 16, 1024, 2048, 32, 3072)
IN_OFF = np.concatenate([[0], np.cumsum(IN_SIZES)]).astype(int)


def fm_vec(v, nch):
    return np.ascontiguousarray(np.swapaxes(v.reshape(v.shape[:-1] + (nch, 128)), -1, -2))


def host_prepare(inp):
    f32 = np.float32
    w_in = inp["w_in"]
    sl = lambda i: w_in[:, :, IN_OFF[i]:IN_OFF[i + 1]]
    sh = {}
    sh["w_ada"] = inp["w_ada"]
    sh["b_ada_fm"] = fm_vec(inp["b_ada"], 48)
    sh["n1g_fm"] = fm_vec(inp["norm1_g"], 8)
    sh["n2g_fm"] = fm_vec(inp["norm2_g"], 8)
    sh["fing_fm"] = fm_vec(inp["final_g"], 8)
    sh["w_q"] = np.ascontiguousarray(sl(0))
    sh["w_kv"] = np.ascontiguousarray(sl(1))
    kr = sl(2)
    w_kr = np.zeros((L, D, 256), f32)
    w_kr[:, :, 64:96] = kr
    w_kr[:, :, 128 + 64:128 + 80] = kr[:, :, 16:32]
    w_kr[:, :, 128 + 80:128 + 96] = kr[:, :, 0:16]
    sh["w_kr"] = w_kr
    sh["w_mqk"] = np.ascontiguousarray(sl(3))
    sh["w_mv"] = np.ascontiguousarray(sl(4))
    sh["w_mo"] = np.ascontiguousarray(sl(5))
    sh["w_mif"] = np.ascontiguousarray(sl(6))
    sh["w_sz"] = np.ascontiguousarray(sl(7))
    xbc = sl(8)
    sh["w_sx"] = np.ascontiguousarray(xbc[:, :, 0:1024])
    sh["w_sB"] = np.ascontiguousarray(xbc[:, :, 1024:1536])
    sh["w_sC"] = np.ascontiguousarray(xbc[:, :, 1536:2048])
    sh["w_sdt"] = np.ascontiguousarray(sl(9))
    sh["w_g"] = np.ascontiguousarray(sl(10))
    uq = inp["mla_w_uq"].reshape(L, 384, 8, 96)
    w_uq = np.zeros((L, 384, 2, 8, 128), f32)
    w_uq[:, :, 0, :, 0:96] = uq
    w_uq[:, :, 1, :, 64:80] = uq[..., 80:96]
    w_uq[:, :, 1, :, 80:96] = uq[..., 64:80]
    sh["w_uq"] = w_uq.reshape(L, 384, 2048)
    ukv = inp["mla_w_ukv"].reshape(L, 256, 8, 128)
    sh["w_uk"] = np.ascontiguousarray(ukv[..., 0:64]).reshape(L, 256, 512)
    sh["w_uv"] = np.ascontiguousarray(ukv[..., 64:128]).reshape(L, 256, 512)
    sh["qn_fm"] = fm_vec(inp["mla_qnorm_g"], 3)
    sh["kvn_fm"] = fm_vec(inp["mla_kvnorm_g"], 2)
    sh["ml_cw"] = inp["ml_conv_w"]
    sh["ml_cb_fm"] = fm_vec(inp["ml_conv_b"], 4)
    sh["ml_gb"] = inp["ml_gate_b"].reshape(L, 1, 16)
    sh["ml_ng"] = inp["ml_norm_g"].reshape(L, 1, 512)
    scw = inp["ssd_conv_w"]
    scb = inp["ssd_conv_b"]
    sh["s_cw"] = scw
    sh["s_cb"] = scb.reshape(L, 1, 2048)
    sh["s_cbB_fm"] = fm_vec(scb[:, 1024:1536], 4)
    sh["s_cbC_fm"] = fm_vec(scb[:, 1536:2048], 4)
    sh["s_dtb"] = inp["ssd_dt_bias"].reshape(L, 1, 32)
    sh["s_alog"] = inp["ssd_a_log"].reshape(L, 1, 32)
    sh["s_d"] = inp["ssd_d"].reshape(L, 1, 16)
    sh["s_ng"] = inp["ssd_norm_g"].reshape(L, 1, 1024)
    sh["w_bra"] = inp["w_br_mla"]
    sh["w_brm"] = inp["w_br_ml"]
    sh["w_brs"] = inp["w_br_ssd"]
    sh["w_out"] = inp["w_out"]
    sh["w_rt"] = inp["w_router"]
    sh["b_rt"] = inp["b_router"].reshape(L, 1, 32)
    sh["w_up"] = inp["w_up"]
    sh["w_dn"] = inp["w_down"]
    sh["b_up_fm"] = fm_vec(inp["b_up"], 16)
    sh["b_dn"] = inp["b_down"]
    r = np.arange(128)
    sh["c_ident"] = np.eye(128, dtype=f32)
    sh["c_ones"] = np.ones((128, 128), f32)
    sh["c_tri_le"] = (r[:, None] <= r[None, :]).astype(f32)
    sh["c_tri_ge"] = (r[:, None] >= r[None, :]).astype(f32)
    sh["c_tri_gt"] = (r[:, None] > r[None, :]).astype(f32)
    sh["c_tri_lt"] = (r[:, None] < r[None, :]).astype(f32)
    sel = np.zeros((32, 32, 128), f32)
    for e in range(32):
        sel[e, e, :] = 1.0
    sh["c_sel"] = sel
    rows = SEQ // 64
    row = np.repeat(np.arange(rows), 64)
    col = np.tile(np.arange(64), rows)
    inv = (10000.0 ** (-np.arange(8, dtype=np.float32) / 8)).astype(np.float32)
    ang = np.concatenate([row[:, None] * inv, col[:, None] * inv], axis=-1).astype(np.float32)
    cs = np.cos(ang).T
    sn = np.sin(ang).T
    rc = np.ones((32, T), f32)
    rs = np.zeros((32, T), f32)
    rc[0:16, CTX:] = cs
    rc[16:32, CTX:] = cs
    rs[0:16, CTX:] = -sn
    rs[16:32, CTX:] = sn
    rope = np.zeros((128, 2, T), f32)
    rope[64:96, 0] = rc
    rope[64:96, 1] = rs
    sh["c_rope"] = rope
    return sh


def core_inputs(inp, b):
    xin = np.concatenate([inp["ctx"][b], inp["x"][b]], axis=0).astype(np.float32)
    cvec = np.stack([fm_vec(inp["c"][b], 8), fm_vec(inp["c_ctx"], 8)], axis=-1).astype(np.float32)
    return {"xin": np.ascontiguousarray(xin), "cvec": np.ascontiguousarray(cvec)}


SHARED_SPECS = None


class K:
    pass


def dma(P, q, out, in_, reads=(), writes=()):
    return P.op(q, lambda e: e.dma_start(out=out, in_=in_), reads=reads, writes=writes, dma=True)


def build_program(shared_shapes, n_layers=L, stop_after=None, debug=False, only=None, scr_inputs=()):
    nc = bass.Bass("TRN2", target_bir_lowering=False)
    P = Prog(nc)
    k = K()
    k.nc, k.P = nc, P
    k.debug = debug
    k.din = {}
    for name, shp in shared_shapes.items():
        k.din[name] = nc.dram_tensor(name, list(shp), F32, kind="ExternalInput").ap()
    k.din["xin"] = nc.dram_tensor("xin", [T, D], F32, kind="ExternalInput").ap()
    k.din["cvec"] = nc.dram_tensor("cvec", [128, 8, 2], F32, kind="ExternalInput").ap()
    k.out = nc.dram_tensor("out", [SEQ, D], F32, kind="ExternalOutput").ap()
    skind = "ExternalOutput" if debug else "Internal"
    k.scr = {}

    def scr(name, shape, dt):
        kd = "ExternalInput" if name in scr_inputs else skind
        k.scr[name] = nc.dram_tensor("scr_" + name, list(shape), dt, kind=kd).ap()

    scr("xres", [D, T], F32)
    scr("uq", [384, T], BF16)
    scr("ukv", [256, T], BF16)
    scr("kr", [256, T], BF16)
    scr("mqk", [512, T], BF16)
    scr("mv", [T, 512], BF16)
    scr("mo", [T, 512], BF16)
    scr("mif", [T, 16], F32)
    scr("sz", [T, 1024], BF16)
    scr("sx", [T, 1024], BF16)
    scr("sBf", [512, T], BF16)
    scr("sBt", [T, 512], BF16)
    scr("sCf", [512, T], BF16)
    scr("sdt", [T, 32], F32)
    scr("gg", [3072, T], BF16)
    scr("att", [512, T], BF16)
    scr("mout", [512, T], BF16)
    scr("sout", [1024, T], BF16)
    scr("hacc", [T, 512], F32)
    scr("yacc", [T, 1024], F32)
    scr("hn2", [D, T], BF16)
    scr("gfm", [32, T], F32)

    C = {}
    k.C = C

    def cload(name, src, shape, dt=F32, q="sp"):
        t = P.sb("k_" + name, shape, dt)
        dma(P, q, t[:], src, writes=[t])
        C[name] = t
        return t

    cload("ident_f", k.din["c_ident"][:, :], [128, 128])
    cload("ones_f", k.din["c_ones"][:, :], [128, 128])
    cload("ident", k.din["c_ident"][:, :], [128, 128], BF16, q="pool")
    cload("ones", k.din["c_ones"][:, :], [128, 128], BF16, q="pool")
    cload("tri_le", k.din["c_tri_le"][:, :], [128, 128])
    cload("tri_ge", k.din["c_tri_ge"][:, :], [128, 128])
    cload("tri_gt", k.din["c_tri_gt"][:, :], [128, 128])
    cload("tri_lt", k.din["c_tri_lt"][:, :], [128, 128])
    for nm in ("tri_le", "tri_ge", "tri_gt", "tri_lt"):
        cload(nm + "_b", k.din["c_" + nm][:, :], [128, 128], BF16, q="pool")
    eps = P.sb("k_eps", [128, 1], F32)
    P.op("dve", lambda e: e.memset(eps[:], EPS), writes=[eps])
    C["eps"] = eps
    one1 = P.sb("k_one1", [128, 1], F32)
    P.op("dve", lambda e: e.memset(one1[:], 1.0), writes=[one1])
    C["one1"] = one1
    neg1 = P.sb("k_neg1", [128, 1], F32)
    P.op("dve", lambda e: e.memset(neg1[:], -1.0), writes=[neg1])
    C["neg1"] = neg1
    seven = P.sb("k_seven", [128, 1], F32)
    P.op("dve", lambda e: e.memset(seven[:], 7.0), writes=[seven])
    C["seven"] = seven
    big = P.sb("k_big", [128, 1], F32)
    P.op("dve", lambda e: e.memset(big[:], 1e9), writes=[big])
    C["big"] = big
    zero1 = P.sb("k_zero1", [128, 1], F32)
    P.op("dve", lambda e: e.memset(zero1[:], 0.0), writes=[zero1])
    C["zero1"] = zero1
    k.psn = [0]

    def alloc_psum(st, nf, nb=0):
        k.psn[0] += 1
        k.psum = [P.ps("psum%d_%d" % (k.psn[0], i), [128, 512], F32, st) for i in range(nf)]
        k.psbf = [P.ps("psbf%d_%d" % (k.psn[0], i), [128, 1024], BF16, st) for i in range(nb)]
    k.alloc_psum = alloc_psum
    k.modv = P.sb("g_modv", [128, 48, 2], F32)
    k.a1 = P.sb("g_a1", [128, 8, 2], F32)
    k.a2 = P.sb("g_a2", [128, 8, 2], F32)
    cv = P.sb("g_cv", [128, 8, 2], F32)
    k.csil = P.sb("g_csil", [128, 8, 2], F32)
    dma(P, "sp", cv[:], k.din["cvec"][:, :, :], writes=[cv])
    P.op("act", lambda e: e.activation(out=k.csil[:], in_=cv[:], func=AF.Silu), reads=[cv], writes=[k.csil])
    P.barrier()

    if only is not None:
        only(k, 0)
        return finish(k)
    phase0(k)
    P.barrier()
    if stop_after == "p0":
        return finish(k)
    for l in range(n_layers):
        phaseA(k, l)
        P.barrier()
        if stop_after == ("A", l):
            return finish(k)
        phaseMLA(k, l, l == n_layers - 1)
        P.barrier()
        if stop_after == ("MLA", l):
            return finish(k)
        phaseML(k, l)
        P.barrier()
        if stop_after == ("ML", l):
            return finish(k)
        phaseSSD(k, l)
        P.barrier()
        if stop_after == ("SSD", l):
            return finish(k)
        phaseOUT(k, l)
        P.barrier()
        if stop_after == ("OUT", l):
            return finish(k)
        phaseMOE(k, l)
        P.barrier()
        if stop_after == ("MOE", l):
            return finish(k)
    phaseFIN(k)
    return finish(k)


def finish(k):
    k.P.emit()
    k.P.close()
    return k.nc


def phase0(k):
    P, C = k.P, k.C
    with ExitStack() as st:
        k.alloc_psum(st, 8)
        xt = [P.sb("p0_x%d" % i, [128, D], F32, st) for i in range(2)]
        ot = [P.sb("p0_o%d" % i, [128, 8, 128], F32, st) for i in range(2)]
        xr3 = k.scr["xres"].rearrange("(c p) t -> p c t", p=128)
        for ch in range(NCH):
            x = xt[ch % 2]
            o = ot[ch % 2]
            dma(P, "sp", x[:], k.din["xin"][ch * 128:(ch + 1) * 128, :], writes=[x])
            for half in range(2):
                ps = k.psum[(ch * 2 + half) % 4]
                for j in range(4):
                    fc = half * 4 + j
                    P.op("pe", lambda e, ps=ps, j=j, fc=fc, x=x: e.transpose(ps[:, j * 128:(j + 1) * 128], x[:, fc * 128:(fc + 1) * 128], C["ident_f"][:]),
                         reads=[x], writes=[ps])
                eng = "act" if half == 0 else "dve"
                if eng == "act":
                    P.op("act", lambda e, ps=ps, o=o, half=half: e.copy(o[:, half * 4:half * 4 + 4, :], ps[:].rearrange("p (a b) -> p a b", a=4)),
                         reads=[ps], writes=[(o, half)])
                else:
                    P.op("dve", lambda e, ps=ps, o=o, half=half: e.tensor_copy(o[:, half * 4:half * 4 + 4, :], ps[:].rearrange("p (a b) -> p a b", a=4)),
                         reads=[ps], writes=[(o, half)])
            dma(P, "sp", xr3[:, :, ch * 128:(ch + 1) * 128], o[:], reads=[(o, 0), (o, 1)], writes=["xres"])


def compute_mod(k, l, st):
    P, C, din = k.P, k.C, k.din
    wa = [P.sb("md_wa%d" % i, [128, 8, 512], F32, st) for i in range(2)]
    bada = P.sb("md_b", [128, 48], F32, st)
    n1g = P.sb("md_n1", [128, 8], F32, st)
    n2g = P.sb("md_n2", [128, 8], F32, st)
    dma(P, "sp", bada[:], din["b_ada_fm"][l], writes=[bada])
    dma(P, "sp", n1g[:], din["n1g_fm"][l], writes=[n1g])
    dma(P, "sp", n2g[:], din["n2g_fm"][l], writes=[n2g])
    psm = k.psum[7]
    w3 = din["w_ada"][l].rearrange("(kc p) n -> p kc n", p=128)
    for blk in range(12):
        w = wa[blk % 2]
        dma(P, "sp", w[:], w3[:, :, blk * 512:(blk + 1) * 512], writes=[w])
        for o4 in range(4):
            oc = blk * 4 + o4
            for kc in range(8):
                P.op("pe", lambda e, w=w, o4=o4, oc=oc, kc=kc: e.matmul(psm[:, oc * 2:oc * 2 + 2], lhsT=w[:, kc, o4 * 128:(o4 + 1) * 128],
                                                                    rhs=k.csil[:, kc, :], start=(kc == 0), stop=(kc == 7)),
                     reads=[w, k.csil], writes=[psm], acc=True)
    modv, a1, a2 = k.modv, k.a1, k.a2
    P.op("dve", lambda e: e.tensor_tensor(out=modv[:], in0=psm[:, 0:96].rearrange("p (o c) -> p o c", c=2),
                                          in1=bada[:].unsqueeze(2).to_broadcast([128, 48, 2]), op=ALU.add),
         reads=[psm, bada], writes=[modv])
    for (a, ng, base) in ((a1, n1g, 8), (a2, n2g, 32)):
        P.op("dve", lambda e, a=a, base=base: e.tensor_scalar(out=a[:], in0=modv[:, base:base + 8, :], scalar1=1.0, scalar2=None, op0=ALU.add),
             reads=[modv], writes=[a])
        P.op("dve", lambda e, a=a, ng=ng: e.tensor_tensor(out=a[:], in0=a[:], in1=ng[:].unsqueeze(2).to_broadcast([128, 8, 2]), op=ALU.mult),
             reads=[a, ng], writes=[a])


def modulate_tiles(k, st, src3, a, bbase, consume):
    P, C = k.P, k.C
    xt = [P.sb("mo_x%d" % i, [128, 8, 512], F32, st) for i in range(2)]
    sq = [P.sb("mo_sq%d" % i, [128, 8, 512], BF16, st) for i in range(2)]
    rs = [P.sb("mo_rs%d" % i, [128, 512], F32, st) for i in range(2)]
    for ti, (t0, n) in enumerate(TILES):
        mc = 1 if t0 < CTX else 0
        x, q, r = xt[ti % 2], sq[ti % 2], rs[ti % 2]
        ps = k.psum[4 + ti % 2]
        dma(P, "sp", x[:, :, :n], src3[:, :, t0:t0 + n], writes=[x])
        P.op("act", lambda e, x=x, q=q, n=n: e.activation(out=q[:, :, :n], in_=x[:, :, :n], func=AF.Square), reads=[x], writes=[q])
        for c in range(8):
            P.op("pe", lambda e, ps=ps, q=q, c=c, n=n: e.matmul(ps[:, :n], lhsT=C["ones"][:], rhs=q[:, c, :n], start=(c == 0), stop=(c == 7)),
                 reads=[q], writes=[ps], acc=True)
        P.op("act", lambda e, ps=ps, r=r, n=n: e.activation(out=r[:, :n], in_=ps[:, :n], func=AF.Sqrt, bias=C["eps"][:, 0:1], scale=1.0 / D),
             reads=[ps], writes=[r])
        P.op("dve", lambda e, r=r, n=n: e.reciprocal(r[:, :n], r[:, :n]), reads=[r], writes=[r])
        P.op("dve", lambda e, x=x, r=r, n=n: e.tensor_tensor(out=x[:, :, :n], in0=x[:, :, :n], in1=r[:, :n].unsqueeze(1).to_broadcast([128, 8, n]), op=ALU.mult),
             reads=[x, r], writes=[x])
        consume(ti, t0, n, mc, x)


def phaseA(k, l):
    P, C, din, scr = k.P, k.C, k.din, k.scr
    with ExitStack() as st:
        k.alloc_psum(st, 8)
        hn = P.sb("A_hn", [128, 8, TP], BF16, st)
        for (a, b) in ((0, 2), (258, 262), (TP - 2, TP)):
            P.op("pool", lambda e, a=a, b=b: e.memset(hn[:, :, a:b], 0.0), writes=[("hnpad", a)])
        xr3 = scr["xres"].rearrange("(c p) t -> p c t", p=128)

        def consume(ti, t0, n, mc, x):
            c0 = colof(t0)
            for c in range(8):
                P.op("act", lambda e, c=c, x=x, n=n, c0=c0, mc=mc: e.activation(out=hn[:, c, c0:c0 + n], in_=x[:, c, :n], func=AF.Identity,
                                                                           bias=k.modv[:, c, mc:mc + 1], scale=k.a1[:, c, mc:mc + 1]),
                     reads=[x, k.modv, k.a1], writes=[("hn", ti, c)])

        with ExitStack() as st2:
            compute_mod(k, l, st2)
            modulate_tiles(k, st2, xr3, k.a1, 0, consume)
            P.barrier()

        wraw = [P.sb("A_wraw%d" % i, [128, 8, 512], BF16, st) for i in range(2)]
        wexp = [P.sb("A_wexp%d" % i, [128, 5, 8, 512], BF16, st) for i in range(1)] * 2
        cwb = [P.sb("A_cwb%d" % i, [128, 5, 512], F32, st) for i in range(1)] * 2
        bbc = [P.sb("A_bbc%d" % i, [128, 512], F32, st) for i in range(2)]
        bfm = [P.sb("A_bfm%d" % i, [128, 4], F32, st) for i in range(2)]
        stg_b = Rot([P.sb("A_sb%d" % i, [128, 512], BF16, st) for i in range(4)])
        stg_f = Rot([P.sb("A_sf%d" % i, [128, 512], F32, st) for i in range(4)])
        tmpf = Rot([P.sb("A_tf%d" % i, [128, 512], F32, st) for i in range(3)])
        psr = Rot(k.psum[0:4])
        gi = [0]

        def load_w(wname, c0, Cn, conv):
            i = gi[0] % 2
            gi[0] += 1
            wr = wraw[i]
            w3 = din[wname][l].rearrange("(kc p) n -> p kc n", p=128)
            dma(P, "pool", wr[:, :, :Cn], w3[:, :, c0:c0 + Cn], writes=[wr])
            if conv is None:
                return (lambda j, kc: wr[:, kc, :Cn]), [wr], 1
            cwname, cc0 = conv
            cw, we = cwb[i], wexp[i]
            for j in range(5):
                dma(P, "sp", cw[:, j, :Cn], din[cwname][l, j:j + 1, cc0:cc0 + Cn].partition_broadcast(128), writes=[(cw, j)])
            for j in range(5):
                P.op("dve", lambda e, j=j: e.tensor_tensor(out=we[:, j, :, :Cn], in0=wr[:, :, :Cn],
                                                           in1=cw[:, j, :Cn].unsqueeze(1).to_broadcast([128, 8, Cn]), op=ALU.mult),
                     reads=[wr, (cw, j)], writes=[(we, j)])
            return (lambda j, kc: we[:, j, kc, :Cn]), [(we, j) for j in range(5)], 5

        def proj_fm(wname, wc0, Cn, oname, orow0, func, bias_name=None, conv=None, post=None, odt=BF16):
            wv, wkeys, nsh = load_w(wname, wc0, Cn, conv)
            noc = Cn // 128
            bt = None
            if bias_name is not None:
                bt = bfm[gi[0] % 2]
                dma(P, "sp", bt[:, :noc], din[bias_name][l], writes=[bt])
            for ti, (t0, n) in enumerate(TILES):
                c0 = colof(t0)
                for oc in range(noc):
                    ps = psr.next()
                    cnt = 0
                    for j in range(nsh):
                        sh = (j - 2) if nsh == 5 else 0
                        for kc in range(8):
                            P.op("pe", lambda e, ps=ps, j=j, kc=kc, oc=oc, sh=sh, c0=c0, n=n, cnt=cnt: e.matmul(
                                ps[:, :n], lhsT=wv(j, kc)[:, oc * 128:(oc + 1) * 128], rhs=hn[:, kc, c0 + sh:c0 + sh + n],
                                start=(cnt == 0), stop=(cnt == nsh * 8 - 1)), reads=wkeys, writes=[ps], acc=True)
                            cnt += 1
                    sg = stg_b.next() if odt == BF16 else stg_f.next()
                    bias_ap = bt[:, oc:oc + 1] if bt is not None else C["zero1"][:, 0:1]
                    P.op("act", lambda e, sg=sg, ps=ps, n=n, bias_ap=bias_ap: e.activation(out=sg[:, :n], in_=ps[:, :n], func=func, bias=bias_ap, scale=1.0),
                         reads=[ps] + ([bt] if bt is not None else []), writes=[sg])
                    if post is not None and post(oc) is not None:
                        sc = post(oc)
                        P.op("pool", lambda e, sg=sg, n=n, sc=sc: e.tensor_scalar(out=sg[:, :n], in0=sg[:, :n], scalar1=sc, scalar2=None, op0=ALU.mult),
                             reads=[sg], writes=[sg])
                    dma(P, "sp", scr[oname][orow0 + oc * 128:orow0 + (oc + 1) * 128, t0:t0 + n], sg[:, :n], reads=[sg], writes=[(oname, "o")])

        def proj_tm(wname, wc0, Cn, oname, ocol0, func, bias=None, conv=None, odt=BF16, softplus=False):
            wv, wkeys, nsh = load_w(wname, wc0, Cn, conv)
            bt = None
            if bias is not None:
                bname, bc0 = bias
                bt = bbc[gi[0] % 2]
                dma(P, "sp", bt[:, :Cn], din[bname][l, 0:1, bc0:bc0 + Cn].partition_broadcast(128), writes=[bt])
            for ch in range(NCH):
                c0 = colof(ch * 128)
                ps = psr.next()
                cnt = 0
                for j in range(nsh):
                    sh = (j - 2) if nsh == 5 else 0
                    for kc in range(8):
                        P.op("pe", lambda e, ps=ps, j=j, kc=kc, sh=sh, c0=c0, cnt=cnt: e.matmul(
                            ps[:, :Cn], lhsT=hn[:, kc, c0 + sh:c0 + sh + 128], rhs=wv(j, kc),
                            start=(cnt == 0), stop=(cnt == nsh * 8 - 1)), reads=wkeys, writes=[ps], acc=True)
                        cnt += 1
                sg = stg_b.next() if odt == BF16 else stg_f.next()
                src, skey = ps, ps
                if bt is not None:
                    tf = tmpf.next() if (func is not None or softplus) else sg
                    P.op("dve", lambda e, tf=tf, ps=ps: e.tensor_tensor(out=tf[:, :Cn], in0=ps[:, :Cn], in1=bt[:, :Cn], op=ALU.add),
                         reads=[ps, bt], writes=[tf])
                    src, skey = tf, tf
                if softplus:
                    tf2 = tmpf.next()
                    P.op("act", lambda e, tf2=tf2, src=src: e.activation(out=tf2[:, :Cn], in_=src[:, :Cn], func=AF.Exp), reads=[skey], writes=[tf2])
                    P.op("act", lambda e, tf2=tf2, sg=sg: e.activation(out=sg[:, :Cn], in_=tf2[:, :Cn], func=AF.Ln, bias=C["one1"][:, 0:1], scale=1.0),
                         reads=[tf2], writes=[sg])
                elif func is not None:
                    P.op("act", lambda e, sg=sg, src=src: e.activation(out=sg[:, :Cn], in_=src[:, :Cn], func=func), reads=[skey], writes=[sg])
                dma(P, "sp", scr[oname][ch * 128:(ch + 1) * 128, ocol0:ocol0 + Cn], sg[:, :Cn], reads=[sg], writes=[(oname, "o")])

        ID, SILU, SIG = AF.Identity, AF.Silu, AF.Sigmoid
        proj_fm("w_q", 0, 384, "uq", 0, ID)
        proj_fm("w_kv", 0, 256, "ukv", 0, ID)
        proj_fm("w_kr", 0, 256, "kr", 0, ID)
        proj_fm("w_mqk", 0, 512, "mqk", 0, SILU, bias_name="ml_cb_fm", conv=("ml_cw", 0), post=lambda oc: 0.125 if oc >= 2 else None)
        proj_tm("w_mv", 0, 512, "mv", 0, ID)
        proj_tm("w_mo", 0, 512, "mo", 0, SIG)
        proj_tm("w_mif", 0, 16, "mif", 0, None, bias=("ml_gb", 0), odt=F32)
        proj_fm("w_sB", 0, 512, "sBf", 0, SILU, bias_name="s_cbB_fm", conv=("s_cw", 1024))
        proj_fm("w_sC", 0, 512, "sCf", 0, SILU, bias_name="s_cbC_fm", conv=("s_cw", 1536))
        proj_tm("w_sB", 0, 512, "sBt", 0, SILU, bias=("s_cb", 1024), conv=("s_cw", 1024))
        for h in range(2):
            proj_tm("w_sz", h * 512, 512, "sz", h * 512, SILU)
            proj_tm("w_sx", h * 512, 512, "sx", h * 512, SILU, bias=("s_cb", h * 512), conv=("s_cw", h * 512))
        proj_tm("w_sdt", 0, 32, "sdt", 0, None, bias=("s_dtb", 0), odt=F32, softplus=True)
        for g in range(6):
            proj_fm("w_g", g * 512, 512, "gg", g * 512, SIG)


def rms_rows(k, st, name, src, nch, dst, tag):
    P, C = k.P, k.C
    nf = nch * 128
    s3 = src.rearrange("(c p) t -> p c t", p=128)
    xt = [P.sb("%s_x%d" % (tag, i), [128, nch, 512], BF16, st) for i in range(2)]
    sq = [P.sb("%s_q%d" % (tag, i), [128, nch, 512], BF16, st) for i in range(2)]
    rs = [P.sb("%s_r%d" % (tag, i), [128, 512], F32, st) for i in range(2)]
    for ti, (t0, n) in enumerate(TILES):
        x, q, r = xt[ti % 2], sq[ti % 2], rs[ti % 2]
        ps = k.psum[5 + ti % 2]
        dma(P, "sp", x[:, :, :n], s3[:, :, t0:t0 + n], writes=[x])
        P.op("act", lambda e, x=x, q=q, n=n: e.activation(out=q[:, :, :n], in_=x[:, :, :n], func=AF.Square), reads=[x], writes=[q])
        for c in range(nch):
            P.op("pe", lambda e, ps=ps, q=q, c=c, n=n: e.matmul(ps[:, :n], lhsT=C["ones"][:], rhs=q[:, c, :n], start=(c == 0), stop=(c == nch - 1)),
                 reads=[q], writes=[ps], acc=True)
        P.op("act", lambda e, ps=ps, r=r, n=n: e.activation(out=r[:, :n], in_=ps[:, :n], func=AF.Sqrt, bias=C["eps"][:, 0:1], scale=1.0 / nf),
             reads=[ps], writes=[r])
        P.op("dve", lambda e, r=r, n=n: e.reciprocal(r[:, :n], r[:, :n]), reads=[r], writes=[r])
        P.op("dve", lambda e, x=x, r=r, n=n, t0=t0: e.tensor_tensor(out=dst[:, :, t0:t0 + n], in0=x[:, :, :n],
                                                                   in1=r[:, :n].unsqueeze(1).to_broadcast([128, nch, n]), op=ALU.mult),
             reads=[x, r], writes=[(dst, ti)])


def phaseMLA(k, l, last):
    P, C, din, scr = k.P, k.C, k.din, k.scr
    with ExitStack() as st:
        k.alloc_psum(st, 8)
        uqn = P.sb("M_uqn", [128, 3, T], BF16, st)
        ukvn = P.sb("M_ukvn", [128, 2, T], BF16, st)
        Kh = P.sb("M_Kh", [128, T], BF16, st)
        Qh = P.sb("M_Qh", [128, T], BF16, st)
        Vh = P.sb("M_Vh", [128, NCH, 128], BF16, st)
        wuq = P.sb("M_wuq", [128, 3, 2048], BF16, st)
        wuk = P.sb("M_wuk", [128, 2, 512], BF16, st)
        wuv = P.sb("M_wuv", [128, 2, 512], BF16, st)
        qn = P.sb("M_qn", [128, 3], F32, st)
        kvn = P.sb("M_kvn", [128, 2], F32, st)
        kmax = P.sb("M_kmax", [128, 1], F32, st)
        kmt = P.sb("M_kmt", [128, 1], F32, st)
        with ExitStack() as st2:
            rms_rows(k, st2, "uq", scr["uq"], 3, uqn, "Mq")
            rms_rows(k, st2, "ukv", scr["ukv"], 2, ukvn, "Mk")
            P.barrier()
        P.op("pool", lambda e: e.memset(Kh[96:128, :], 0.0), writes=["Kc0"])
        P.op("pool", lambda e: e.memset(Kh[96:97, :], 1.0), reads=["Kc0"], writes=["Kc1"])
        P.op("pool", lambda e: e.memset(Qh[96:128, :], 0.0), writes=["Qc0"])
        P.op("pool", lambda e: e.memset(Vh[:, :, 64:128], 1.0), writes=["Vc0"])
        dma(P, "sp", qn[:], din["qn_fm"][l], writes=[qn])
        dma(P, "sp", kvn[:], din["kvn_fm"][l], writes=[kvn])
        dma(P, "pool", wuq[:], din["w_uq"][l].rearrange("(kc p) n -> p kc n", p=128), writes=[wuq])
        dma(P, "pool", wuk[:], din["w_uk"][l].rearrange("(kc p) n -> p kc n", p=128), writes=[wuk])
        dma(P, "pool", wuv[:], din["w_uv"][l].rearrange("(kc p) n -> p kc n", p=128), writes=[wuv])
        for kc in range(3):
            P.op("dve", lambda e, kc=kc: e.tensor_scalar(out=wuq[:, kc, :], in0=wuq[:, kc, :], scalar1=qn[:, kc:kc + 1], scalar2=None, op0=ALU.mult),
                 reads=[wuq, qn], writes=[wuq])
        for kc in range(2):
            P.op("dve", lambda e, kc=kc: e.tensor_scalar(out=wuk[:, kc, :], in0=wuk[:, kc, :], scalar1=kvn[:, kc:kc + 1], scalar2=None, op0=ALU.mult),
                 reads=[wuk, kvn], writes=[wuk])
            P.op("dve", lambda e, kc=kc: e.tensor_scalar(out=wuv[:, kc, :], in0=wuv[:, kc, :], scalar1=kvn[:, kc:kc + 1], scalar2=None, op0=ALU.mult),
                 reads=[wuv, kvn], writes=[wuv])
        rp = [P.sb("M_rp%d" % i, [128, 2, 512], F32, st) for i in range(2)]
        kr = [P.sb("M_kr%d" % i, [128, 2, 512], BF16, st) for i in range(2)]
        t1 = [P.sb("M_t1%d" % i, [128, 512], F32, st) for i in range(2)]
        t2 = [P.sb("M_t2%d" % i, [128, 512], F32, st) for i in range(2)]
        for ti, (t0, n) in enumerate(TILES):
            r_, kr_, a_, b_ = rp[ti % 2], kr[ti % 2], t1[ti % 2], t2[ti % 2]
            dma(P, "sp", r_[64:96, :, :n], din["c_rope"][64:96, :, t0:t0 + n], writes=[r_])
            dma(P, "sp", kr_[64:96, 0, :n], scr["kr"][64:96, t0:t0 + n], writes=[(kr_, 0)])
            dma(P, "sp", kr_[64:96, 1, :n], scr["kr"][192:224, t0:t0 + n], writes=[(kr_, 1)])
            P.op("dve", lambda e, r_=r_, kr_=kr_, a_=a_, n=n: e.tensor_tensor(out=a_[64:96, :n], in0=kr_[64:96, 0, :n], in1=r_[64:96, 0, :n], op=ALU.mult),
                 reads=[r_, (kr_, 0)], writes=[a_])
            P.op("dve", lambda e, r_=r_, kr_=kr_, b_=b_, n=n: e.tensor_tensor(out=b_[64:96, :n], in0=kr_[64:96, 1, :n], in1=r_[64:96, 1, :n], op=ALU.mult),
                 reads=[r_, (kr_, 1)], writes=[b_])
            P.op("pool", lambda e, a_=a_, b_=b_, n=n, t0=t0: e.tensor_tensor(out=Kh[64:96, t0:t0 + n], in0=a_[64:96, :n], in1=b_[64:96, :n], op=ALU.add),
                 reads=[a_, b_], writes=[("Kr", ti)])
        P.barrier()

        sqb = [P.sb("M_sq%d" % i, [128, 512], BF16, st) for i in range(2)]
        Et = Rot([P.sb("M_E%d" % i, [128, 512], BF16, st) for i in range(3)])
        dn = [P.sb("M_dn%d" % i, [64, 512], F32, st) for i in range(2)]
        ao = [P.sb("M_ao%d" % i, [64, 512], BF16, st) for i in range(2)]
        mt = [P.sb("M_mt%d" % i, [128, 512], F32, st) for i in range(2)]
        ps_s = Rot(k.psum[0:3])
        ps_o = Rot(k.psum[3:5])
        ps_p = Rot(k.psum[5:7])
        ps_m = k.psum[7]
        for h in range(8):
            for ti, (t0, n) in enumerate(TILES):
                ps = ps_p.next()
                for kc in range(2):
                    P.op("pe", lambda e, ps=ps, kc=kc, n=n, t0=t0, h=h: e.matmul(ps[0:64, :n], lhsT=wuk[:, kc, h * 64:(h + 1) * 64], rhs=ukvn[:, kc, t0:t0 + n],
                                                                            start=(kc == 0), stop=(kc == 1)), reads=[wuk], writes=[ps], acc=True)
                P.op("act", lambda e, ps=ps, n=n, t0=t0: e.copy(Kh[0:64, t0:t0 + n], ps[0:64, :n]), reads=[ps], writes=[("Kn", ti)])
                q = sqb[ti % 2]
                P.op("act", lambda e, q=q, n=n, t0=t0: e.activation(out=q[0:96, :n], in_=Kh[0:96, t0:t0 + n], func=AF.Square), reads=[("Kn", ti)], writes=[q])
                P.op("pe", lambda e, q=q, n=n: e.matmul(ps_m[:, :n], lhsT=C["ones"][0:96, :], rhs=q[0:96, :n], start=True, stop=True), reads=[q], writes=[ps_m])
                if ti == 0:
                    P.op("dve", lambda e, n=n: e.reduce_max(out=kmax[:], in_=ps_m[:, :n], axis=AX.X), reads=[ps_m], writes=[kmax])
                else:
                    P.op("dve", lambda e, n=n: e.reduce_max(out=kmt[:], in_=ps_m[:, :n], axis=AX.X), reads=[ps_m], writes=[kmt])
                    P.op("dve", lambda e: e.tensor_tensor(out=kmax[:], in0=kmax[:], in1=kmt[:], op=ALU.max), reads=[kmax, kmt], writes=[kmax])
            for c0 in range(0, NCH, 8):
                nb = min(8, NCH - c0)
                ps = ps_p.next()
                for j in range(nb):
                    ch = c0 + j
                    for kc in range(2):
                        P.op("pe", lambda e, ps=ps, j=j, ch=ch, kc=kc, h=h: e.matmul(ps[:, j * 64:(j + 1) * 64], lhsT=ukvn[:, kc, ch * 128:(ch + 1) * 128],
                                                                                rhs=wuv[:, kc, h * 64:(h + 1) * 64], start=(kc == 0), stop=(kc == 1)),
                             reads=[wuv], writes=[ps], acc=True)
                P.op("act", lambda e, ps=ps, c0=c0, nb=nb: e.copy(Vh[:, c0:c0 + nb, 0:64], ps[:, :nb * 64].rearrange("p (a b) -> p a b", b=64)),
                     reads=[ps], writes=[("Vh", c0)])
            for ti, (t0, n) in enumerate(TILES):
                pr, pw = ps_p.next(), ps_p.next()
                for kc in range(3):
                    P.op("pe", lambda e, pr=pr, kc=kc, n=n, t0=t0, h=h: e.matmul(pr[:, :n], lhsT=wuq[:, kc, h * 128:(h + 1) * 128], rhs=uqn[:, kc, t0:t0 + n],
                                                                            start=(kc == 0), stop=(kc == 2)), reads=[wuq], writes=[pr], acc=True)
                for kc in range(3):
                    P.op("pe", lambda e, pw=pw, kc=kc, n=n, t0=t0, h=h: e.matmul(pw[:, :n], lhsT=wuq[:, kc, 1024 + h * 128:1024 + (h + 1) * 128], rhs=uqn[:, kc, t0:t0 + n],
                                                                            start=(kc == 0), stop=(kc == 2)), reads=[wuq], writes=[pw], acc=True)
                r_, a_, b_ = rp[ti % 2], t1[ti % 2], t2[ti % 2]
                dma(P, "sp", r_[64:96, :, :n], din["c_rope"][64:96, :, t0:t0 + n], writes=[r_])
                P.op("act", lambda e, pr=pr, n=n, t0=t0: e.copy(Qh[0:64, t0:t0 + n], pr[0:64, :n]), reads=[pr], writes=[("Qn", ti)])
                P.op("dve", lambda e, pr=pr, r_=r_, a_=a_, n=n: e.tensor_tensor(out=a_[64:96, :n], in0=pr[64:96, :n], in1=r_[64:96, 0, :n], op=ALU.mult),
                     reads=[pr, r_], writes=[a_])
                P.op("dve", lambda e, pw=pw, r_=r_, b_=b_, n=n: e.tensor_tensor(out=b_[64:96, :n], in0=pw[64:96, :n], in1=r_[64:96, 1, :n], op=ALU.mult),
                     reads=[pw, r_], writes=[b_])
                P.op("pool", lambda e, a_=a_, b_=b_, n=n, t0=t0: e.tensor_tensor(out=Qh[64:96, t0:t0 + n], in0=a_[64:96, :n], in1=b_[64:96, :n], op=ALU.add),
                     reads=[a_, b_], writes=[("Qr", ti)])
                q = sqb[ti % 2]
                P.op("act", lambda e, q=q, n=n, t0=t0: e.activation(out=q[0:96, :n], in_=Qh[0:96, t0:t0 + n], func=AF.Square),
                     reads=[("Qn", ti), ("Qr", ti)], writes=[q])
                P.op("pe", lambda e, q=q, n=n: e.matmul(ps_m[:, :n], lhsT=C["ones"][0:96, :], rhs=q[0:96, :n], start=True, stop=True), reads=[q], writes=[ps_m])
                m_ = mt[ti % 2]
                P.op("act", lambda e, m_=m_, n=n: e.activation(out=m_[96:97, :n], in_=ps_m[96:97, :n], func=AF.Sqrt, bias=C["zero1"][96:97, 0:1], scale=kmax[96:97, 0:1]),
                     reads=[ps_m, kmax], writes=[m_])
                P.op("dve", lambda e, m_=m_, n=n, t0=t0: e.tensor_scalar(out=Qh[96:97, t0:t0 + n], in0=m_[96:97, :n], scalar1=-1.0, scalar2=None, op0=ALU.mult),
                     reads=[m_], writes=[("Qm", ti)])
            for ti, (t0, n) in enumerate(TILES):
                chunks = [0, 1] if t0 < CTX else list(range(NCH))
                po = ps_o.next()
                for ci, kc in enumerate(chunks):
                    ps = ps_s.next()
                    kti = 0 if kc < 2 else 1 + (kc - 2) // 4
                    P.op("pe", lambda e, ps=ps, kc=kc, n=n, t0=t0: e.matmul(ps[:, :n], lhsT=Kh[:, kc * 128:(kc + 1) * 128], rhs=Qh[:, t0:t0 + n], start=True, stop=True),
                         reads=[("Kn", kti), ("Qn", ti), ("Qr", ti), ("Qm", ti)], writes=[ps])
                    E = Et.next()
                    P.op("act", lambda e, ps=ps, E=E, n=n: e.activation(out=E[:, :n], in_=ps[:, :n], func=AF.Exp, scale=MLA_SCALE), reads=[ps], writes=[E])
                    P.op("pe", lambda e, po=po, E=E, kc=kc, n=n, ci=ci, nc_=len(chunks): e.matmul(po[:, :n], lhsT=Vh[:, kc, :], rhs=E[:, :n], start=(ci == 0), stop=(ci == nc_ - 1)),
                         reads=[E, ("Vh", (kc // 8) * 8)], writes=[po], acc=True)
                d_, a_ = dn[ti % 2], ao[ti % 2]
                P.op("act", lambda e, po=po, d_=d_, n=n: e.copy(d_[:, :n], po[64:128, :n]), reads=[po], writes=[d_])
                P.op("dve", lambda e, d_=d_, n=n: e.reciprocal(d_[:, :n], d_[:, :n]), reads=[d_], writes=[d_])
                P.op("dve", lambda e, po=po, d_=d_, a_=a_, n=n: e.tensor_tensor(out=a_[:, :n], in0=po[0:64, :n], in1=d_[:, :n], op=ALU.mult),
                     reads=[po, d_], writes=[a_])
                dma(P, "sp", scr["att"][h * 64:(h + 1) * 64, t0:t0 + n], a_[:, :n], reads=[a_], writes=["att_o"])


def scan_order(rev):
    return list(range(NCH)) if not rev else [1, 0] + list(range(NCH - 1, 1, -1))


def phaseML(k, l):
    P, C, din, scr = k.P, k.C, k.din, k.scr
    with ExitStack() as st:
        k.alloc_psum(st, 7, 1)
        NG = NCH * 8
        gt = P.sb("L_gt", [128, NCH, 16], F32, st)
        lf = P.sb("L_lf", [128, NCH, 2, 4], F32, st)
        aex = P.sb("L_aex", [128, NG], F32, st)
        esrc = P.sb("L_esrc", [128, NG], F32, st)
        iosc = P.sb("L_iosc", [128, NG], F32, st)
        etot = P.sb("L_etot", [128, NG], F32, st)
        tmpg = P.sb("L_tmpg", [128, NCH, 2, 4], F32, st)
        ngb = P.sb("L_ngb", [128, 512], F32, st)
        dma(P, "sp", gt[:], scr["mif"].rearrange("(c p) g -> p c g", p=128), writes=[gt])
        dma(P, "sp", ngb[:], din["ml_ng"][l, 0:1, :].partition_broadcast(128), writes=[ngb])
        gt5 = gt[:].rearrange("p c (d i h) -> p c d i h", d=2, i=2)
        P.op("act", lambda e: e.activation(out=tmpg[:], in_=gt5[:, :, :, 1, :], func=AF.Exp, scale=-1.0), reads=[gt], writes=[tmpg])
        P.op("act", lambda e: e.activation(out=tmpg[:], in_=tmpg[:], func=AF.Ln, bias=C["one1"][:, 0:1], scale=1.0), reads=[tmpg], writes=[tmpg])
        P.op("dve", lambda e: e.tensor_scalar(out=lf[:], in0=tmpg[:], scalar1=-1.0, scalar2=None, op0=ALU.mult), reads=[tmpg], writes=[lf])
        psA, psT = k.psum[5], k.psum[6]
        for c in range(NCH):
            P.op("pe", lambda e, c=c: e.matmul(psA[:, c * 8:c * 8 + 4], lhsT=C["tri_gt"][:], rhs=lf[:, c, 0, :], start=True, stop=True), reads=[lf], writes=[psA], acc=True)
            P.op("pe", lambda e, c=c: e.matmul(psA[:, c * 8 + 4:c * 8 + 8], lhsT=C["tri_lt"][:], rhs=lf[:, c, 1, :], start=True, stop=True), reads=[lf], writes=[psA], acc=True)
        P.op("pe", lambda e: e.matmul(psT[:, :NG], lhsT=C["ones_f"][:], rhs=lf[:].rearrange("p c d h -> p (c d h)"), start=True, stop=True), reads=[lf], writes=[psT])
        P.op("dve", lambda e: e.tensor_copy(aex[:], psA[:, :NG]), reads=[psA], writes=[aex])
        P.op("act", lambda e: e.activation(out=iosc[:], in_=aex[:], func=AF.Exp), reads=[aex], writes=[iosc])
        P.op("act", lambda e: e.activation(out=etot[:], in_=psT[:, :NG], func=AF.Exp), reads=[psT], writes=[etot])
        P.op("dve", lambda e: e.tensor_tensor(out=esrc[:].rearrange("p (c d h) -> p c d h", d=2, h=4), in0=aex[:].rearrange("p (c d h) -> p c d h", d=2, h=4),
                                              in1=gt5[:, :, :, 0, :], op=ALU.add), reads=[aex, gt], writes=[esrc])
        P.op("act", lambda e: e.activation(out=esrc[:], in_=esrc[:], func=AF.Exp), reads=[esrc], writes=[esrc])
        P.barrier()

        gidx = lambda c, d, h: c * 8 + d * 4 + h
        qk = [P.sb("L_qk%d" % i, [128, 4, 128], BF16, st) for i in range(3)]
        va = [P.sb("L_va%d" % i, [128, 4, 129], BF16, st) for i in range(3)]
        for v_ in va:
            P.op("pool", lambda e, v_=v_: e.memset(v_[:, :, 128:129], 1.0), writes=[(v_, "one")])
        ktm = [P.sb("L_ktm%d" % i, [128, 2, 128], BF16, st) for i in range(2)]
        SM = Rot([P.sb("L_SM%d" % i, [128, 128], BF16, st) for i in range(4)])
        vp = Rot([P.sb("L_vp%d" % i, [128, 129], BF16, st) for i in range(4)])
        Cf = P.sb("L_Cf", [128, 4, 129], F32, st)
        Cb = P.sb("L_Cb", [128, 4, 129], BF16, st)
        ctmp = Rot([P.sb("L_ct%d" % i, [128, 129], F32, st) for i in range(4)])
        mx = Rot([P.sb("L_mx%d" % i, [128, 1], F32, st) for i in range(8)])
        hch = [P.sb("L_h%d" % i, [128, 512], F32, st) for i in range(2)]
        hpv = [P.sb("L_hp%d" % i, [128, 512], F32, st) for i in range(2)]
        mog = [P.sb("L_mo%d" % i, [128, 512], BF16, st) for i in range(2)]
        sq = P.sb("L_sq", [128, 512], F32, st)
        ss = P.sb("L_ss", [128, 4], F32, st)
        mtm = P.sb("L_mtm", [128, 512], BF16, st)
        mfm = [P.sb("L_mfm%d" % i, [128, 4, 128], BF16, st) for i in range(2)]
        ps_s = Rot(k.psum[0:2])
        ps_p = Rot(k.psum[2:4])
        ps_c = Rot(k.psum[4:6])
        pst = k.psbf[0]
        mqk3 = scr["mqk"].rearrange("(c p) t -> p c t", p=128)
        mout3 = scr["mout"].rearrange("(c p) t -> p c t", p=128)
        for d in range(2):
            order = scan_order(d == 1)
            mask = C["tri_le"] if d == 0 else C["tri_ge"]
            if d == 1:
                P.barrier()
            P.op("dve", lambda e: e.memset(Cf[:], 0.0), writes=[(Cf, h) for h in range(4)])
            P.op("pool", lambda e: e.memset(Cb[:], 0.0), writes=[(Cb, h) for h in range(4)])
            for oi, c in enumerate(order):
                q_, v_, kt_ = qk[oi % 3], va[oi % 3], ktm[oi % 2]
                dma(P, "sp", q_[:], mqk3[:, :, c * 128:(c + 1) * 128], writes=[q_])
                dma(P, "sp", v_[:, :, 0:128], scr["mv"][c * 128:(c + 1) * 128, :].rearrange("p (h v) -> p h v", h=4), writes=[v_])
                for j in range(2):
                    P.op("pe", lambda e, q_=q_, j=j: e.transpose(pst[:, j * 128:(j + 1) * 128], q_[:, 2 + j, :], C["ident"][:]), reads=[q_], writes=[pst])
                P.op("act", lambda e, kt_=kt_: e.copy(kt_[:], pst[:, 0:256].rearrange("p (a b) -> p a b", a=2)), reads=[pst], writes=[kt_])
                h_ = hch[oi % 2]
                if d == 1:
                    hp_, mo_ = hpv[oi % 2], mog[oi % 2]
                    dma(P, "sp", hp_[:], scr["hacc"][c * 128:(c + 1) * 128, :], writes=[hp_])
                    dma(P, "sp", mo_[:], scr["mo"][c * 128:(c + 1) * 128, :], writes=[mo_])
                for h in range(4):
                    pb = (h % 2) * 64
                    g = gidx(c, d, h)
                    pss, psp, psc = ps_s.next(), ps_p.next(), ps_c.next()
                    P.op("pe", lambda e, pss=pss, q_=q_, h=h, pb=pb: e.matmul(pss[:, 0:128], lhsT=q_[pb:pb + 64, 2 + h // 2, :], rhs=q_[pb:pb + 64, h // 2, :], start=True, stop=True),
                         reads=[q_], writes=[pss])
                    sm = SM.next()
                    P.op("dve", lambda e, pss=pss, sm=sm, mask=mask: e.tensor_tensor(out=sm[:], in0=pss[:, 0:128], in1=mask[:], op=ALU.mult), reads=[pss], writes=[sm])
                    vp_ = vp.next()
                    P.op("pool", lambda e, vp_=vp_, v_=v_, h=h, g=g: e.tensor_scalar(out=vp_[:], in0=v_[:, h, :], scalar1=esrc[:, g:g + 1], scalar2=None, op0=ALU.mult),
                         reads=[v_, (v_, "one")], writes=[vp_])
                    P.op("pe", lambda e, psp=psp, sm=sm, vp_=vp_: e.matmul(psp[:, 0:129], lhsT=sm[:], rhs=vp_[:], start=True, stop=False), reads=[sm, vp_], writes=[psp], acc=True)
                    P.op("pe", lambda e, psp=psp, q_=q_, h=h, pb=pb: e.matmul(psp[:, 0:129], lhsT=q_[pb:pb + 64, h // 2, :], rhs=Cb[pb:pb + 64, h, :], start=False, stop=True),
                         reads=[q_, (Cb, h)], writes=[psp], acc=True)
                    m_ = mx.next()
                    P.op("dve", lambda e, m_=m_, psp=psp, g=g: e.tensor_scalar(out=m_[:], in0=psp[:, 128:129], scalar1=C["neg1"][:, 0:1], scalar2=iosc[:, g:g + 1], op0=ALU.mult, op1=ALU.max),
                         reads=[psp], writes=[m_])
                    P.op("dve", lambda e, m_=m_, psp=psp: e.tensor_tensor(out=m_[:], in0=psp[:, 128:129], in1=m_[:], op=ALU.max),
                         reads=[psp, m_], writes=[m_])
                    P.op("dve", lambda e, m_=m_: e.reciprocal(m_[:], m_[:]), reads=[m_], writes=[m_])
                    if d == 0:
                        P.op("act", lambda e, h_=h_, psp=psp, m_=m_, h=h: e.activation(out=h_[:, h * 128:(h + 1) * 128], in_=psp[:, 0:128], func=AF.Identity,
                                                                                 bias=C["zero1"][:, 0:1], scale=m_[:, 0:1]), reads=[psp, m_], writes=[(h_, h)])
                    else:
                        P.op("dve", lambda e, h_=h_, psp=psp, m_=m_, h=h, hp_=hp_: e.scalar_tensor_tensor(out=h_[:, h * 128:(h + 1) * 128], in0=psp[:, 0:128], scalar=m_[:, 0:1],
                                                                                                   in1=hp_[:, h * 128:(h + 1) * 128], op0=ALU.mult, op1=ALU.add),
                             reads=[psp, m_, hp_], writes=[(h_, h)])
                    if oi + 1 < len(order):
                        gn = gidx(order[oi + 1], d, h)
                        P.op("pe", lambda e, psc=psc, kt_=kt_, h=h, vp_=vp_: e.matmul(psc[:, 0:129], lhsT=kt_[:, h // 2, :], rhs=vp_[:], start=True, stop=True),
                             reads=[kt_, vp_], writes=[psc])
                        ct = ctmp.next()
                        P.op("dve", lambda e, ct=ct, psc=psc, h=h, pb=pb: e.tensor_tensor(out=ct[pb:pb + 64, :], in0=psc[pb:pb + 64, 0:129], in1=Cf[pb:pb + 64, h, :], op=ALU.add),
                             reads=[psc, (Cf, h)], writes=[ct])
                        P.op("act", lambda e, ct=ct, h=h, pb=pb, gn=gn: e.activation(out=Cf[pb:pb + 64, h, :], in_=ct[pb:pb + 64, :], func=AF.Identity,
                                                                               bias=C["zero1"][pb:pb + 64, 0:1], scale=etot[pb:pb + 64, gn:gn + 1]), reads=[ct], writes=[(Cf, h)])
                        P.op("act", lambda e, ct=ct, h=h, pb=pb, gn=gn: e.activation(out=Cb[pb:pb + 64, h, :], in_=ct[pb:pb + 64, :], func=AF.Identity,
                                                                               bias=C["zero1"][pb:pb + 64, 0:1], scale=etot[pb:pb + 64, gn:gn + 1]), reads=[ct], writes=[(Cb, h)])
                hkeys = [(h_, h) for h in range(4)]
                if d == 0:
                    dma(P, "sp", scr["hacc"][c * 128:(c + 1) * 128, :], h_[:], reads=hkeys, writes=["hacc_o"])
                else:
                    P.op("act", lambda e, h_=h_: e.activation(out=sq[:], in_=h_[:], func=AF.Square), reads=hkeys, writes=[sq])
                    P.op("dve", lambda e: e.reduce_sum(out=ss[:], in_=sq[:].rearrange("p (h v) -> p h v", h=4), axis=AX.X), reads=[sq], writes=[ss])
                    P.op("act", lambda e: e.activation(out=ss[:], in_=ss[:], func=AF.Sqrt, bias=C["eps"][:, 0:1], scale=1.0 / 128), reads=[ss], writes=[ss])
                    P.op("dve", lambda e: e.reciprocal(ss[:], ss[:]), reads=[ss], writes=[ss])
                    P.op("dve", lambda e, h_=h_: e.tensor_tensor(out=sq[:].rearrange("p (h v) -> p h v", h=4), in0=h_[:].rearrange("p (h v) -> p h v", h=4),
                                                                 in1=ss[:].unsqueeze(2).to_broadcast([128, 4, 128]), op=ALU.mult), reads=hkeys + [ss], writes=[sq])
                    P.op("pool", lambda e: e.tensor_tensor(out=sq[:], in0=sq[:], in1=ngb[:], op=ALU.mult), reads=[sq], writes=[sq])
                    P.op("dve", lambda e, mo_=mo_: e.tensor_tensor(out=mtm[:], in0=sq[:], in1=mo_[:], op=ALU.mult), reads=[sq, mo_], writes=[mtm])
                    for j in range(4):
                        P.op("pe", lambda e, j=j: e.transpose(pst[:, 512 + j * 128:512 + (j + 1) * 128], mtm[:, j * 128:(j + 1) * 128], C["ident"][:]), reads=[mtm], writes=[(pst, "m")])
                    mf = mfm[oi % 2]
                    P.op("act", lambda e, mf=mf: e.copy(mf[:], pst[:, 512:1024].rearrange("p (a b) -> p a b", a=4)), reads=[(pst, "m")], writes=[mf])
                    dma(P, "sp", mout3[:, :, c * 128:(c + 1) * 128], mf[:], reads=[mf], writes=["mout_o"])


SSD_DEBUG_LIMIT = None
SSD_PRE_STOP = None


def phaseSSD(k, l):
    P, C, din, scr = k.P, k.C, k.din, k.scr
    with ExitStack() as st:
        k.alloc_psum(st, 7, 1)
        NW = NCH * 32
        dtt = P.sb("S_dtt", [128, 2, NCH, 16], F32, st)
        dta = P.sb("S_dta", [128, 2, NCH, 16], F32, st)
        ainc = P.sb("S_ainc", [128, 2, NCH, 16], F32, st)
        nain = P.sb("S_nain", [128, 2, NCH, 16], F32, st)
        eain = P.sb("S_eain", [128, 2, NCH, 16], F32, st)
        wsrc = P.sb("S_wsrc", [128, 2, NCH, 16], F32, st)
        etot = P.sb("S_etot", [128, 2, NCH, 16], F32, st)
        abc = P.sb("S_abc", [128, 32], F32, st)
        dbc = P.sb("S_dbc", [128, 16], F32, st)
        sngb = P.sb("S_sngb", [128, 1024], F32, st)
        negm = [P.sb("S_negm%d" % i, [128, 4, 128], BF16, st) for i in range(2)]
        dsp = [P.sb("S_dsp%d" % i, [128, 2, NCH, 16], BF16, st) for i in range(3)]
        dr1 = P.sb("S_dr1", [128, 2, NCH, 16], F32, st)
        for d_ in range(2):
            dma(P, "sp", dtt[:, d_], scr["sdt"][:, d_ * 16:(d_ + 1) * 16].rearrange("(c p) h -> p c h", p=128), writes=[(dtt, d_)])
        dma(P, "sp", abc[:], din["s_alog"][l, 0:1, :].partition_broadcast(128), writes=[abc])
        dma(P, "sp", dbc[:], din["s_d"][l, 0:1, :].partition_broadcast(128), writes=[dbc])
        dma(P, "sp", sngb[:], din["s_ng"][l, 0:1, :].partition_broadcast(128), writes=[sngb])
        if SSD_PRE_STOP == 1:
            return
        P.op("act", lambda e: e.activation(out=abc[:], in_=abc[:], func=AF.Exp), reads=[abc], writes=[abc])
        P.op("dve", lambda e: e.tensor_scalar(out=abc[:], in0=abc[:], scalar1=-1.0, scalar2=None, op0=ALU.mult), reads=[abc], writes=[abc])
        for d_ in range(2):
            P.op("dve", lambda e, d_=d_: e.tensor_tensor(out=dta[:, d_], in0=dtt[:, d_], in1=abc[:, d_ * 16:(d_ + 1) * 16].unsqueeze(1).to_broadcast([128, NCH, 16]), op=ALU.mult), reads=[(dtt, d_), abc], writes=[(dta, d_)])
        P.op("dve", lambda e: e.tensor_copy(dsp[0][:], dta[:]), reads=[(dta, 0), (dta, 1)], writes=[dsp[0]])
        P.op("dve", lambda e: e.tensor_tensor(out=dr1[:], in0=dta[:], in1=dsp[0][:], op=ALU.subtract), reads=[(dta, 0), (dta, 1), dsp[0]], writes=[dr1])
        P.op("dve", lambda e: e.tensor_copy(dsp[1][:], dr1[:]), reads=[dr1], writes=[dsp[1]])
        P.op("dve", lambda e: e.tensor_tensor(out=dsp[2][:], in0=dr1[:], in1=dsp[1][:], op=ALU.subtract), reads=[dr1, dsp[1]], writes=[dsp[2]])
        P.op("dve", lambda e: e.tensor_scalar(out=negm[0][:], in0=C["tri_gt"][:].unsqueeze(1).to_broadcast([128, 4, 128]), scalar1=NEG, scalar2=None, op0=ALU.mult), writes=[negm[0]])
        P.op("dve", lambda e: e.tensor_scalar(out=negm[1][:], in0=C["tri_lt"][:].unsqueeze(1).to_broadcast([128, 4, 128]), scalar1=NEG, scalar2=None, op0=ALU.mult), writes=[negm[1]])
        if SSD_PRE_STOP == 2:
            return
        def cums(tri0, tri1, post):
            for d_ in range(2):
                for hf_ in range(2):
                    ps = k.psum[d_ * 2 + hf_]
                    ca = hf_ * 17
                    for si in range(3):
                        P.op("pe", lambda e, ps=ps, d_=d_, ca=ca, si=si: e.matmul(ps[:, :272], lhsT=C[(tri0, tri1)[d_]][:], rhs=dsp[si][:, d_, ca:ca + 17, :].rearrange("p c h -> p (c h)"), start=(si == 0), stop=(si == 2)),
                             reads=[dsp[si]], writes=[ps], acc=True)
                    post(ps, d_, ca)

        def post_inc(ps, d_, ca):
            v = lambda t: t[:, d_, ca:ca + 17, :].rearrange("p c h -> p (c h)")
            P.op("dve", lambda e: e.tensor_copy(v(ainc), ps[:, :272]), reads=[ps], writes=[(ainc, d_, ca)])
            P.op("act", lambda e: e.activation(out=v(eain), in_=ps[:, :272], func=AF.Exp), reads=[ps], writes=[(eain, d_, ca)])
            P.op("dve", lambda e: e.tensor_scalar(out=v(nain), in0=v(ainc), scalar1=-1.0, scalar2=None, op0=ALU.mult), reads=[(ainc, d_, ca)], writes=[(nain, d_, ca)])

        def post_exc(ps, d_, ca):
            v = lambda t: t[:, d_, ca:ca + 17, :].rearrange("p c h -> p (c h)")
            P.op("act", lambda e: e.activation(out=v(wsrc), in_=ps[:, :272], func=AF.Exp), reads=[ps], writes=[(wsrc, d_, ca)])
            P.op("dve", lambda e: e.tensor_tensor(out=v(wsrc), in0=v(wsrc), in1=v(dtt), op=ALU.mult), reads=[(wsrc, d_, ca)], writes=[(wsrc, d_, ca)])

        def post_tot(ps, d_, ca):
            v = lambda t: t[:, d_, ca:ca + 17, :].rearrange("p c h -> p (c h)")
            P.op("act", lambda e: e.activation(out=v(etot), in_=ps[:, :272], func=AF.Exp), reads=[ps], writes=[(etot, d_, ca)])

        cums("tri_le_b", "tri_ge_b", post_inc)
        P.barrier()
        if SSD_PRE_STOP == 3:
            return
        cums("tri_gt_b", "tri_lt_b", post_exc)
        P.barrier()
        if SSD_PRE_STOP == 4:
            return
        cums("ones", "ones", post_tot)
        P.barrier()
        if SSD_PRE_STOP == 5:
            return

        xt = [P.sb("S_x%d" % i, [128, 1024], BF16, st) for i in range(2)]
        Bt = [P.sb("S_Bt%d" % i, [128, 512], BF16, st) for i in range(2)]
        Bf = [P.sb("S_Bf%d" % i, [128, 4, 128], BF16, st) for i in range(2)]
        Cf = [P.sb("S_Cf%d" % i, [128, 4, 128], BF16, st) for i in range(2)]
        X = [P.sb("S_X%d" % i, [128, 2, 16, 128], BF16, st) for i in range(2)]
        cbm = [P.sb("S_cbm%d" % i, [128, 4, 128], F32, st) for i in range(2)]
        Eh = [P.sb("S_E%d" % i, [128, 8, 128], F32, st) for i in range(2)]
        MT = [P.sb("S_MT%d" % i, [128, 8, 128], BF16, st) for i in range(2)]
        xw = [P.sb("S_xw%d" % i, [128, 1024], BF16, st) for i in range(2)]
        Hf = P.sb("S_Hf", [128, 1024], F32, st)
        Hb = P.sb("S_Hb", [128, 1024], BF16, st)
        ht = P.sb("S_ht", [128, 1024], F32, st)
        ych = [P.sb("S_y%d" % i, [128, 1024], F32, st) for i in range(2)]
        ypv = [P.sb("S_yp%d" % i, [128, 1024], F32, st) for i in range(2)]
        szt = [P.sb("S_sz%d" % i, [128, 1024], BF16, st) for i in range(2)]
        y2 = P.sb("S_y2", [128, 1024], F32, st)
        sq = P.sb("S_sq", [128, 1024], F32, st)
        ss = P.sb("S_ss", [128, 4], F32, st)
        stm = P.sb("S_stm", [128, 1024], BF16, st)
        sfm = [P.sb("S_sfm%d" % i, [128, 8, 128], BF16, st) for i in range(2)]
        ps_cb = k.psum[0]
        ps_A = Rot([(k.psum[1], k.psum[2]), (k.psum[3], k.psum[4])])
        ps_y, ps_i, ps_h = k.psum[5], k.psum[6], k.psum[0]
        pst = k.psbf[0]
        sBf3 = scr["sBf"].rearrange("(g p) t -> p g t", p=128)
        sCf3 = scr["sCf"].rearrange("(g p) t -> p g t", p=128)
        sout3 = scr["sout"].rearrange("(c p) t -> p c t", p=128)
        for d in range(2):
            order = scan_order(d == 1)
            mask = C["tri_le"] if d == 0 else C["tri_ge"]
            maskb = C["tri_le_b"] if d == 0 else C["tri_ge_b"]
            if d == 1:
                P.barrier()
            P.op("dve", lambda e: e.memset(Hf[:], 0.0), writes=[Hf])
            P.op("pool", lambda e: e.memset(Hb[:], 0.0), writes=[Hb])
            for oi, c in enumerate(order):
                if SSD_DEBUG_LIMIT is not None and (d * NCH + oi) >= SSD_DEBUG_LIMIT:
                    break
                i2 = oi % 2
                x_, bt_, bf_, cf_, X_, cb_, xw_, y_ = xt[i2], Bt[i2], Bf[i2], Cf[i2], X[i2], cbm[i2], xw[i2], ych[i2]
                dma(P, "sp", x_[:], scr["sx"][c * 128:(c + 1) * 128, :], writes=[x_])
                dma(P, "sp", bt_[:], scr["sBt"][c * 128:(c + 1) * 128, :], writes=[bt_])
                dma(P, "sp", bf_[:], sBf3[:, :, c * 128:(c + 1) * 128], writes=[bf_])
                dma(P, "sp", cf_[:], sCf3[:, :, c * 128:(c + 1) * 128], writes=[cf_])
                if d == 1:
                    yp_, sz_ = ypv[i2], szt[i2]
                    dma(P, "sp", yp_[:], scr["yacc"][c * 128:(c + 1) * 128, :], writes=[yp_])
                    dma(P, "sp", sz_[:], scr["sz"][c * 128:(c + 1) * 128, :], writes=[sz_])
                go = d * 16
                for g in range(4):
                    P.op("pe", lambda e, g=g, bf_=bf_, cf_=cf_: e.matmul(ps_cb[:, g * 128:(g + 1) * 128], lhsT=bf_[:, g, :], rhs=cf_[:, g, :], start=True, stop=True),
                         reads=[bf_, cf_], writes=[ps_cb], acc=True)
                P.op("dve", lambda e, cb_=cb_, mask=mask: e.tensor_tensor(out=cb_[:], in0=ps_cb[:].rearrange("p (g t) -> p g t", g=4), in1=mask[:].unsqueeze(1).to_broadcast([128, 4, 128]), op=ALU.mult),
                     reads=[ps_cb], writes=[cb_])
                for si in range(2):
                    P.op("dve", lambda e, X_=X_, c=c, d=d, maskb=maskb, si=si: e.tensor_tensor(out=X_[:, si], in0=maskb[:].unsqueeze(1).to_broadcast([128, 16, 128]),
                                                                                        in1=dsp[si][:, d, c, :].unsqueeze(2).to_broadcast([128, 16, 128]), op=ALU.mult), writes=[(X_, si)])
                for hf in range(2):
                    pA = ps_A.next()
                    E_, M_ = Eh[hf], MT[hf]
                    for j in range(2):
                        gg = hf * 2 + j
                        for si in range(2):
                            P.op("pe", lambda e, pA=pA, j=j, gg=gg, X_=X_, si=si: e.matmul(pA[j][:, :], lhsT=C["ones"][:], rhs=X_[:, si, gg * 4:(gg + 1) * 4, :].rearrange("p h t -> p (h t)"), start=(si == 0), stop=False),
                                 reads=[(X_, si)], writes=[pA[j]], acc=True)
                        P.op("pe", lambda e, pA=pA, j=j, d=d: e.matmul(pA[j][:, :], lhsT=C["ident"][:], rhs=negm[d][:].rearrange("p h t -> p (h t)"), start=False, stop=True),
                             reads=[negm[d]], writes=[pA[j]], acc=True)
                    for hh in range(8):
                        h = hf * 8 + hh
                        P.op("act", lambda e, pA=pA, hh=hh, h=h, E_=E_, c=c, d=d: e.activation(out=E_[:, hh, :], in_=pA[hh // 4][:, (hh % 4) * 128:(hh % 4 + 1) * 128], func=AF.Exp,
                                                                                         bias=nain[:, d, c, h:h + 1], scale=1.0), reads=[pA[hh // 4]], writes=[(E_, hh)])
                        P.op("dve", lambda e, hh=hh, h=h, E_=E_, M_=M_, cb_=cb_, c=c, d=d: e.scalar_tensor_tensor(out=M_[:, hh, :], in0=E_[:, hh, :], scalar=dtt[:, d, c, h:h + 1],
                                                                                                        in1=cb_[:, h // 4, :], op0=ALU.mult, op1=ALU.mult),
                             reads=[(E_, hh), cb_], writes=[(M_, hh)])
                    for hh in range(8):
                        h = hf * 8 + hh
                        P.op("pe", lambda e, hh=hh, h=h, M_=M_, x_=x_: e.matmul(ps_y[:, hh * 64:(hh + 1) * 64], lhsT=M_[:, hh, :], rhs=x_[:, h * 64:(h + 1) * 64], start=True, stop=True),
                             reads=[(M_, hh), x_], writes=[ps_y], acc=True)
                    for j in range(2):
                        gg = hf * 2 + j
                        P.op("pe", lambda e, j=j, gg=gg, cf_=cf_: e.matmul(ps_i[:, j * 256:(j + 1) * 256], lhsT=cf_[:, gg, :], rhs=Hb[:, gg * 256:(gg + 1) * 256], start=True, stop=True),
                             reads=[cf_, Hb], writes=[ps_i], acc=True)
                    ysl = y_[:, hf * 512:(hf + 1) * 512]
                    P.op("dve", lambda e, ysl=ysl, c=c, d=d, hf=hf: e.tensor_tensor(out=ysl.rearrange("p (h q) -> p h q", h=8), in0=ps_i[:].rearrange("p (h q) -> p h q", h=8),
                                                                                 in1=eain[:, d, c, hf * 8:hf * 8 + 8].unsqueeze(2).to_broadcast([128, 8, 64]), op=ALU.mult),
                         reads=[ps_i], writes=[(y_, hf)])
                    P.op("dve", lambda e, ysl=ysl: e.tensor_tensor(out=ysl, in0=ps_y[:], in1=ysl, op=ALU.add), reads=[ps_y, (y_, hf)], writes=[(y_, hf)])
                    if d == 1:
                        P.op("pool", lambda e, ysl=ysl, yp_=yp_, hf=hf: e.tensor_tensor(out=ysl, in0=ysl, in1=yp_[:, hf * 512:(hf + 1) * 512], op=ALU.add),
                             reads=[(y_, hf), yp_], writes=[(y_, hf)])
                if oi + 1 < len(order):
                    P.op("dve", lambda e, xw_=xw_, x_=x_, c=c, d=d: e.tensor_tensor(out=xw_[:].rearrange("p (h q) -> p h q", h=16), in0=x_[:].rearrange("p (h q) -> p h q", h=16),
                                                                                  in1=wsrc[:, d, c, :].unsqueeze(2).to_broadcast([128, 16, 64]), op=ALU.mult),
                         reads=[x_], writes=[xw_])
                    P.op("dve", lambda e, c=c, d=d: e.tensor_tensor(out=ht[:].rearrange("p (h q) -> p h q", h=16), in0=Hf[:].rearrange("p (h q) -> p h q", h=16),
                                                                      in1=etot[:, d, c, :].unsqueeze(2).to_broadcast([128, 16, 64]), op=ALU.mult),
                         reads=[Hf], writes=[ht])
                    for hf in range(2):
                        for j in range(2):
                            gg = hf * 2 + j
                            P.op("pe", lambda e, j=j, gg=gg, bt_=bt_, xw_=xw_: e.matmul(ps_h[:, j * 256:(j + 1) * 256], lhsT=bt_[:, gg * 128:(gg + 1) * 128], rhs=xw_[:, gg * 256:(gg + 1) * 256], start=True, stop=True),
                                 reads=[bt_, xw_], writes=[ps_h], acc=True)
                        P.op("dve", lambda e, hf=hf: e.tensor_tensor(out=Hf[:, hf * 512:(hf + 1) * 512], in0=ps_h[:], in1=ht[:, hf * 512:(hf + 1) * 512], op=ALU.add),
                             reads=[ps_h, ht], writes=[Hf])
                    P.op("act", lambda e: e.copy(Hb[:], Hf[:]), reads=[Hf], writes=[Hb])
                ykeys = [(y_, 0), (y_, 1)]
                if d == 0:
                    dma(P, "sp", scr["yacc"][c * 128:(c + 1) * 128, :], y_[:], reads=ykeys, writes=["yacc_o"])
                else:
                    P.op("dve", lambda e, x_=x_: e.tensor_tensor(out=y2[:].rearrange("p (h q) -> p h q", h=16), in0=x_[:].rearrange("p (h q) -> p h q", h=16),
                                                                  in1=dbc[:].unsqueeze(2).to_broadcast([128, 16, 64]), op=ALU.mult), reads=[x_], writes=[y2])
                    P.op("dve", lambda e, y_=y_: e.tensor_tensor(out=y2[:], in0=y2[:], in1=y_[:], op=ALU.add), reads=[y2] + ykeys, writes=[y2])
                    P.op("dve", lambda e, sz_=sz_: e.tensor_tensor(out=y2[:], in0=y2[:], in1=sz_[:], op=ALU.mult), reads=[y2, sz_], writes=[y2])
                    P.op("act", lambda e: e.activation(out=sq[:], in_=y2[:], func=AF.Square), reads=[y2], writes=[sq])
                    P.op("dve", lambda e: e.reduce_sum(out=ss[:], in_=sq[:].rearrange("p (g v) -> p g v", g=4), axis=AX.X), reads=[sq], writes=[ss])
                    P.op("act", lambda e: e.activation(out=ss[:], in_=ss[:], func=AF.Sqrt, bias=C["eps"][:, 0:1], scale=1.0 / 256), reads=[ss], writes=[ss])
                    P.op("dve", lambda e: e.reciprocal(ss[:], ss[:]), reads=[ss], writes=[ss])
                    P.op("dve", lambda e: e.tensor_tensor(out=sq[:].rearrange("p (g v) -> p g v", g=4), in0=y2[:].rearrange("p (g v) -> p g v", g=4),
                                                          in1=ss[:].unsqueeze(2).to_broadcast([128, 4, 256]), op=ALU.mult), reads=[y2, ss], writes=[sq])
                    P.op("pool", lambda e: e.tensor_tensor(out=stm[:], in0=sq[:], in1=sngb[:], op=ALU.mult), reads=[sq], writes=[stm])
                    sf = sfm[i2]
                    for j in range(8):
                        P.op("pe", lambda e, j=j: e.transpose(pst[:, j * 128:(j + 1) * 128], stm[:, j * 128:(j + 1) * 128], C["ident"][:]), reads=[stm], writes=[pst])
                    P.op("act", lambda e, sf=sf: e.copy(sf[:], pst[:].rearrange("p (a b) -> p a b", a=8)), reads=[pst], writes=[sf])
                    dma(P, "sp", sout3[:, :, c * 128:(c + 1) * 128], sf[:], reads=[sf], writes=["sout_o"])


def phaseOUT(k, l):
    P, C, din, scr = k.P, k.C, k.din, k.scr
    with ExitStack() as st:
        k.alloc_psum(st, 8)
        wa = P.sb("O_wa", [64, 8, 1024], BF16, st)
        wm = P.sb("O_wm", [128, 4, 1024], BF16, st)
        ws = P.sb("O_ws", [128, 8, 1024], BF16, st)
        wo = P.sb("O_wo", [128, 8, 1024], BF16, st)
        wrf = P.sb("O_wrf", [128, 8, 32], F32, st)
        wrh = P.sb("O_wrh", [128, 8, 32], BF16, st)
        wrl = P.sb("O_wrl", [128, 8, 32], BF16, st)
        brt = P.sb("O_brt", [128, 32], F32, st)
        dma(P, "pool", wa[:], din["w_bra"][l].rearrange("(h p) n -> p h n", p=64), writes=[wa])
        dma(P, "pool", wm[:], din["w_brm"][l].rearrange("(c p) n -> p c n", p=128), writes=[wm])
        dma(P, "pool", ws[:], din["w_brs"][l].rearrange("(c p) n -> p c n", p=128), writes=[ws])
        dma(P, "pool", wo[:], din["w_out"][l].rearrange("(c p) n -> p c n", p=128), writes=[wo])
        dma(P, "sp", wrf[:], din["w_rt"][l].rearrange("(c p) n -> p c n", p=128), writes=[wrf])
        dma(P, "sp", brt[:], din["b_rt"][l, 0:1, :].partition_broadcast(128), writes=[brt])
        P.op("dve", lambda e: e.tensor_copy(wrh[:], wrf[:]), reads=[wrf], writes=[wrh])
        P.op("dve", lambda e: e.tensor_tensor(out=wrl[:], in0=wrf[:], in1=wrh[:], op=ALU.subtract), reads=[wrf, wrh], writes=[wrl])
        att = P.sb("O_att", [64, 8, 512], BF16, st)
        mo_ = P.sb("O_mo", [128, 4, 512], BF16, st)
        so_ = P.sb("O_so", [128, 8, 512], BF16, st)
        gg_ = P.sb("O_gg", [128, 24, 512], BF16, st)
        xr = P.sb("O_xr", [128, 8, 512], F32, st)
        mg = P.sb("O_mg", [128, 8, 512], BF16, st)
        tt = [[P.sb("O_t%d%d" % (i, j), [128, 512], F32, st) for j in range(3)] for i in range(2)]
        sq = P.sb("O_sq", [128, 8, 512], BF16, st)
        rs = P.sb("O_rs", [128, 512], F32, st)
        hfc = [P.sb("O_hf%d" % i, [128, 512], F32, st) for i in range(2)]
        h2b = P.sb("O_h2b", [128, 8, 512], BF16, st)
        hlo = P.sb("O_hlo", [128, 8, 512], BF16, st)
        lg = P.sb("O_lg", [128, 4, 32], F32, st)
        mk = P.sb("O_mk", [128, 4, 32], F32, st)
        ex = P.sb("O_ex", [128, 4, 32], F32, st)
        mx8 = P.sb("O_mx8", [128, 4, 8], F32, st)
        nmx = P.sb("O_nmx", [128, 4], F32, st)
        sm = P.sb("O_sm", [128, 4], F32, st)
        gfs = P.sb("O_gfs", [32, 512], F32, st)
        att3 = scr["att"].rearrange("(h p) t -> p h t", p=64)
        mo3 = scr["mout"].rearrange("(c p) t -> p c t", p=128)
        so3 = scr["sout"].rearrange("(c p) t -> p c t", p=128)
        gg3 = scr["gg"].rearrange("(c p) t -> p c t", p=128)
        xr3 = scr["xres"].rearrange("(c p) t -> p c t", p=128)
        h23 = scr["hn2"].rearrange("(c p) t -> p c t", p=128)
        modv, a2 = k.modv, k.a2
        for ti, (t0, n) in enumerate(TILES):
            mc = 1 if t0 < CTX else 0
            nj = n // 128
            dma(P, "sp", att[:, :, :n], att3[:, :, t0:t0 + n], writes=[att])
            dma(P, "sp", mo_[:, :, :n], mo3[:, :, t0:t0 + n], writes=[mo_])
            dma(P, "sp", so_[:, :, :n], so3[:, :, t0:t0 + n], writes=[so_])
            for g3 in range(3):
                dma(P, "sp", gg_[:, g3 * 8:(g3 + 1) * 8, :n], gg3[:, g3 * 8:(g3 + 1) * 8, t0:t0 + n], writes=[(gg_, g3)])
            dma(P, "sp", xr[:, :, :n], xr3[:, :, t0:t0 + n], writes=[xr] + [(xr, "n", oc) for oc in range(8)])
            for oc in range(8):
                pp = k.psum[(oc % 2) * 3:(oc % 2) * 3 + 3]
                tq = tt[oc % 2]
                for h in range(8):
                    P.op("pe", lambda e, p0=pp[0], h=h, oc=oc, n=n: e.matmul(p0[:, :n], lhsT=wa[:, h, oc * 128:(oc + 1) * 128], rhs=att[:, h, :n], start=(h == 0), stop=(h == 7)),
                         reads=[wa, att], writes=[pp[0]], acc=True)
                for c in range(4):
                    P.op("pe", lambda e, p1=pp[1], c=c, oc=oc, n=n: e.matmul(p1[:, :n], lhsT=wm[:, c, oc * 128:(oc + 1) * 128], rhs=mo_[:, c, :n], start=(c == 0), stop=(c == 3)),
                         reads=[wm, mo_], writes=[pp[1]], acc=True)
                for c in range(8):
                    P.op("pe", lambda e, p2=pp[2], c=c, oc=oc, n=n: e.matmul(p2[:, :n], lhsT=ws[:, c, oc * 128:(oc + 1) * 128], rhs=so_[:, c, :n], start=(c == 0), stop=(c == 7)),
                         reads=[ws, so_], writes=[pp[2]], acc=True)
                for b in range(3):
                    P.op("dve", lambda e, b=b, pb=pp[b], tq=tq, oc=oc, n=n: e.tensor_tensor(out=tq[b][:, :n], in0=pb[:, :n], in1=gg_[:, b * 8 + oc, :n], op=ALU.mult),
                         reads=[pp[b], (gg_, b)], writes=[tq[b]])
                P.op("pool", lambda e, tq=tq, n=n: e.tensor_tensor(out=tq[0][:, :n], in0=tq[0][:, :n], in1=tq[1][:, :n], op=ALU.add), reads=[tq[0], tq[1]], writes=[tq[0]])
                P.op("pool", lambda e, tq=tq, n=n, oc=oc: e.tensor_tensor(out=mg[:, oc, :n], in0=tq[0][:, :n], in1=tq[2][:, :n], op=ALU.add), reads=[tq[0], tq[2]], writes=[(mg, oc)])
            for oc in range(8):
                ps = k.psum[6 + oc % 2]
                for c in range(8):
                    P.op("pe", lambda e, ps=ps, c=c, oc=oc, n=n: e.matmul(ps[:, :n], lhsT=wo[:, c, oc * 128:(oc + 1) * 128], rhs=mg[:, c, :n], start=(c == 0), stop=(c == 7)),
                         reads=[wo] + [(mg, c2) for c2 in range(8)], writes=[ps], acc=True)
                P.op("dve", lambda e, ps=ps, oc=oc, n=n, mc=mc: e.scalar_tensor_tensor(out=xr[:, oc, :n], in0=ps[:, :n], scalar=modv[:, 16 + oc, mc:mc + 1], in1=xr[:, oc, :n], op0=ALU.mult, op1=ALU.add),
                     reads=[ps, xr], writes=[(xr, "n", oc)])
            xkeys = [(xr, "n", oc) for oc in range(8)]
            dma(P, "sp", xr3[:, :, t0:t0 + n], xr[:, :, :n], reads=xkeys, writes=["xres_o"])
            P.op("act", lambda e, n=n: e.activation(out=sq[:, :, :n], in_=xr[:, :, :n], func=AF.Square), reads=xkeys, writes=[sq])
            ps = k.psum[6]
            for c in range(8):
                P.op("pe", lambda e, ps=ps, c=c, n=n: e.matmul(ps[:, :n], lhsT=C["ones"][:], rhs=sq[:, c, :n], start=(c == 0), stop=(c == 7)), reads=[sq], writes=[ps], acc=True)
            P.op("act", lambda e, ps=ps, n=n: e.activation(out=rs[:, :n], in_=ps[:, :n], func=AF.Sqrt, bias=C["eps"][:, 0:1], scale=1.0 / D), reads=[ps], writes=[rs])
            P.op("dve", lambda e, n=n: e.reciprocal(rs[:, :n], rs[:, :n]), reads=[rs], writes=[rs])
            for c in range(8):
                hf = hfc[c % 2]
                P.op("dve", lambda e, hf=hf, c=c, n=n: e.tensor_tensor(out=hf[:, :n], in0=xr[:, c, :n], in1=rs[:, :n], op=ALU.mult), reads=xkeys + [rs, "xres_o"], writes=[hf])
                P.op("act", lambda e, hf=hf, c=c, n=n, mc=mc: e.activation(out=hf[:, :n], in_=hf[:, :n], func=AF.Identity, bias=modv[:, 24 + c, mc:mc + 1], scale=a2[:, c, mc:mc + 1]),
                     reads=[hf], writes=[hf])
                P.op("dve", lambda e, hf=hf, c=c, n=n: e.tensor_copy(h2b[:, c, :n], hf[:, :n]), reads=[hf], writes=[(h2b, c)])
                P.op("dve", lambda e, hf=hf, c=c, n=n: e.tensor_tensor(out=hlo[:, c, :n], in0=hf[:, :n], in1=h2b[:, c, :n], op=ALU.subtract), reads=[hf, (h2b, c)], writes=[(hlo, c)])
            hkeys = [(h2b, c) for c in range(8)]
            dma(P, "sp", h23[:, :, t0:t0 + n], h2b[:, :, :n], reads=hkeys, writes=["hn2_o"])
            pr = k.psum[7]
            for j in range(nj):
                cnt = 0
                for (ha, wb_) in ((h2b, wrh), (h2b, wrl), (hlo, wrh)):
                    for c in range(8):
                        P.op("pe", lambda e, j=j, c=c, ha=ha, wb_=wb_, cnt=cnt: e.matmul(pr[:, j * 32:(j + 1) * 32], lhsT=ha[:, c, j * 128:(j + 1) * 128], rhs=wb_[:, c, :], start=(cnt == 0), stop=(cnt == 23)),
                             reads=hkeys + [(hlo, c2) for c2 in range(8)] + [wrh, wrl], writes=[pr], acc=True)
                        cnt += 1
            P.op("dve", lambda e, nj=nj: e.tensor_tensor(out=lg[:, :nj, :], in0=pr[:, :nj * 32].rearrange("p (j x) -> p j x", x=32), in1=brt[:].unsqueeze(1).to_broadcast([128, nj, 32]), op=ALU.add),
                 reads=[pr, brt], writes=[lg])
            for j in range(nj):
                P.op("dve", lambda e, j=j: e.max(out=mx8[:, j, :], in_=lg[:, j, :]), reads=[lg], writes=[(mx8, j)])
            mkeys = [(mx8, j) for j in range(nj)]
            P.op("dve", lambda e, nj=nj: e.tensor_scalar(out=nmx[:, :nj], in0=mx8[:, :nj, 0], scalar1=-1.0, scalar2=None, op0=ALU.mult), reads=mkeys, writes=[nmx])
            for j in range(nj):
                P.op("dve", lambda e, j=j: e.tensor_scalar(out=mk[:, j, :], in0=lg[:, j, :], scalar1=mx8[:, j, 3:4], scalar2=C["big"][:, 0:1], op0=ALU.subtract, op1=ALU.mult), reads=[lg] + mkeys, writes=[(mk, j)])
                P.op("dve", lambda e, j=j: e.tensor_scalar(out=mk[:, j, :], in0=mk[:, j, :], scalar1=1.0, scalar2=0.0, op0=ALU.add, op1=ALU.max), reads=[(mk, j)], writes=[(mk, j)])
                P.op("dve", lambda e, j=j: e.tensor_scalar(out=mk[:, j, :], in0=mk[:, j, :], scalar1=1.0, scalar2=None, op0=ALU.min), reads=[(mk, j)], writes=[(mk, j)])
                P.op("act", lambda e, j=j: e.activation(out=ex[:, j, :], in_=lg[:, j, :], func=AF.Exp, bias=nmx[:, j:j + 1], scale=1.0), reads=[lg, nmx], writes=[(ex, j)])
                P.op("dve", lambda e, j=j: e.tensor_tensor(out=ex[:, j, :], in0=ex[:, j, :], in1=mk[:, j, :], op=ALU.mult), reads=[(ex, j), (mk, j)], writes=[(ex, j)])
            ekeys = [(ex, j) for j in range(nj)]
            P.op("dve", lambda e, nj=nj: e.reduce_sum(out=sm[:, :nj], in_=ex[:, :nj, :], axis=AX.X), reads=ekeys, writes=[sm])
            P.op("dve", lambda e, nj=nj: e.reciprocal(sm[:, :nj], sm[:, :nj]), reads=[sm], writes=[sm])
            P.op("dve", lambda e, nj=nj: e.tensor_tensor(out=ex[:, :nj, :], in0=ex[:, :nj, :], in1=sm[:, :nj].unsqueeze(2).to_broadcast([128, nj, 32]), op=ALU.mult), reads=ekeys + [sm], writes=ekeys)
            pg = k.psum[6]
            for j in range(nj):
                P.op("pe", lambda e, j=j: e.transpose(pg[0:32, j * 128:(j + 1) * 128], ex[:, j, :], C["ident_f"][:]), reads=ekeys, writes=[pg], acc=True)
            P.op("act", lambda e, n=n: e.copy(gfs[:, :n], pg[0:32, :n]), reads=[pg], writes=[gfs])
            dma(P, "sp", scr["gfm"][:, t0:t0 + n], gfs[:, :n], reads=[gfs], writes=["gfm_o"])


MOE_BLOCKS = [
    [(0, 256), (256, 384), (640, 512)],
    [(1152, 384), (1536, 384), (1920, 384)],
    [(2304, 512), (2816, 512)],
    [(3328, 512), (3840, 512)],
]
MOE_EXPERTS = NE


def phaseMOE(k, l):
    P, C, din, scr = k.P, k.C, k.din, k.scr
    with ExitStack() as st:
        k.alloc_psum(st, 8)
        NB = 1152
        h2 = P.sb("E_h2", [128, 8, NB], BF16, st)
        Gt = P.sb("E_G", [32, NB], F32, st)
        yacc = P.sb("E_y", [128, 8, NB], F32, st)
        wup = [P.sb("E_wu%d" % i, [128, 8, 2048], BF16, st) for i in range(2)]
        wdn = [P.sb("E_wd%d" % i, [128, 8, 1024], BF16, st) for i in range(2)]
        bup = [P.sb("E_bu%d" % i, [128, 16], F32, st) for i in range(2)]
        bdn = P.sb("E_bdn", [32, 1024], F32, st)
        gb = [P.sb("E_gb%d" % i, [128, 512], F32, st) for i in range(2)]
        glu = [P.sb("E_gl%d" % i, [128, 512], BF16, st) for i in range(2)]
        sig = [P.sb("E_sg%d" % i, [128, 512], BF16, st) for i in range(2)]
        lin = [P.sb("E_ln%d" % i, [128, 512], BF16, st) for i in range(2)]
        avs = [P.sb("E_a%d" % i, [128, 8, 512], BF16, st) for i in range(2)]
        avi = [0]
        xc = [P.sb("E_xc%d" % i, [128, 512], F32, st) for i in range(2)]
        h23 = scr["hn2"].rearrange("(c p) t -> p c t", p=128)
        xr3 = scr["xres"].rearrange("(c p) t -> p c t", p=128)
        dma(P, "sp", bdn[:], din["b_dn"][l], writes=[bdn])
        ps_gl = [k.psum[0], k.psum[1]]
        ps_ln = [k.psum[2], k.psum[3]]
        ps_y = [k.psum[4], k.psum[5]]
        ps_b = k.psum[6]
        wi = 0
        for blk in MOE_BLOCKS:
            b0 = blk[0][0]
            nb = sum(n for _, n in blk)
            dma(P, "sp", h2[:, :, :nb], h23[:, :, b0:b0 + nb], writes=[h2])
            dma(P, "sp", Gt[:, :nb], scr["gfm"][:, b0:b0 + nb], writes=[Gt])
            for (t0, n) in blk:
                o = t0 - b0
                for oc in range(8):
                    P.op("pe", lambda e, oc=oc, o=o, n=n: e.matmul(ps_b[:, :n], lhsT=bdn[:, oc * 128:(oc + 1) * 128], rhs=Gt[:, o:o + n], start=True, stop=True),
                         reads=[bdn, Gt], writes=[ps_b])
                    P.op("act", lambda e, oc=oc, o=o, n=n: e.copy(yacc[:, oc, o:o + n], ps_b[:, :n]), reads=[ps_b], writes=[(yacc, oc, t0)])
            for ex in range(MOE_EXPERTS):
                wu, wd, bu = wup[wi % 2], wdn[wi % 2], bup[wi % 2]
                wi += 1
                wu3 = din["w_up"][l, ex].rearrange("(kc p) n -> p kc n", p=128)
                wd3 = din["w_dn"][l, ex].rearrange("(kc p) n -> p kc n", p=128)
                for q4 in range(4):
                    dma(P, "pool", wu[:, q4 * 2:q4 * 2 + 2, :], wu3[:, q4 * 2:q4 * 2 + 2, :], writes=[wu] if q4 == 0 else [(wu, q4)])
                for q2 in range(2):
                    dma(P, "pool", wd[:, q2 * 4:q2 * 4 + 4, :], wd3[:, q2 * 4:q2 * 4 + 4, :], writes=[wd] if q2 == 0 else [(wd, q2)])
                dma(P, "sp", bu[:], din["b_up_fm"][l, ex], writes=[bu])
                wukeys = [wu] + [(wu, q) for q in range(1, 4)]
                wdkeys = [wd, (wd, 1)]
                for ti, (t0, n) in enumerate(blk):
                    o = t0 - b0
                    g_ = gb[ti % 2]
                    av = avs[avi[0] % 2]
                    avi[0] += 1
                    dma(P, "sp", g_[:, :n], scr["gfm"][ex:ex + 1, t0:t0 + n].partition_broadcast(128), writes=[g_])
                    for fc in range(8):
                        i2 = fc % 2
                        pg, pl = ps_gl[i2], ps_ln[i2]
                        for kc in range(8):
                            P.op("pe", lambda e, pg=pg, kc=kc, fc=fc, o=o, n=n, wu=wu: e.matmul(pg[:, :n], lhsT=wu[:, kc, fc * 128:(fc + 1) * 128], rhs=h2[:, kc, o:o + n], start=(kc == 0), stop=(kc == 7)),
                                 reads=wukeys + [h2], writes=[pg], acc=True)
                        for kc in range(8):
                            P.op("pe", lambda e, pl=pl, kc=kc, fc=fc, o=o, n=n, wu=wu: e.matmul(pl[:, :n], lhsT=wu[:, kc, 1024 + fc * 128:1024 + (fc + 1) * 128], rhs=h2[:, kc, o:o + n], start=(kc == 0), stop=(kc == 7)),
                                 reads=wukeys + [h2], writes=[pl], acc=True)
                        gl, sg, ln = glu[i2], sig[i2], lin[i2]
                        P.op("dve", lambda e, gl=gl, pg=pg, fc=fc, n=n, bu=bu: e.tensor_scalar(out=gl[:, :n], in0=pg[:, :n], scalar1=bu[:, fc:fc + 1], scalar2=C["seven"][:, 0:1], op0=ALU.add, op1=ALU.min),
                             reads=[pg, bu], writes=[gl])
                        P.op("act", lambda e, gl=gl, sg=sg, n=n: e.activation(out=sg[:, :n], in_=gl[:, :n], func=AF.Sigmoid, scale=1.702), reads=[gl], writes=[sg])
                        P.op("dve", lambda e, ln=ln, pl=pl, fc=fc, n=n, bu=bu: e.tensor_scalar(out=ln[:, :n], in0=pl[:, :n], scalar1=bu[:, 8 + fc:9 + fc], scalar2=C["seven"][:, 0:1], op0=ALU.add, op1=ALU.min),
                             reads=[pl, bu], writes=[ln])
                        P.op("dve", lambda e, ln=ln, n=n: e.tensor_scalar(out=ln[:, :n], in0=ln[:, :n], scalar1=-7.0, scalar2=1.0, op0=ALU.max, op1=ALU.add), reads=[ln], writes=[ln])
                        P.op("dve", lambda e, gl=gl, sg=sg, n=n: e.tensor_tensor(out=sg[:, :n], in0=gl[:, :n], in1=sg[:, :n], op=ALU.mult), reads=[gl, sg], writes=[sg])
                        P.op("dve", lambda e, ln=ln, sg=sg, n=n: e.tensor_tensor(out=sg[:, :n], in0=sg[:, :n], in1=ln[:, :n], op=ALU.mult), reads=[ln, sg], writes=[sg])
                        P.op("dve", lambda e, sg=sg, g_=g_, fc=fc, n=n, av=av: e.tensor_tensor(out=av[:, fc, :n], in0=sg[:, :n], in1=g_[:, :n], op=ALU.mult), reads=[sg, g_], writes=[(av, fc)])
                    akeys = [(av, fc) for fc in range(8)]
                    for oc in range(8):
                        py = ps_y[oc % 2]
                        for fc in range(8):
                            P.op("pe", lambda e, py=py, fc=fc, oc=oc, n=n, wd=wd, av=av: e.matmul(py[:, :n], lhsT=wd[:, fc, oc * 128:(oc + 1) * 128], rhs=av[:, fc, :n], start=(fc == 0), stop=(fc == 7)),
                                 reads=wdkeys + akeys, writes=[py], acc=True)
                        P.op("dve", lambda e, py=py, oc=oc, o=o, n=n: e.tensor_tensor(out=yacc[:, oc, o:o + n], in0=py[:, :n], in1=yacc[:, oc, o:o + n], op=ALU.add),
                             reads=[py, (yacc, oc, t0)], writes=[(yacc, oc, t0)])
            for (t0, n) in blk:
                o = t0 - b0
                mc = 1 if t0 < CTX else 0
                for oc in range(8):
                    x_ = xc[oc % 2]
                    dma(P, "sp", x_[:, :n], xr3[:, oc, t0:t0 + n], writes=[x_])
                    P.op("dve", lambda e, x_=x_, oc=oc, o=o, n=n, mc=mc: e.scalar_tensor_tensor(out=x_[:, :n], in0=yacc[:, oc, o:o + n], scalar=k.modv[:, 40 + oc, mc:mc + 1], in1=x_[:, :n], op0=ALU.mult, op1=ALU.add),
                         reads=[x_, (yacc, oc, t0)], writes=[x_])
                    dma(P, "sp", xr3[:, oc, t0:t0 + n], x_[:, :n], reads=[x_], writes=["xres_o"])


def phaseFIN(k):
    P, C, din, scr = k.P, k.C, k.din, k.scr
    with ExitStack() as st:
        k.alloc_psum(st, 8)
        fg = P.sb("F_g", [128, 8], F32, st)
        dma(P, "sp", fg[:], din["fing_fm"][:, :], writes=[fg])
        xt = [P.sb("F_x%d" % i, [128, 8, 512], F32, st) for i in range(2)]
        sq = [P.sb("F_sq%d" % i, [128, 8, 512], BF16, st) for i in range(2)]
        rs = [P.sb("F_rs%d" % i, [128, 512], F32, st) for i in range(2)]
        ot = [P.sb("F_o%d" % i, [128, 1024], F32, st) for i in range(2)]
        xr3 = scr["xres"].rearrange("(c p) t -> p c t", p=128)
        oi = 0
        for ti, (t0, n) in enumerate(TILES):
            if t0 < CTX:
                continue
            x, q, r = xt[ti % 2], sq[ti % 2], rs[ti % 2]
            ps = k.psum[ti % 2]
            dma(P, "sp", x[:, :, :n], xr3[:, :, t0:t0 + n], writes=[x])
            P.op("act", lambda e, x=x, q=q, n=n: e.activation(out=q[:, :, :n], in_=x[:, :, :n], func=AF.Square), reads=[x], writes=[q])
            for c in range(8):
                P.op("pe", lambda e, ps=ps, q=q, c=c, n=n: e.matmul(ps[:, :n], lhsT=C["ones"][:], rhs=q[:, c, :n], start=(c == 0), stop=(c == 7)), reads=[q], writes=[ps], acc=True)
            P.op("act", lambda e, ps=ps, r=r, n=n: e.activation(out=r[:, :n], in_=ps[:, :n], func=AF.Sqrt, bias=C["eps"][:, 0:1], scale=1.0 / D), reads=[ps], writes=[r])
            P.op("dve", lambda e, r=r, n=n: e.reciprocal(r[:, :n], r[:, :n]), reads=[r], writes=[r])
            P.op("dve", lambda e, x=x, r=r, n=n: e.tensor_tensor(out=x[:, :, :n], in0=x[:, :, :n], in1=r[:, :n].unsqueeze(1).to_broadcast([128, 8, n]), op=ALU.mult), reads=[x, r], writes=[x])
            P.op("dve", lambda e, x=x, n=n: e.tensor_tensor(out=x[:, :, :n], in0=x[:, :, :n], in1=fg[:].unsqueeze(2).to_broadcast([128, 8, n]), op=ALU.mult), reads=[x, fg], writes=[x])
            for j in range(n // 128):
                o_ = ot[oi % 2]
                oi += 1
                for half in range(2):
                    pt = k.psum[2 + (oi * 2 + half) % 4]
                    for f4 in range(4):
                        fc = half * 4 + f4
                        P.op("pe", lambda e, pt=pt, f4=f4, fc=fc, j=j, x=x: e.transpose(pt[:, f4 * 128:(f4 + 1) * 128], x[:, fc, j * 128:(j + 1) * 128], C["ident_f"][:]), reads=[x], writes=[pt], acc=True)
                    if half == 0:
                        P.op("act", lambda e, pt=pt, o_=o_: e.copy(o_[:, 0:512], pt[:]), reads=[pt], writes=[(o_, 0)])
                    else:
                        P.op("dve", lambda e, pt=pt, o_=o_: e.tensor_copy(o_[:, 512:1024], pt[:]), reads=[pt], writes=[(o_, 1)])
                r0 = t0 - CTX + j * 128
                dma(P, "sp", k.out[r0:r0 + 128, :], o_[:], reads=[(o_, 0), (o_, 1)], writes=["out_o"])


_PROG_CACHE = {}


def kernel(**inputs):
    inp = {k_: np.asarray(v) for k_, v in inputs.items()}
    sh = host_prepare(inp)
    sh = {k_: np.ascontiguousarray(v, dtype=np.float32) for k_, v in sh.items()}
    if "nc" not in _PROG_CACHE:
        _PROG_CACHE["nc"] = build_program({k_: v.shape for k_, v in sh.items()})
    nc = _PROG_CACHE["nc"]
    in_maps = []
    for b in range(8):
        m = dict(sh)
        m.update(core_inputs(inp, b))
        in_maps.append(m)
    res = run_bass_kernel_spmd(nc, in_maps, core_ids=list(range(8)))
    out = np.stack([np.asarray(r["out"], dtype=np.float32) for r in res.results], axis=0)
    return out
```

```python
import numpy as np
from contextlib import ExitStack
import concourse.bass as bass
import concourse.mybir as mybir
from concourse.bass_utils import run_bass_kernel_spmd

F32 = mybir.dt.float32
BF16 = mybir.dt.bfloat16
AF = mybir.ActivationFunctionType
ALU = mybir.AluOpType
AX = mybir.AxisListType

SEM_LIMIT = 30000
N_DMA_SEMS = 12

L = 4
D = 1024
CTX = 256
SEQ = 4096
T = CTX + SEQ
NCH = T // 128
TP = T + 8
EPS = 1e-6
NE = 32
MLA_SCALE = 96 ** -0.5
NEG = -30000.0


def colof(t):
    return t + 2 if t < CTX else t + 6


TILES = [(0, 256)] + [(CTX + 512 * i, 512) for i in range(8)]


class Tok:
    __slots__ = ("sem", "val", "eng")

    def __init__(self, sem, val, eng):
        self.sem = sem
        self.val = val
        self.eng = eng


class Prog:
    ENGS = ("pe", "dve", "act", "pool", "sp")

    def __init__(self, nc):
        self.nc = nc
        self.es = ExitStack()
        self.ops = {e: [] for e in self.ENGS}
        self.cur_sem = {}
        self.cnt = {}
        self.nsem = 0
        for e in self.ENGS:
            self._new_sem(e)
        self.last_tok = {e: None for e in self.ENGS}
        self.known = {e: {} for e in self.ENGS}
        self.last_write = {}
        self.readers = {}
        self.dma_sems = {}
        self.dma_idx = {}
        self.dma_last = {}
        self.dma_cnt = {}
        for q in ("sp", "pool", "act"):
            self.dma_sems[q] = [self._sem("d%s%d" % (q, i)) for i in range(N_DMA_SEMS)]
            self.dma_idx[q] = 0
            self.dma_last[q] = [None] * N_DMA_SEMS
            self.dma_cnt[q] = [0] * N_DMA_SEMS
        self.n_ops = 0

    def _sem(self, name):
        self.nsem += 1
        return self.es.enter_context(self.nc.semaphore("s%d_%s" % (self.nsem, name)))

    def _new_sem(self, e):
        self.cur_sem[e] = self._sem(e)
        self.cnt[e] = 0

    def sb(self, name, shape, dtype, stack=None):
        self.n_tiles = getattr(self, "n_tiles", 0) + 1
        return (stack or self.es).enter_context(self.nc.sbuf_tensor("%s_u%d" % (name, self.n_tiles), list(shape), dtype))

    def ps(self, name, shape, dtype=F32, stack=None):
        return (stack or self.es).enter_context(self.nc.psum_tensor(name, list(shape), dtype))

    def _need(self, eng, tok, waits):
        if tok is None:
            return
        k = self.known[eng]
        if k.get(tok.sem, 0) >= tok.val:
            return
        k[tok.sem] = tok.val
        waits.append((tok.sem, tok.val))

    @staticmethod
    def _k(k):
        if isinstance(k, (str, int)):
            return k
        if isinstance(k, tuple):
            return tuple(Prog._k(x) for x in k)
        return k.name

    def op(self, eng, fn, reads=(), writes=(), acc=False, dma=False):
        reads = [self._k(k) for k in reads]
        writes = [self._k(k) for k in writes]
        waits = []
        for k in reads:
            self._need(eng, self.last_write.get(k), waits)
            kn = k[0] if isinstance(k, tuple) else k
            if isinstance(kn, str) and kn.startswith("ps"):
                for r in self.readers.get(k, ()):
                    if r.eng != eng:
                        self._need(eng, r, waits)
        for k in writes:
            w = self.last_write.get(k)
            if w is not None and not (acc and w.eng == "pe" and eng == "pe"):
                self._need(eng, w, waits)
            for r in self.readers.get(k, ()):
                self._need(eng, r, waits)
        if dma:
            q = eng
            i = self.dma_idx[q]
            self.dma_idx[q] = (i + 1) % N_DMA_SEMS
            self._need(eng, self.dma_last[q][i], waits)
            self.dma_cnt[q][i] += 16
            tok = Tok(self.dma_sems[q][i], self.dma_cnt[q][i], "dma")
            self.dma_last[q][i] = tok
            inc = 16
        else:
            if self.cnt[eng] >= SEM_LIMIT:
                self._new_sem(eng)
            self.cnt[eng] += 1
            tok = Tok(self.cur_sem[eng], self.cnt[eng], eng)
            self.last_tok[eng] = tok
            inc = 1
        for k in writes:
            self.last_write[k] = tok
            self.readers[k] = []
        for k in reads:
            lst = self.readers.setdefault(k, [])
            if tok.eng != "dma":
                lst[:] = [r for r in lst if r.eng != tok.eng]
            lst.append(tok)
        self.ops[eng].append((waits, fn, tok.sem, inc))
        self.n_ops += 1
        return tok

    def barrier(self):
        toks = [self.last_tok[e] for e in self.ENGS if self.last_tok[e] is not None]
        for q in self.dma_last:
            toks += [t for t in self.dma_last[q] if t is not None]
        for e in self.ENGS:
            waits = []
            for t in toks:
                self._need(e, t, waits)
            if waits:
                self.ops[e].append((waits, None, None, 0))
        self.last_write = {}
        self.readers = {}

    def emit(self):
        self.barrier()
        nc = self.nc
        ops = self.ops
        with nc.Block() as block:
            def run(eng_obj, lst):
                for waits, fn, sem, inc in lst:
                    for s, v in waits:
                        eng_obj.wait_ge(s, v)
                    if fn is not None:
                        fn(eng_obj).then_inc(sem, inc)

            @block.tensor
            def _(e):
                run(e, ops["pe"])

            @block.vector
            def _(e):
                run(e, ops["dve"])

            @block.scalar
            def _(e):
                run(e, ops["act"])

            @block.gpsimd
            def _(e):
                run(e, ops["pool"])

            @block.sync
            def _(e):
                run(e, ops["sp"])

    def close(self):
        self.es.close()


class Rot:
    def __init__(self, items):
        self.items = items
        self.i = 0

    def next(self):
        x = self.items[self.i % len(self.items)]
        self.i += 1
        return x


IN_SIZES = (384, 256, 32, 512, 512, 512, 16, 1024, 2048, 32, 3072)
IN_OFF = np.concatenate([[0], np.cumsum(IN_SIZES)]).astype(int)


def fm_vec(v, nch):
    return np.ascontiguousarray(np.swapaxes(v.reshape(v.shape[:-1] + (nch, 128)), -1, -2))


def host_prepare(inp):
    f32 = np.float32
    w_in = inp["w_in"]
    sl = lambda i: w_in[:, :, IN_OFF[i]:IN_OFF[i + 1]]
    sh = {}
    sh["w_ada"] = inp["w_ada"]
    sh["b_ada_fm"] = fm_vec(inp["b_ada"], 48)
    sh["n1g_fm"] = fm_vec(inp["norm1_g"], 8)
    sh["n2g_fm"] = fm_vec(inp["norm2_g"], 8)
    sh["fing_fm"] = fm_vec(inp["final_g"], 8)
    sh["w_q"] = np.ascontiguousarray(sl(0))
    sh["w_kv"] = np.ascontiguousarray(sl(1))
    kr = sl(2)
    w_kr = np.zeros((L, D, 256), f32)
    w_kr[:, :, 64:96] = kr
    w_kr[:, :, 128 + 64:128 + 80] = kr[:, :, 16:32]
    w_kr[:, :, 128 + 80:128 + 96] = kr[:, :, 0:16]
    sh["w_kr"] = w_kr
    sh["w_mqk"] = np.ascontiguousarray(sl(3))
    sh["w_mv"] = np.ascontiguousarray(sl(4))
    sh["w_mo"] = np.ascontiguousarray(sl(5))
    sh["w_mif"] = np.ascontiguousarray(sl(6))
    sh["w_sz"] = np.ascontiguousarray(sl(7))
    xbc = sl(8)
    sh["w_sx"] = np.ascontiguousarray(xbc[:, :, 0:1024])
    sh["w_sB"] = np.ascontiguousarray(xbc[:, :, 1024:1536])
    sh["w_sC"] = np.ascontiguousarray(xbc[:, :, 1536:2048])
    sh["w_sdt"] = np.ascontiguousarray(sl(9))
    sh["w_g"] = np.ascontiguousarray(sl(10))
    uq = inp["mla_w_uq"].reshape(L, 384, 8, 96)
    w_uq = np.zeros((L, 384, 2, 8, 128), f32)
    w_uq[:, :, 0, :, 0:96] = uq
    w_uq[:, :, 1, :, 64:80] = uq[..., 80:96]
    w_uq[:, :, 1, :, 80:96] = uq[..., 64:80]
    sh["w_uq"] = w_uq.reshape(L, 384, 2048)
    ukv = inp["mla_w_ukv"].reshape(L, 256, 8, 128)
    sh["w_uk"] = np.ascontiguousarray(ukv[..., 0:64]).reshape(L, 256, 512)
    sh["w_uv"] = np.ascontiguousarray(ukv[..., 64:128]).reshape(L, 256, 512)
    sh["qn_fm"] = fm_vec(inp["mla_qnorm_g"], 3)
    sh["kvn_fm"] = fm_vec(inp["mla_kvnorm_g"], 2)
    sh["ml_cw"] = inp["ml_conv_w"]
    sh["ml_cb_fm"] = fm_vec(inp["ml_conv_b"], 4)
    sh["ml_gb"] = inp["ml_gate_b"].reshape(L, 1, 16)
    sh["ml_ng"] = inp["ml_norm_g"].reshape(L, 1, 512)
    scw = inp["ssd_conv_w"]
    scb = inp["ssd_conv_b"]
    sh["s_cw"] = scw
    sh["s_cb"] = scb.reshape(L, 1, 2048)
    sh["s_cbB_fm"] = fm_vec(scb[:, 1024:1536], 4)
    sh["s_cbC_fm"] = fm_vec(scb[:, 1536:2048], 4)
    sh["s_dtb"] = inp["ssd_dt_bias"].reshape(L, 1, 32)
    sh["s_alog"] = inp["ssd_a_log"].reshape(L, 1, 32)
    sh["s_d"] = inp["ssd_d"].reshape(L, 1, 16)
    sh["s_ng"] = inp["ssd_norm_g"].reshape(L, 1, 1024)
    sh["w_bra"] = inp["w_br_mla"]
    sh["w_brm"] = inp["w_br_ml"]
    sh["w_brs"] = inp["w_br_ssd"]
    sh["w_out"] = inp["w_out"]
    sh["w_rt"] = inp["w_router"]
    sh["b_rt"] = inp["b_router"].reshape(L, 1, 32)
    sh["w_up"] = inp["w_up"]
    sh["w_dn"] = inp["w_down"]
    sh["b_up_fm"] = fm_vec(inp["b_up"], 16)
    sh["b_dn"] = inp["b_down"]
    r = np.arange(128)
    sh["c_ident"] = np.eye(128, dtype=f32)
    sh["c_ones"] = np.ones((128, 128), f32)
    sh["c_tri_le"] = (r[:, None] <= r[None, :]).astype(f32)
    sh["c_tri_ge"] = (r[:, None] >= r[None, :]).astype(f32)
    sh["c_tri_gt"] = (r[:, None] > r[None, :]).astype(f32)
    sh["c_tri_lt"] = (r[:, None] < r[None, :]).astype(f32)
    sel = np.zeros((32, 32, 128), f32)
    for e in range(32):
        sel[e, e, :] = 1.0
    sh["c_sel"] = sel
    rows = SEQ // 64
    row = np.repeat(np.arange(rows), 64)
    col = np.tile(np.arange(64), rows)
    inv = (10000.0 ** (-np.arange(8, dtype=np.float32) / 8)).astype(np.float32)
    ang = np.concatenate([row[:, None] * inv, col[:, None] * inv], axis=-1).astype(np.float32)
    cs = np.cos(ang).T
    sn = np.sin(ang).T
    rc = np.ones((32, T), f32)
    rs = np.zeros((32, T), f32)
    rc[0:16, CTX:] = cs
    rc[16:32, CTX:] = cs
    rs[0:16, CTX:] = -sn
    rs[16:32, CTX:] = sn
    rope = np.zeros((128, 2, T), f32)
    rope[64:96, 0] = rc
    rope[64:96, 1] = rs
    sh["c_rope"] = rope
    return sh


def core_inputs(inp, b):
    xin = np.concatenate([inp["ctx"][b], inp["x"][b]], axis=0).astype(np.float32)
    cvec = np.stack([fm_vec(inp["c"][b], 8), fm_vec(inp["c_ctx"], 8)], axis=-1).astype(np.float32)
    return {"xin": np.ascontiguousarray(xin), "cvec": np.ascontiguousarray(cvec)}


SHARED_SPECS = None


class K:
    pass


def dma(P, q, out, in_, reads=(), writes=()):
    return P.op(q, lambda e: e.dma_start(out=out, in_=in_), reads=reads, writes=writes, dma=True)


def build_program(shared_shapes, n_layers=L, stop_after=None, debug=False, only=None, scr_inputs=()):
    nc = bass.Bass("TRN2", target_bir_lowering=False)
    P = Prog(nc)
    k = K()
    k.nc, k.P = nc, P
    k.debug = debug
    k.din = {}
    for name, shp in shared_shapes.items():
        k.din[name] = nc.dram_tensor(name, list(shp), F32, kind="ExternalInput").ap()
    k.din["xin"] = nc.dram_tensor("xin", [T, D], F32, kind="ExternalInput").ap()
    k.din["cvec"] = nc.dram_tensor("cvec", [128, 8, 2], F32, kind="ExternalInput").ap()
    k.out = nc.dram_tensor("out", [SEQ, D], F32, kind="ExternalOutput").ap()
    skind = "ExternalOutput" if debug else "Internal"
    k.scr = {}

    def scr(name, shape, dt):
        kd = "ExternalInput" if name in scr_inputs else skind
        k.scr[name] = nc.dram_tensor("scr_" + name, list(shape), dt, kind=kd).ap()

    scr("xres", [D, T], F32)
    scr("uq", [384, T], BF16)
    scr("ukv", [256, T], BF16)
    scr("kr", [256, T], BF16)
    scr("mqk", [512, T], BF16)
    scr("mv", [T, 512], BF16)
    scr("mo", [T, 512], BF16)
    scr("mif", [T, 16], F32)
    scr("sz", [T, 1024], BF16)
    scr("sx", [T, 1024], BF16)
    scr("sBf", [512, T], BF16)
    scr("sBt", [T, 512], BF16)
    scr("sCf", [512, T], BF16)
    scr("sdt", [T, 32], F32)
    scr("gg", [3072, T], BF16)
    scr("att", [512, T], BF16)
    scr("mout", [512, T], BF16)
    scr("sout", [1024, T], BF16)
    scr("hacc", [T, 512], F32)
    scr("yacc", [T, 1024], F32)
    scr("hn2", [D, T], BF16)
    scr("gfm", [32, T], F32)

    C = {}
    k.C = C

    def cload(name, src, shape, dt=F32, q="sp"):
        t = P.sb("k_" + name, shape, dt)
        dma(P, q, t[:], src, writes=[t])
        C[name] = t
        return t

    cload("ident_f", k.din["c_ident"][:, :], [128, 128])
    cload("ones_f", k.din["c_ones"][:, :], [128, 128])
    cload("ident", k.din["c_ident"][:, :], [128, 128], BF16, q="pool")
    cload("ones", k.din["c_ones"][:, :], [128, 128], BF16, q="pool")
    cload("tri_le", k.din["c_tri_le"][:, :], [128, 128])
    cload("tri_ge", k.din["c_tri_ge"][:, :], [128, 128])
    cload("tri_gt", k.din["c_tri_gt"][:, :], [128, 128])
    cload("tri_lt", k.din["c_tri_lt"][:, :], [128, 128])
    for nm in ("tri_le", "tri_ge", "tri_gt", "tri_lt"):
        cload(nm + "_b", k.din["c_" + nm][:, :], [128, 128], BF16, q="pool")
    eps = P.sb("k_eps", [128, 1], F32)
    P.op("dve", lambda e: e.memset(eps[:], EPS), writes=[eps])
    C["eps"] = eps
    one1 = P.sb("k_one1", [128, 1], F32)
    P.op("dve", lambda e: e.memset(one1[:], 1.0), writes=[one1])
    C["one1"] = one1
    neg1 = P.sb("k_neg1", [128, 1], F32)
    P.op("dve", lambda e: e.memset(neg1[:], -1.0), writes=[neg1])
    C["neg1"] = neg1
    seven = P.sb("k_seven", [128, 1], F32)
    P.op("dve", lambda e: e.memset(seven[:], 7.0), writes=[seven])
    C["seven"] = seven
    big = P.sb("k_big", [128, 1], F32)
    P.op("dve", lambda e: e.memset(big[:], 1e9), writes=[big])
    C["big"] = big
    zero1 = P.sb("k_zero1", [128, 1], F32)
    P.op("dve", lambda e: e.memset(zero1[:], 0.0), writes=[zero1])
    C["zero1"] = zero1
    k.psn = [0]

    def alloc_psum(st, nf, nb=0):
        k.psn[0] += 1
        k.psum = [P.ps("psum%d_%d" % (k.psn[0], i), [128, 512], F32, st) for i in range(nf)]
        k.psbf = [P.ps("psbf%d_%d" % (k.psn[0], i), [128, 1024], BF16, st) for i in range(nb)]
    k.alloc_psum = alloc_psum
    k.modv = P.sb("g_modv", [128, 48, 2], F32)
    k.a1 = P.sb("g_a1", [128, 8, 2], F32)
    k.a2 = P.sb("g_a2", [128, 8, 2], F32)
    cv = P.sb("g_cv", [128, 8, 2], F32)
    k.csil = P.sb("g_csil", [128, 8, 2], F32)
    dma(P, "sp", cv[:], k.din["cvec"][:, :, :], writes=[cv])
    P.op("act", lambda e: e.activation(out=k.csil[:], in_=cv[:], func=AF.Silu), reads=[cv], writes=[k.csil])
    P.barrier()

    if only is not None:
        only(k, 0)
        return finish(k)
    phase0(k)
    P.barrier()
    if stop_after == "p0":
        return finish(k)
    for l in range(n_layers):
        phaseA(k, l)
        P.barrier()
        if stop_after == ("A", l):
            return finish(k)
        phaseMLA(k, l, l == n_layers - 1)
        P.barrier()
        if stop_after == ("MLA", l):
            return finish(k)
        phaseML(k, l)
        P.barrier()
        if stop_after == ("ML", l):
            return finish(k)
        phaseSSD(k, l)
        P.barrier()
        if stop_after == ("SSD", l):
            return finish(k)
        phaseOUT(k, l)
        P.barrier()
        if stop_after == ("OUT", l):
            return finish(k)
        phaseMOE(k, l)
        P.barrier()
        if stop_after == ("MOE", l):
            return finish(k)
    phaseFIN(k)
    return finish(k)


def finish(k):
    k.P.emit()
    k.P.close()
    return k.nc


def phase0(k):
    P, C = k.P, k.C
    with ExitStack() as st:
        k.alloc_psum(st, 8)
        xt = [P.sb("p0_x%d" % i, [128, D], F32, st) for i in range(2)]
        ot = [P.sb("p0_o%d" % i, [128, 8, 128], F32, st) for i in range(2)]
        xr3 = k.scr["xres"].rearrange("(c p) t -> p c t", p=128)
        for ch in range(NCH):
            x = xt[ch % 2]
            o = ot[ch % 2]
            dma(P, "sp", x[:], k.din["xin"][ch * 128:(ch + 1) * 128, :], writes=[x])
            for half in range(2):
                ps = k.psum[(ch * 2 + half) % 4]
                for j in range(4):
                    fc = half * 4 + j
                    P.op("pe", lambda e, ps=ps, j=j, fc=fc, x=x: e.transpose(ps[:, j * 128:(j + 1) * 128], x[:, fc * 128:(fc + 1) * 128], C["ident_f"][:]),
                         reads=[x], writes=[ps])
                eng = "act" if half == 0 else "dve"
                if eng == "act":
                    P.op("act", lambda e, ps=ps, o=o, half=half: e.copy(o[:, half * 4:half * 4 + 4, :], ps[:].rearrange("p (a b) -> p a b", a=4)),
                         reads=[ps], writes=[(o, half)])
                else:
                    P.op("dve", lambda e, ps=ps, o=o, half=half: e.tensor_copy(o[:, half * 4:half * 4 + 4, :], ps[:].rearrange("p (a b) -> p a b", a=4)),
                         reads=[ps], writes=[(o, half)])
            dma(P, "sp", xr3[:, :, ch * 128:(ch + 1) * 128], o[:], reads=[(o, 0), (o, 1)], writes=["xres"])


def compute_mod(k, l, st):
    P, C, din = k.P, k.C, k.din
    wa = [P.sb("md_wa%d" % i, [128, 8, 512], F32, st) for i in range(2)]
    bada = P.sb("md_b", [128, 48], F32, st)
    n1g = P.sb("md_n1", [128, 8], F32, st)
    n2g = P.sb("md_n2", [128, 8], F32, st)
    dma(P, "sp", bada[:], din["b_ada_fm"][l], writes=[bada])
    dma(P, "sp", n1g[:], din["n1g_fm"][l], writes=[n1g])
    dma(P, "sp", n2g[:], din["n2g_fm"][l], writes=[n2g])
    psm = k.psum[7]
    w3 = din["w_ada"][l].rearrange("(kc p) n -> p kc n", p=128)
    for blk in range(12):
        w = wa[blk % 2]
        dma(P, "sp", w[:], w3[:, :, blk * 512:(blk + 1) * 512], writes=[w])
        for o4 in range(4):
            oc = blk * 4 + o4
            for kc in range(8):
                P.op("pe", lambda e, w=w, o4=o4, oc=oc, kc=kc: e.matmul(psm[:, oc * 2:oc * 2 + 2], lhsT=w[:, kc, o4 * 128:(o4 + 1) * 128],
                                                                    rhs=k.csil[:, kc, :], start=(kc == 0), stop=(kc == 7)),
                     reads=[w, k.csil], writes=[psm], acc=True)
    modv, a1, a2 = k.modv, k.a1, k.a2
    P.op("dve", lambda e: e.tensor_tensor(out=modv[:], in0=psm[:, 0:96].rearrange("p (o c) -> p o c", c=2),
                                          in1=bada[:].unsqueeze(2).to_broadcast([128, 48, 2]), op=ALU.add),
         reads=[psm, bada], writes=[modv])
    for (a, ng, base) in ((a1, n1g, 8), (a2, n2g, 32)):
        P.op("dve", lambda e, a=a, base=base: e.tensor_scalar(out=a[:], in0=modv[:, base:base + 8, :], scalar1=1.0, scalar2=None, op0=ALU.add),
             reads=[modv], writes=[a])
        P.op("dve", lambda e, a=a, ng=ng: e.tensor_tensor(out=a[:], in0=a[:], in1=ng[:].unsqueeze(2).to_broadcast([128, 8, 2]), op=ALU.mult),
             reads=[a, ng], writes=[a])


def modulate_tiles(k, st, src3, a, bbase, consume):
    P, C = k.P, k.C
    xt = [P.sb("mo_x%d" % i, [128, 8, 512], F32, st) for i in range(2)]
    sq = [P.sb("mo_sq%d" % i, [128, 8, 512], BF16, st) for i in range(2)]
    rs = [P.sb("mo_rs%d" % i, [128, 512], F32, st) for i in range(2)]
    for ti, (t0, n) in enumerate(TILES):
        mc = 1 if t0 < CTX else 0
        x, q, r = xt[ti % 2], sq[ti % 2], rs[ti % 2]
        ps = k.psum[4 + ti % 2]
        dma(P, "sp", x[:, :, :n], src3[:, :, t0:t0 + n], writes=[x])
        P.op("act", lambda e, x=x, q=q, n=n: e.activation(out=q[:, :, :n], in_=x[:, :, :n], func=AF.Square), reads=[x], writes=[q])
        for c in range(8):
            P.op("pe", lambda e, ps=ps, q=q, c=c, n=n: e.matmul(ps[:, :n], lhsT=C["ones"][:], rhs=q[:, c, :n], start=(c == 0), stop=(c == 7)),
                 reads=[q], writes=[ps], acc=True)
        P.op("act", lambda e, ps=ps, r=r, n=n: e.activation(out=r[:, :n], in_=ps[:, :n], func=AF.Sqrt, bias=C["eps"][:, 0:1], scale=1.0 / D),
             reads=[ps], writes=[r])
        P.op("dve", lambda e, r=r, n=n: e.reciprocal(r[:, :n], r[:, :n]), reads=[r], writes=[r])
        P.op("dve", lambda e, x=x, r=r, n=n: e.tensor_tensor(out=x[:, :, :n], in0=x[:, :, :n], in1=r[:, :n].unsqueeze(1).to_broadcast([128, 8, n]), op=ALU.mult),
             reads=[x, r], writes=[x])
        consume(ti, t0, n, mc, x)


def phaseA(k, l):
    P, C, din, scr = k.P, k.C, k.din, k.scr
    with ExitStack() as st:
        k.alloc_psum(st, 8)
        hn = P.sb("A_hn", [128, 8, TP], BF16, st)
        for (a, b) in ((0, 2), (258, 262), (TP - 2, TP)):
            P.op("pool", lambda e, a=a, b=b: e.memset(hn[:, :, a:b], 0.0), writes=[("hnpad", a)])
        xr3 = scr["xres"].rearrange("(c p) t -> p c t", p=128)

        def consume(ti, t0, n, mc, x):
            c0 = colof(t0)
            for c in range(8):
                P.op("act", lambda e, c=c, x=x, n=n, c0=c0, mc=mc: e.activation(out=hn[:, c, c0:c0 + n], in_=x[:, c, :n], func=AF.Identity,
                                                                           bias=k.modv[:, c, mc:mc + 1], scale=k.a1[:, c, mc:mc + 1]),
                     reads=[x, k.modv, k.a1], writes=[("hn", ti, c)])

        with ExitStack() as st2:
            compute_mod(k, l, st2)
            modulate_tiles(k, st2, xr3, k.a1, 0, consume)
            P.barrier()

        wraw = [P.sb("A_wraw%d" % i, [128, 8, 512], BF16, st) for i in range(2)]
        wexp = [P.sb("A_wexp%d" % i, [128, 5, 8, 512], BF16, st) for i in range(1)] * 2
        cwb = [P.sb("A_cwb%d" % i, [128, 5, 512], F32, st) for i in range(1)] * 2
        bbc = [P.sb("A_bbc%d" % i, [128, 512], F32, st) for i in range(2)]
        bfm = [P.sb("A_bfm%d" % i, [128, 4], F32, st) for i in range(2)]
        stg_b = Rot([P.sb("A_sb%d" % i, [128, 512], BF16, st) for i in range(4)])
        stg_f = Rot([P.sb("A_sf%d" % i, [128, 512], F32, st) for i in range(4)])
        tmpf = Rot([P.sb("A_tf%d" % i, [128, 512], F32, st) for i in range(3)])
        psr = Rot(k.psum[0:4])
        gi = [0]

        def load_w(wname, c0, Cn, conv):
            i = gi[0] % 2
            gi[0] += 1
            wr = wraw[i]
            w3 = din[wname][l].rearrange("(kc p) n -> p kc n", p=128)
            dma(P, "pool", wr[:, :, :Cn], w3[:, :, c0:c0 + Cn], writes=[wr])
            if conv is None:
                return (lambda j, kc: wr[:, kc, :Cn]), [wr], 1
            cwname, cc0 = conv
            cw, we = cwb[i], wexp[i]
            for j in range(5):
                dma(P, "sp", cw[:, j, :Cn], din[cwname][l, j:j + 1, cc0:cc0 + Cn].partition_broadcast(128), writes=[(cw, j)])
            for j in range(5):
                P.op("dve", lambda e, j=j: e.tensor_tensor(out=we[:, j, :, :Cn], in0=wr[:, :, :Cn],
                                                           in1=cw[:, j, :Cn].unsqueeze(1).to_broadcast([128, 8, Cn]), op=ALU.mult),
                     reads=[wr, (cw, j)], writes=[(we, j)])
            return (lambda j, kc: we[:, j, kc, :Cn]), [(we, j) for j in range(5)], 5

        def proj_fm(wname, wc0, Cn, oname, orow0, func, bias_name=None, conv=None, post=None, odt=BF16):
            wv, wkeys, nsh = load_w(wname, wc0, Cn, conv)
            noc = Cn // 128
            bt = None
            if bias_name is not None:
                bt = bfm[gi[0] % 2]
                dma(P, "sp", bt[:, :noc], din[bias_name][l], writes=[bt])
            for ti, (t0, n) in enumerate(TILES):
                c0 = colof(t0)
                for oc in range(noc):
                    ps = psr.next()
                    cnt = 0
                    for j in range(nsh):
                        sh = (j - 2) if nsh == 5 else 0
                        for kc in range(8):
                            P.op("pe", lambda e, ps=ps, j=j, kc=kc, oc=oc, sh=sh, c0=c0, n=n, cnt=cnt: e.matmul(
                                ps[:, :n], lhsT=wv(j, kc)[:, oc * 128:(oc + 1) * 128], rhs=hn[:, kc, c0 + sh:c0 + sh + n],
                                start=(cnt == 0), stop=(cnt == nsh * 8 - 1)), reads=wkeys, writes=[ps], acc=True)
                            cnt += 1
                    sg = stg_b.next() if odt == BF16 else stg_f.next()
                    bias_ap = bt[:, oc:oc + 1] if bt is not None else C["zero1"][:, 0:1]
                    P.op("act", lambda e, sg=sg, ps=ps, n=n, bias_ap=bias_ap: e.activation(out=sg[:, :n], in_=ps[:, :n], func=func, bias=bias_ap, scale=1.0),
                         reads=[ps] + ([bt] if bt is not None else []), writes=[sg])
                    if post is not None and post(oc) is not None:
                        sc = post(oc)
                        P.op("pool", lambda e, sg=sg, n=n, sc=sc: e.tensor_scalar(out=sg[:, :n], in0=sg[:, :n], scalar1=sc, scalar2=None, op0=ALU.mult),
                             reads=[sg], writes=[sg])
                    dma(P, "sp", scr[oname][orow0 + oc * 128:orow0 + (oc + 1) * 128, t0:t0 + n], sg[:, :n], reads=[sg], writes=[(oname, "o")])

        def proj_tm(wname, wc0, Cn, oname, ocol0, func, bias=None, conv=None, odt=BF16, softplus=False):
            wv, wkeys, nsh = load_w(wname, wc0, Cn, conv)
            bt = None
            if bias is not None:
                bname, bc0 = bias
                bt = bbc[gi[0] % 2]
                dma(P, "sp", bt[:, :Cn], din[bname][l, 0:1, bc0:bc0 + Cn].partition_broadcast(128), writes=[bt])
            for ch in range(NCH):
                c0 = colof(ch * 128)
                ps = psr.next()
                cnt = 0
                for j in range(nsh):
                    sh = (j - 2) if nsh == 5 else 0
                    for kc in range(8):
                        P.op("pe", lambda e, ps=ps, j=j, kc=kc, sh=sh, c0=c0, cnt=cnt: e.matmul(
                            ps[:, :Cn], lhsT=hn[:, kc, c0 + sh:c0 + sh + 128], rhs=wv(j, kc),
                            start=(cnt == 0), stop=(cnt == nsh * 8 - 1)), reads=wkeys, writes=[ps], acc=True)
                        cnt += 1
                sg = stg_b.next() if odt == BF16 else stg_f.next()
                src, skey = ps, ps
                if bt is not None:
                    tf = tmpf.next() if (func is not None or softplus) else sg
                    P.op("dve", lambda e, tf=tf, ps=ps: e.tensor_tensor(out=tf[:, :Cn], in0=ps[:, :Cn], in1=bt[:, :Cn], op=ALU.add),
                         reads=[ps, bt], writes=[tf])
                    src, skey = tf, tf
                if softplus:
                    tf2 = tmpf.next()
                    P.op("act", lambda e, tf2=tf2, src=src: e.activation(out=tf2[:, :Cn], in_=src[:, :Cn], func=AF.Exp), reads=[skey], writes=[tf2])
                    P.op("act", lambda e, tf2=tf2, sg=sg: e.activation(out=sg[:, :Cn], in_=tf2[:, :Cn], func=AF.Ln, bias=C["one1"][:, 0:1], scale=1.0),
                         reads=[tf2], writes=[sg])
                elif func is not None:
                    P.op("act", lambda e, sg=sg, src=src: e.activation(out=sg[:, :Cn], in_=src[:, :Cn], func=func), reads=[skey], writes=[sg])
                dma(P, "sp", scr[oname][ch * 128:(ch + 1) * 128, ocol0:ocol0 + Cn], sg[:, :Cn], reads=[sg], writes=[(oname, "o")])

        ID, SILU, SIG = AF.Identity, AF.Silu, AF.Sigmoid
        proj_fm("w_q", 0, 384, "uq", 0, ID)
        proj_fm("w_kv", 0, 256, "ukv", 0, ID)
        proj_fm("w_kr", 0, 256, "kr", 0, ID)
        proj_fm("w_mqk", 0, 512, "mqk", 0, SILU, bias_name="ml_cb_fm", conv=("ml_cw", 0), post=lambda oc: 0.125 if oc >= 2 else None)
        proj_tm("w_mv", 0, 512, "mv", 0, ID)
        proj_tm("w_mo", 0, 512, "mo", 0, SIG)
        proj_tm("w_mif", 0, 16, "mif", 0, None, bias=("ml_gb", 0), odt=F32)
        proj_fm("w_sB", 0, 512, "sBf", 0, SILU, bias_name="s_cbB_fm", conv=("s_cw", 1024))
        proj_fm("w_sC", 0, 512, "sCf", 0, SILU, bias_name="s_cbC_fm", conv=("s_cw", 1536))
        proj_tm("w_sB", 0, 512, "sBt", 0, SILU, bias=("s_cb", 1024), conv=("s_cw", 1024))
        for h in range(2):
            proj_tm("w_sz", h * 512, 512, "sz", h * 512, SILU)
            proj_tm("w_sx", h * 512, 512, "sx", h * 512, SILU, bias=("s_cb", h * 512), conv=("s_cw", h * 512))
        proj_tm("w_sdt", 0, 32, "sdt", 0, None, bias=("s_dtb", 0), odt=F32, softplus=True)
        for g in range(6):
            proj_fm("w_g", g * 512, 512, "gg", g * 512, SIG)


def rms_rows(k, st, name, src, nch, dst, tag):
    P, C = k.P, k.C
    nf = nch * 128
    s3 = src.rearrange("(c p) t -> p c t", p=128)
    xt = [P.sb("%s_x%d" % (tag, i), [128, nch, 512], BF16, st) for i in range(2)]
    sq = [P.sb("%s_q%d" % (tag, i), [128, nch, 512], BF16, st) for i in range(2)]
    rs = [P.sb("%s_r%d" % (tag, i), [128, 512], F32, st) for i in range(2)]
    for ti, (t0, n) in enumerate(TILES):
        x, q, r = xt[ti % 2], sq[ti % 2], rs[ti % 2]
        ps = k.psum[5 + ti % 2]
        dma(P, "sp", x[:, :, :n], s3[:, :, t0:t0 + n], writes=[x])
        P.op("act", lambda e, x=x, q=q, n=n: e.activation(out=q[:, :, :n], in_=x[:, :, :n], func=AF.Square), reads=[x], writes=[q])
        for c in range(nch):
            P.op("pe", lambda e, ps=ps, q=q, c=c, n=n: e.matmul(ps[:, :n], lhsT=C["ones"][:], rhs=q[:, c, :n], start=(c == 0), stop=(c == nch - 1)),
                 reads=[q], writes=[ps], acc=True)
        P.op("act", lambda e, ps=ps, r=r, n=n: e.activation(out=r[:, :n], in_=ps[:, :n], func=AF.Sqrt, bias=C["eps"][:, 0:1], scale=1.0 / nf),
             reads=[ps], writes=[r])
        P.op("dve", lambda e, r=r, n=n: e.reciprocal(r[:, :n], r[:, :n]), reads=[r], writes=[r])
        P.op("dve", lambda e, x=x, r=r, n=n, t0=t0: e.tensor_tensor(out=dst[:, :, t0:t0 + n], in0=x[:, :, :n],
                                                                   in1=r[:, :n].unsqueeze(1).to_broadcast([128, nch, n]), op=ALU.mult),
             reads=[x, r], writes=[(dst, ti)])


def phaseMLA(k, l, last):
    P, C, din, scr = k.P, k.C, k.din, k.scr
    with ExitStack() as st:
        k.alloc_psum(st, 8)
        uqn = P.sb("M_uqn", [128, 3, T], BF16, st)
        ukvn = P.sb("M_ukvn", [128, 2, T], BF16, st)
        Kh = P.sb("M_Kh", [128, T], BF16, st)
        Qh = P.sb("M_Qh", [128, T], BF16, st)
        Vh = P.sb("M_Vh", [128, NCH, 128], BF16, st)
        wuq = P.sb("M_wuq", [128, 3, 2048], BF16, st)
        wuk = P.sb("M_wuk", [128, 2, 512], BF16, st)
        wuv = P.sb("M_wuv", [128, 2, 512], BF16, st)
        qn = P.sb("M_qn", [128, 3], F32, st)
        kvn = P.sb("M_kvn", [128, 2], F32, st)
        kmax = P.sb("M_kmax", [128, 1], F32, st)
        kmt = P.sb("M_kmt", [128, 1], F32, st)
        with ExitStack() as st2:
            rms_rows(k, st2, "uq", scr["uq"], 3, uqn, "Mq")
            rms_rows(k, st2, "ukv", scr["ukv"], 2, ukvn, "Mk")
            P.barrier()
        P.op("pool", lambda e: e.memset(Kh[96:128, :], 0.0), writes=["Kc0"])
        P.op("pool", lambda e: e.memset(Kh[96:97, :], 1.0), reads=["Kc0"], writes=["Kc1"])
        P.op("pool", lambda e: e.memset(Qh[96:128, :], 0.0), writes=["Qc0"])
        P.op("pool", lambda e: e.memset(Vh[:, :, 64:128], 1.0), writes=["Vc0"])
        dma(P, "sp", qn[:], din["qn_fm"][l], writes=[qn])
        dma(P, "sp", kvn[:], din["kvn_fm"][l], writes=[kvn])
        dma(P, "pool", wuq[:], din["w_uq"][l].rearrange("(kc p) n -> p kc n", p=128), writes=[wuq])
        dma(P, "pool", wuk[:], din["w_uk"][l].rearrange("(kc p) n -> p kc n", p=128), writes=[wuk])
        dma(P, "pool", wuv[:], din["w_uv"][l].rearrange("(kc p) n -> p kc n", p=128), writes=[wuv])
        for kc in range(3):
            P.op("dve", lambda e, kc=kc: e.tensor_scalar(out=wuq[:, kc, :], in0=wuq[:, kc, :], scalar1=qn[:, kc:kc + 1], scalar2=None, op0=ALU.mult),
                 reads=[wuq, qn], writes=[wuq])
        for kc in range(2):
            P.op("dve", lambda e, kc=kc: e.tensor_scalar(out=wuk[:, kc, :], in0=wuk[:, kc, :], scalar1=kvn[:, kc:kc + 1], scalar2=None, op0=ALU.mult),
                 reads=[wuk, kvn], writes=[wuk])
            P.op("dve", lambda e, kc=kc: e.tensor_scalar(out=wuv[:, kc, :], in0=wuv[:, kc, :], scalar1=kvn[:, kc:kc + 1], scalar2=None, op0=ALU.mult),
                 reads=[wuv, kvn], writes=[wuv])
        rp = [P.sb("M_rp%d" % i, [128, 2, 512], F32, st) for i in range(2)]
        kr = [P.sb("M_kr%d" % i, [128, 2, 512], BF16, st) for i in range(2)]
        t1 = [P.sb("M_t1%d" % i, [128, 512], F32, st) for i in range(2)]
        t2 = [P.sb("M_t2%d" % i, [128, 512], F32, st) for i in range(2)]
        for ti, (t0, n) in enumerate(TILES):
            r_, kr_, a_, b_ = rp[ti % 2], kr[ti % 2], t1[ti % 2], t2[ti % 2]
            dma(P, "sp", r_[64:96, :, :n], din["c_rope"][64:96, :, t0:t0 + n], writes=[r_])
            dma(P, "sp", kr_[64:96, 0, :n], scr["kr"][64:96, t0:t0 + n], writes=[(kr_, 0)])
            dma(P, "sp", kr_[64:96, 1, :n], scr["kr"][192:224, t0:t0 + n], writes=[(kr_, 1)])
            P.op("dve", lambda e, r_=r_, kr_=kr_, a_=a_, n=n: e.tensor_tensor(out=a_[64:96, :n], in0=kr_[64:96, 0, :n], in1=r_[64:96, 0, :n], op=ALU.mult),
                 reads=[r_, (kr_, 0)], writes=[a_])
            P.op("dve", lambda e, r_=r_, kr_=kr_, b_=b_, n=n: e.tensor_tensor(out=b_[64:96, :n], in0=kr_[64:96, 1, :n], in1=r_[64:96, 1, :n], op=ALU.mult),
                 reads=[r_, (kr_, 1)], writes=[b_])
            P.op("pool", lambda e, a_=a_, b_=b_, n=n, t0=t0: e.tensor_tensor(out=Kh[64:96, t0:t0 + n], in0=a_[64:96, :n], in1=b_[64:96, :n], op=ALU.add),
                 reads=[a_, b_], writes=[("Kr", ti)])
        P.barrier()

        sqb = [P.sb("M_sq%d" % i, [128, 512], BF16, st) for i in range(2)]
        Et = Rot([P.sb("M_E%d" % i, [128, 512], BF16, st) for i in range(3)])
        dn = [P.sb("M_dn%d" % i, [64, 512], F32, st) for i in range(2)]
        ao = [P.sb("M_ao%d" % i, [64, 512], BF16, st) for i in range(2)]
        mt = [P.sb("M_mt%d" % i, [128, 512], F32, st) for i in range(2)]
        ps_s = Rot(k.psum[0:3])
        ps_o = Rot(k.psum[3:5])
        ps_p = Rot(k.psum[5:7])
        ps_m = k.psum[7]
        for h in range(8):
            for ti, (t0, n) in enumerate(TILES):
                ps = ps_p.next()
                for kc in range(2):
                    P.op("pe", lambda e, ps=ps, kc=kc, n=n, t0=t0, h=h: e.matmul(ps[0:64, :n], lhsT=wuk[:, kc, h * 64:(h + 1) * 64], rhs=ukvn[:, kc, t0:t0 + n],
                                                                            start=(kc == 0), stop=(kc == 1)), reads=[wuk], writes=[ps], acc=True)
                P.op("act", lambda e, ps=ps, n=n, t0=t0: e.copy(Kh[0:64, t0:t0 + n], ps[0:64, :n]), reads=[ps], writes=[("Kn", ti)])
                q = sqb[ti % 2]
                P.op("act", lambda e, q=q, n=n, t0=t0: e.activation(out=q[0:96, :n], in_=Kh[0:96, t0:t0 + n], func=AF.Square), reads=[("Kn", ti)], writes=[q])
                P.op("pe", lambda e, q=q, n=n: e.matmul(ps_m[:, :n], lhsT=C["ones"][0:96, :], rhs=q[0:96, :n], start=True, stop=True), reads=[q], writes=[ps_m])
                if ti == 0:
                    P.op("dve", lambda e, n=n: e.reduce_max(out=kmax[:], in_=ps_m[:, :n], axis=AX.X), reads=[ps_m], writes=[kmax])
                else:
                    P.op("dve", lambda e, n=n: e.reduce_max(out=kmt[:], in_=ps_m[:, :n], axis=AX.X), reads=[ps_m], writes=[kmt])
                    P.op("dve", lambda e: e.tensor_tensor(out=kmax[:], in0=kmax[:], in1=kmt[:], op=ALU.max), reads=[kmax, kmt], writes=[kmax])
            for c0 in range(0, NCH, 8):
                nb = min(8, NCH - c0)
                ps = ps_p.next()
                for j in range(nb):
                    ch = c0 + j
                    for kc in range(2):
                        P.op("pe", lambda e, ps=ps, j=j, ch=ch, kc=kc, h=h: e.matmul(ps[:, j * 64:(j + 1) * 64], lhsT=ukvn[:, kc, ch * 128:(ch + 1) * 128],
                                                                                rhs=wuv[:, kc, h * 64:(h + 1) * 64], start=(kc == 0), stop=(kc == 1)),
                             reads=[wuv], writes=[ps], acc=True)
                P.op("act", lambda e, ps=ps, c0=c0, nb=nb: e.copy(Vh[:, c0:c0 + nb, 0:64], ps[:, :nb * 64].rearrange("p (a b) -> p a b", b=64)),
                     reads=[ps], writes=[("Vh", c0)])
            for ti, (t0, n) in enumerate(TILES):
                pr, pw = ps_p.next(), ps_p.next()
                for kc in range(3):
                    P.op("pe", lambda e, pr=pr, kc=kc, n=n, t0=t0, h=h: e.matmul(pr[:, :n], lhsT=wuq[:, kc, h * 128:(h + 1) * 128], rhs=uqn[:, kc, t0:t0 + n],
                                                                            start=(kc == 0), stop=(kc == 2)), reads=[wuq], writes=[pr], acc=True)
                for kc in range(3):
                    P.op("pe", lambda e, pw=pw, kc=kc, n=n, t0=t0, h=h: e.matmul(pw[:, :n], lhsT=wuq[:, kc, 1024 + h * 128:1024 + (h + 1) * 128], rhs=uqn[:, kc, t0:t0 + n],
                                                                            start=(kc == 0), stop=(kc == 2)), reads=[wuq], writes=[pw], acc=True)
                r_, a_, b_ = rp[ti % 2], t1[ti % 2], t2[ti % 2]
                dma(P, "sp", r_[64:96, :, :n], din["c_rope"][64:96, :, t0:t0 + n], writes=[r_])
                P.op("act", lambda e, pr=pr, n=n, t0=t0: e.copy(Qh[0:64, t0:t0 + n], pr[0:64, :n]), reads=[pr], writes=[("Qn", ti)])
                P.op("dve", lambda e, pr=pr, r_=r_, a_=a_, n=n: e.tensor_tensor(out=a_[64:96, :n], in0=pr[64:96, :n], in1=r_[64:96, 0, :n], op=ALU.mult),
                     reads=[pr, r_], writes=[a_])
                P.op("dve", lambda e, pw=pw, r_=r_, b_=b_, n=n: e.tensor_tensor(out=b_[64:96, :n], in0=pw[64:96, :n], in1=r_[64:96, 1, :n], op=ALU.mult),
                     reads=[pw, r_], writes=[b_])
                P.op("pool", lambda e, a_=a_, b_=b_, n=n, t0=t0: e.tensor_tensor(out=Qh[64:96, t0:t0 + n], in0=a_[64:96, :n], in1=b_[64:96, :n], op=ALU.add),
                     reads=[a_, b_], writes=[("Qr", ti)])
                q = sqb[ti % 2]
                P.op("act", lambda e, q=q, n=n, t0=t0: e.activation(out=q[0:96, :n], in_=Qh[0:96, t0:t0 + n], func=AF.Square),
                     reads=[("Qn", ti), ("Qr", ti)], writes=[q])
                P.op("pe", lambda e, q=q, n=n: e.matmul(ps_m[:, :n], lhsT=C["ones"][0:96, :], rhs=q[0:96, :n], start=True, stop=True), reads=[q], writes=[ps_m])
                m_ = mt[ti % 2]
                P.op("act", lambda e, m_=m_, n=n: e.activation(out=m_[96:97, :n], in_=ps_m[96:97, :n], func=AF.Sqrt, bias=C["zero1"][96:97, 0:1], scale=kmax[96:97, 0:1]),
                     reads=[ps_m, kmax], writes=[m_])
                P.op("dve", lambda e, m_=m_, n=n, t0=t0: e.tensor_scalar(out=Qh[96:97, t0:t0 + n], in0=m_[96:97, :n], scalar1=-1.0, scalar2=None, op0=ALU.mult),
                     reads=[m_], writes=[("Qm", ti)])
            for ti, (t0, n) in enumerate(TILES):
                chunks = [0, 1] if t0 < CTX else list(range(NCH))
                po = ps_o.next()
                LOOK = 2
                pend = {}

                def emit_S(ci, ti=ti, t0=t0, n=n, chunks=chunks):
                    kc = chunks[ci]
                    ps = ps_s.next()
                    kti = 0 if kc < 2 else 1 + (kc - 2) // 4
                    P.op("pe", lambda e, ps=ps, kc=kc, n=n, t0=t0: e.matmul(ps[:, :n], lhsT=Kh[:, kc * 128:(kc + 1) * 128], rhs=Qh[:, t0:t0 + n], start=True, stop=True),
                         reads=[("Kn", kti), ("Qn", ti), ("Qr", ti), ("Qm", ti)], writes=[ps])
                    return ps

                for ci in range(min(LOOK, len(chunks))):
                    pend[ci] = emit_S(ci)
                for ci, kc in enumerate(chunks):
                    ps = pend.pop(ci)
                    E = Et.next()
                    P.op("act", lambda e, ps=ps, E=E, n=n: e.activation(out=E[:, :n], in_=ps[:, :n], func=AF.Exp, scale=MLA_SCALE), reads=[ps], writes=[E])
                    if ci + LOOK < len(chunks):
                        pend[ci + LOOK] = emit_S(ci + LOOK)
                    P.op("pe", lambda e, po=po, E=E, kc=kc, n=n, ci=ci, nc_=len(chunks): e.matmul(po[:, :n], lhsT=Vh[:, kc, :], rhs=E[:, :n], start=(ci == 0), stop=(ci == nc_ - 1)),
                         reads=[E, ("Vh", (kc // 8) * 8)], writes=[po], acc=True)
                d_, a_ = dn[ti % 2], ao[ti % 2]
                P.op("act", lambda e, po=po, d_=d_, n=n: e.copy(d_[:, :n], po[64:128, :n]), reads=[po], writes=[d_])
                P.op("dve", lambda e, d_=d_, n=n: e.reciprocal(d_[:, :n], d_[:, :n]), reads=[d_], writes=[d_])
                P.op("dve", lambda e, po=po, d_=d_, a_=a_, n=n: e.tensor_tensor(out=a_[:, :n], in0=po[0:64, :n], in1=d_[:, :n], op=ALU.mult),
                     reads=[po, d_], writes=[a_])
                dma(P, "sp", scr["att"][h * 64:(h + 1) * 64, t0:t0 + n], a_[:, :n], reads=[a_], writes=["att_o"])


def scan_order(rev):
    return list(range(NCH)) if not rev else [1, 0] + list(range(NCH - 1, 1, -1))


def phaseML(k, l):
    P, C, din, scr = k.P, k.C, k.din, k.scr
    with ExitStack() as st:
        k.alloc_psum(st, 7, 1)
        NG = NCH * 8
        gt = P.sb("L_gt", [128, NCH, 16], F32, st)
        lf = P.sb("L_lf", [128, NCH, 2, 4], F32, st)
        aex = P.sb("L_aex", [128, NG], F32, st)
        esrc = P.sb("L_esrc", [128, NG], F32, st)
        iosc = P.sb("L_iosc", [128, NG], F32, st)
        etot = P.sb("L_etot", [128, NG], F32, st)
        tmpg = P.sb("L_tmpg", [128, NCH, 2, 4], F32, st)
        ngb = P.sb("L_ngb", [128, 512], F32, st)
        dma(P, "sp", gt[:], scr["mif"].rearrange("(c p) g -> p c g", p=128), writes=[gt])
        dma(P, "sp", ngb[:], din["ml_ng"][l, 0:1, :].partition_broadcast(128), writes=[ngb])
        gt5 = gt[:].rearrange("p c (d i h) -> p c d i h", d=2, i=2)
        P.op("act", lambda e: e.activation(out=tmpg[:], in_=gt5[:, :, :, 1, :], func=AF.Exp, scale=-1.0), reads=[gt], writes=[tmpg])
        P.op("act", lambda e: e.activation(out=tmpg[:], in_=tmpg[:], func=AF.Ln, bias=C["one1"][:, 0:1], scale=1.0), reads=[tmpg], writes=[tmpg])
        P.op("dve", lambda e: e.tensor_scalar(out=lf[:], in0=tmpg[:], scalar1=-1.0, scalar2=None, op0=ALU.mult), reads=[tmpg], writes=[lf])
        psA, psT = k.psum[5], k.psum[6]
        for c in range(NCH):
            P.op("pe", lambda e, c=c: e.matmul(psA[:, c * 8:c * 8 + 4], lhsT=C["tri_gt"][:], rhs=lf[:, c, 0, :], start=True, stop=True), reads=[lf], writes=[psA], acc=True)
            P.op("pe", lambda e, c=c: e.matmul(psA[:, c * 8 + 4:c * 8 + 8], lhsT=C["tri_lt"][:], rhs=lf[:, c, 1, :], start=True, stop=True), reads=[lf], writes=[psA], acc=True)
        P.op("pe", lambda e: e.matmul(psT[:, :NG], lhsT=C["ones_f"][:], rhs=lf[:].rearrange("p c d h -> p (c d h)"), start=True, stop=True), reads=[lf], writes=[psT])
        P.op("dve", lambda e: e.tensor_copy(aex[:], psA[:, :NG]), reads=[psA], writes=[aex])
        P.op("act", lambda e: e.activation(out=iosc[:], in_=aex[:], func=AF.Exp), reads=[aex], writes=[iosc])
        P.op("act", lambda e: e.activation(out=etot[:], in_=psT[:, :NG], func=AF.Exp), reads=[psT], writes=[etot])
        P.op("dve", lambda e: e.tensor_tensor(out=esrc[:].rearrange("p (c d h) -> p c d h", d=2, h=4), in0=aex[:].rearrange("p (c d h) -> p c d h", d=2, h=4),
                                              in1=gt5[:, :, :, 0, :], op=ALU.add), reads=[aex, gt], writes=[esrc])
        P.op("act", lambda e: e.activation(out=esrc[:], in_=esrc[:], func=AF.Exp), reads=[esrc], writes=[esrc])
        P.barrier()

        gidx = lambda c, d, h: c * 8 + d * 4 + h
        qk = [P.sb("L_qk%d" % i, [128, 4, 128], BF16, st) for i in range(3)]
        va = [P.sb("L_va%d" % i, [128, 4, 129], BF16, st) for i in range(3)]
        for v_ in va:
            P.op("pool", lambda e, v_=v_: e.memset(v_[:, :, 128:129], 1.0), writes=[(v_, "one")])
        ktm = [P.sb("L_ktm%d" % i, [128, 2, 128], BF16, st) for i in range(2)]
        SM = Rot([P.sb("L_SM%d" % i, [128, 128], BF16, st) for i in range(4)])
        vp = Rot([P.sb("L_vp%d" % i, [128, 129], BF16, st) for i in range(4)])
        Cf = P.sb("L_Cf", [128, 4, 129], F32, st)
        Cb = P.sb("L_Cb", [128, 4, 129], BF16, st)
        ctmp = Rot([P.sb("L_ct%d" % i, [128, 129], F32, st) for i in range(4)])
        mx = Rot([P.sb("L_mx%d" % i, [128, 1], F32, st) for i in range(8)])
        hch = [P.sb("L_h%d" % i, [128, 512], F32, st) for i in range(2)]
        hpv = [P.sb("L_hp%d" % i, [128, 512], F32, st) for i in range(2)]
        mog = [P.sb("L_mo%d" % i, [128, 512], BF16, st) for i in range(2)]
        sq = P.sb("L_sq", [128, 512], F32, st)
        ss = P.sb("L_ss", [128, 4], F32, st)
        mtm = P.sb("L_mtm", [128, 512], BF16, st)
        mfm = [P.sb("L_mfm%d" % i, [128, 4, 128], BF16, st) for i in range(2)]
        ps_s = Rot(k.psum[0:2])
        ps_p = Rot(k.psum[2:4])
        ps_c = Rot(k.psum[4:6])
        pst = k.psbf[0]
        mqk3 = scr["mqk"].rearrange("(c p) t -> p c t", p=128)
        mout3 = scr["mout"].rearrange("(c p) t -> p c t", p=128)
        for d in range(2):
            order = scan_order(d == 1)
            mask = C["tri_le"] if d == 0 else C["tri_ge"]
            if d == 1:
                P.barrier()
            P.op("dve", lambda e: e.memset(Cf[:], 0.0), writes=[(Cf, h) for h in range(4)])
            P.op("pool", lambda e: e.memset(Cb[:], 0.0), writes=[(Cb, h) for h in range(4)])
            for oi, c in enumerate(order):
                q_, v_, kt_ = qk[oi % 3], va[oi % 3], ktm[oi % 2]
                dma(P, "sp", q_[:], mqk3[:, :, c * 128:(c + 1) * 128], writes=[q_])
                dma(P, "sp", v_[:, :, 0:128], scr["mv"][c * 128:(c + 1) * 128, :].rearrange("p (h v) -> p h v", h=4), writes=[v_])
                for j in range(2):
                    P.op("pe", lambda e, q_=q_, j=j: e.transpose(pst[:, j * 128:(j + 1) * 128], q_[:, 2 + j, :], C["ident"][:]), reads=[q_], writes=[pst])
                P.op("act", lambda e, kt_=kt_: e.copy(kt_[:], pst[:, 0:256].rearrange("p (a b) -> p a b", a=2)), reads=[pst], writes=[kt_])
                h_ = hch[oi % 2]
                if d == 1:
                    hp_, mo_ = hpv[oi % 2], mog[oi % 2]
                    dma(P, "sp", hp_[:], scr["hacc"][c * 128:(c + 1) * 128, :], writes=[hp_])
                    dma(P, "sp", mo_[:], scr["mo"][c * 128:(c + 1) * 128, :], writes=[mo_])
                for h in range(4):
                    pb = (h % 2) * 64
                    g = gidx(c, d, h)
                    pss, psp, psc = ps_s.next(), ps_p.next(), ps_c.next()
                    P.op("pe", lambda e, pss=pss, q_=q_, h=h, pb=pb: e.matmul(pss[:, 0:128], lhsT=q_[pb:pb + 64, 2 + h // 2, :], rhs=q_[pb:pb + 64, h // 2, :], start=True, stop=True),
                         reads=[q_], writes=[pss])
                    sm = SM.next()
                    P.op("dve", lambda e, pss=pss, sm=sm, mask=mask: e.tensor_tensor(out=sm[:], in0=pss[:, 0:128], in1=mask[:], op=ALU.mult), reads=[pss], writes=[sm])
                    vp_ = vp.next()
                    P.op("pool", lambda e, vp_=vp_, v_=v_, h=h, g=g: e.tensor_scalar(out=vp_[:], in0=v_[:, h, :], scalar1=esrc[:, g:g + 1], scalar2=None, op0=ALU.mult),
                         reads=[v_, (v_, "one")], writes=[vp_])
                    P.op("pe", lambda e, psp=psp, sm=sm, vp_=vp_: e.matmul(psp[:, 0:129], lhsT=sm[:], rhs=vp_[:], start=True, stop=False), reads=[sm, vp_], writes=[psp], acc=True)
                    P.op("pe", lambda e, psp=psp, q_=q_, h=h, pb=pb: e.matmul(psp[:, 0:129], lhsT=q_[pb:pb + 64, h // 2, :], rhs=Cb[pb:pb + 64, h, :], start=False, stop=True),
                         reads=[q_, (Cb, h)], writes=[psp], acc=True)
                    m_ = mx.next()
                    P.op("dve", lambda e, m_=m_, psp=psp, g=g: e.tensor_scalar(out=m_[:], in0=psp[:, 128:129], scalar1=C["neg1"][:, 0:1], scalar2=iosc[:, g:g + 1], op0=ALU.mult, op1=ALU.max),
                         reads=[psp], writes=[m_])
                    P.op("dve", lambda e, m_=m_, psp=psp: e.tensor_tensor(out=m_[:], in0=psp[:, 128:129], in1=m_[:], op=ALU.max),
                         reads=[psp, m_], writes=[m_])
                    P.op("dve", lambda e, m_=m_: e.reciprocal(m_[:], m_[:]), reads=[m_], writes=[m_])
                    if d == 0:
                        P.op("act", lambda e, h_=h_, psp=psp, m_=m_, h=h: e.activation(out=h_[:, h * 128:(h + 1) * 128], in_=psp[:, 0:128], func=AF.Identity,
                                                                                 bias=C["zero1"][:, 0:1], scale=m_[:, 0:1]), reads=[psp, m_], writes=[(h_, h)])
                    else:
                        P.op("dve", lambda e, h_=h_, psp=psp, m_=m_, h=h, hp_=hp_: e.scalar_tensor_tensor(out=h_[:, h * 128:(h + 1) * 128], in0=psp[:, 0:128], scalar=m_[:, 0:1],
                                                                                                   in1=hp_[:, h * 128:(h + 1) * 128], op0=ALU.mult, op1=ALU.add),
                             reads=[psp, m_, hp_], writes=[(h_, h)])
                    if oi + 1 < len(order):
                        gn = gidx(order[oi + 1], d, h)
                        P.op("pe", lambda e, psc=psc, kt_=kt_, h=h, vp_=vp_: e.matmul(psc[:, 0:129], lhsT=kt_[:, h // 2, :], rhs=vp_[:], start=True, stop=True),
                             reads=[kt_, vp_], writes=[psc])
                        ct = ctmp.next()
                        P.op("dve", lambda e, ct=ct, psc=psc, h=h, pb=pb: e.tensor_tensor(out=ct[pb:pb + 64, :], in0=psc[pb:pb + 64, 0:129], in1=Cf[pb:pb + 64, h, :], op=ALU.add),
                             reads=[psc, (Cf, h)], writes=[ct])
                        P.op("act", lambda e, ct=ct, h=h, pb=pb, gn=gn: e.activation(out=Cf[pb:pb + 64, h, :], in_=ct[pb:pb + 64, :], func=AF.Identity,
                                                                               bias=C["zero1"][pb:pb + 64, 0:1], scale=etot[pb:pb + 64, gn:gn + 1]), reads=[ct], writes=[(Cf, h)])
                        P.op("act", lambda e, ct=ct, h=h, pb=pb, gn=gn: e.activation(out=Cb[pb:pb + 64, h, :], in_=ct[pb:pb + 64, :], func=AF.Identity,
                                                                               bias=C["zero1"][pb:pb + 64, 0:1], scale=etot[pb:pb + 64, gn:gn + 1]), reads=[ct], writes=[(Cb, h)])
                hkeys = [(h_, h) for h in range(4)]
                if d == 0:
                    dma(P, "sp", scr["hacc"][c * 128:(c + 1) * 128, :], h_[:], reads=hkeys, writes=["hacc_o"])
                else:
                    P.op("act", lambda e, h_=h_: e.activation(out=sq[:], in_=h_[:], func=AF.Square), reads=hkeys, writes=[sq])
                    P.op("dve", lambda e: e.reduce_sum(out=ss[:], in_=sq[:].rearrange("p (h v) -> p h v", h=4), axis=AX.X), reads=[sq], writes=[ss])
                    P.op("act", lambda e: e.activation(out=ss[:], in_=ss[:], func=AF.Sqrt, bias=C["eps"][:, 0:1], scale=1.0 / 128), reads=[ss], writes=[ss])
                    P.op("dve", lambda e: e.reciprocal(ss[:], ss[:]), reads=[ss], writes=[ss])
                    P.op("dve", lambda e, h_=h_: e.tensor_tensor(out=sq[:].rearrange("p (h v) -> p h v", h=4), in0=h_[:].rearrange("p (h v) -> p h v", h=4),
                                                                 in1=ss[:].unsqueeze(2).to_broadcast([128, 4, 128]), op=ALU.mult), reads=hkeys + [ss], writes=[sq])
                    P.op("pool", lambda e: e.tensor_tensor(out=sq[:], in0=sq[:], in1=ngb[:], op=ALU.mult), reads=[sq], writes=[sq])
                    P.op("dve", lambda e, mo_=mo_: e.tensor_tensor(out=mtm[:], in0=sq[:], in1=mo_[:], op=ALU.mult), reads=[sq, mo_], writes=[mtm])
                    for j in range(4):
                        P.op("pe", lambda e, j=j: e.transpose(pst[:, 512 + j * 128:512 + (j + 1) * 128], mtm[:, j * 128:(j + 1) * 128], C["ident"][:]), reads=[mtm], writes=[(pst, "m")])
                    mf = mfm[oi % 2]
                    P.op("act", lambda e, mf=mf: e.copy(mf[:], pst[:, 512:1024].rearrange("p (a b) -> p a b", a=4)), reads=[(pst, "m")], writes=[mf])
                    dma(P, "sp", mout3[:, :, c * 128:(c + 1) * 128], mf[:], reads=[mf], writes=["mout_o"])


SSD_DEBUG_LIMIT = None
SSD_PRE_STOP = None


def phaseSSD(k, l):
    P, C, din, scr = k.P, k.C, k.din, k.scr
    with ExitStack() as st:
        k.alloc_psum(st, 7, 1)
        NW = NCH * 32
        dtt = P.sb("S_dtt", [128, 2, NCH, 16], F32, st)
        dta = P.sb("S_dta", [128, 2, NCH, 16], F32, st)
        ainc = P.sb("S_ainc", [128, 2, NCH, 16], F32, st)
        nain = P.sb("S_nain", [128, 2, NCH, 16], F32, st)
        eain = P.sb("S_eain", [128, 2, NCH, 16], F32, st)
        wsrc = P.sb("S_wsrc", [128, 2, NCH, 16], F32, st)
        etot = P.sb("S_etot", [128, 2, NCH, 16], F32, st)
        abc = P.sb("S_abc", [128, 32], F32, st)
        dbc = P.sb("S_dbc", [128, 16], F32, st)
        sngb = P.sb("S_sngb", [128, 1024], F32, st)
        negm = [P.sb("S_negm%d" % i, [128, 4, 128], BF16, st) for i in range(2)]
        dsp = [P.sb("S_dsp%d" % i, [128, 2, NCH, 16], BF16, st) for i in range(3)]
        dr1 = P.sb("S_dr1", [128, 2, NCH, 16], F32, st)
        for d_ in range(2):
            dma(P, "sp", dtt[:, d_], scr["sdt"][:, d_ * 16:(d_ + 1) * 16].rearrange("(c p) h -> p c h", p=128), writes=[(dtt, d_)])
        dma(P, "sp", abc[:], din["s_alog"][l, 0:1, :].partition_broadcast(128), writes=[abc])
        dma(P, "sp", dbc[:], din["s_d"][l, 0:1, :].partition_broadcast(128), writes=[dbc])
        dma(P, "sp", sngb[:], din["s_ng"][l, 0:1, :].partition_broadcast(128), writes=[sngb])
        if SSD_PRE_STOP == 1:
            return
        P.op("act", lambda e: e.activation(out=abc[:], in_=abc[:], func=AF.Exp), reads=[abc], writes=[abc])
        P.op("dve", lambda e: e.tensor_scalar(out=abc[:], in0=abc[:], scalar1=-1.0, scalar2=None, op0=ALU.mult), reads=[abc], writes=[abc])
        for d_ in range(2):
            P.op("dve", lambda e, d_=d_: e.tensor_tensor(out=dta[:, d_], in0=dtt[:, d_], in1=abc[:, d_ * 16:(d_ + 1) * 16].unsqueeze(1).to_broadcast([128, NCH, 16]), op=ALU.mult), reads=[(dtt, d_), abc], writes=[(dta, d_)])
        P.op("dve", lambda e: e.tensor_copy(dsp[0][:], dta[:]), reads=[(dta, 0), (dta, 1)], writes=[dsp[0]])
        P.op("dve", lambda e: e.tensor_tensor(out=dr1[:], in0=dta[:], in1=dsp[0][:], op=ALU.subtract), reads=[(dta, 0), (dta, 1), dsp[0]], writes=[dr1])
        P.op("dve", lambda e: e.tensor_copy(dsp[1][:], dr1[:]), reads=[dr1], writes=[dsp[1]])
        P.op("dve", lambda e: e.tensor_tensor(out=dsp[2][:], in0=dr1[:], in1=dsp[1][:], op=ALU.subtract), reads=[dr1, dsp[1]], writes=[dsp[2]])
        P.op("dve", lambda e: e.tensor_scalar(out=negm[0][:], in0=C["tri_gt"][:].unsqueeze(1).to_broadcast([128, 4, 128]), scalar1=NEG, scalar2=None, op0=ALU.mult), writes=[negm[0]])
        P.op("dve", lambda e: e.tensor_scalar(out=negm[1][:], in0=C["tri_lt"][:].unsqueeze(1).to_broadcast([128, 4, 128]), scalar1=NEG, scalar2=None, op0=ALU.mult), writes=[negm[1]])
        if SSD_PRE_STOP == 2:
            return
        def cums(tri0, tri1, post):
            for d_ in range(2):
                for hf_ in range(2):
                    ps = k.psum[d_ * 2 + hf_]
                    ca = hf_ * 17
                    for si in range(3):
                        P.op("pe", lambda e, ps=ps, d_=d_, ca=ca, si=si: e.matmul(ps[:, :272], lhsT=C[(tri0, tri1)[d_]][:], rhs=dsp[si][:, d_, ca:ca + 17, :].rearrange("p c h -> p (c h)"), start=(si == 0), stop=(si == 2)),
                             reads=[dsp[si]], writes=[ps], acc=True)
                    post(ps, d_, ca)

        def post_inc(ps, d_, ca):
            v = lambda t: t[:, d_, ca:ca + 17, :].rearrange("p c h -> p (c h)")
            P.op("dve", lambda e: e.tensor_copy(v(ainc), ps[:, :272]), reads=[ps], writes=[(ainc, d_, ca)])
            P.op("act", lambda e: e.activation(out=v(eain), in_=ps[:, :272], func=AF.Exp), reads=[ps], writes=[(eain, d_, ca)])
            P.op("dve", lambda e: e.tensor_scalar(out=v(nain), in0=v(ainc), scalar1=-1.0, scalar2=None, op0=ALU.mult), reads=[(ainc, d_, ca)], writes=[(nain, d_, ca)])

        def post_exc(ps, d_, ca):
            v = lambda t: t[:, d_, ca:ca + 17, :].rearrange("p c h -> p (c h)")
            P.op("act", lambda e: e.activation(out=v(wsrc), in_=ps[:, :272], func=AF.Exp), reads=[ps], writes=[(wsrc, d_, ca)])
            P.op("dve", lambda e: e.tensor_tensor(out=v(wsrc), in0=v(wsrc), in1=v(dtt), op=ALU.mult), reads=[(wsrc, d_, ca)], writes=[(wsrc, d_, ca)])

        def post_tot(ps, d_, ca):
            v = lambda t: t[:, d_, ca:ca + 17, :].rearrange("p c h -> p (c h)")
            P.op("act", lambda e: e.activation(out=v(etot), in_=ps[:, :272], func=AF.Exp), reads=[ps], writes=[(etot, d_, ca)])

        cums("tri_le_b", "tri_ge_b", post_inc)
        P.barrier()
        if SSD_PRE_STOP == 3:
            return
        cums("tri_gt_b", "tri_lt_b", post_exc)
        P.barrier()
        if SSD_PRE_STOP == 4:
            return
        cums("ones", "ones", post_tot)
        P.barrier()
        if SSD_PRE_STOP == 5:
            return

        xt = [P.sb("S_x%d" % i, [128, 1024], BF16, st) for i in range(2)]
        Bt = [P.sb("S_Bt%d" % i, [128, 512], BF16, st) for i in range(2)]
        Bf = [P.sb("S_Bf%d" % i, [128, 4, 128], BF16, st) for i in range(2)]
        Cf = [P.sb("S_Cf%d" % i, [128, 4, 128], BF16, st) for i in range(2)]
        X = [P.sb("S_X%d" % i, [128, 2, 16, 128], BF16, st) for i in range(2)]
        cbm = [P.sb("S_cbm%d" % i, [128, 4, 128], F32, st) for i in range(2)]
        Eh = [P.sb("S_E%d" % i, [128, 8, 128], F32, st) for i in range(2)]
        MT = [P.sb("S_MT%d" % i, [128, 8, 128], BF16, st) for i in range(2)]
        xw = [P.sb("S_xw%d" % i, [128, 1024], BF16, st) for i in range(2)]
        Hf = P.sb("S_Hf", [128, 1024], F32, st)
        Hb = P.sb("S_Hb", [128, 1024], BF16, st)
        ht = P.sb("S_ht", [128, 1024], F32, st)
        ych = [P.sb("S_y%d" % i, [128, 1024], F32, st) for i in range(2)]
        ypv = [P.sb("S_yp%d" % i, [128, 1024], F32, st) for i in range(2)]
        szt = [P.sb("S_sz%d" % i, [128, 1024], BF16, st) for i in range(2)]
        y2 = P.sb("S_y2", [128, 1024], F32, st)
        sq = P.sb("S_sq", [128, 1024], F32, st)
        ss = P.sb("S_ss", [128, 4], F32, st)
        stm = P.sb("S_stm", [128, 1024], BF16, st)
        sfm = [P.sb("S_sfm%d" % i, [128, 8, 128], BF16, st) for i in range(2)]
        ps_cb = k.psum[0]
        ps_A = Rot([(k.psum[1], k.psum[2]), (k.psum[3], k.psum[4])])
        ps_y, ps_i, ps_h = k.psum[5], k.psum[6], k.psum[0]
        pst = k.psbf[0]
        sBf3 = scr["sBf"].rearrange("(g p) t -> p g t", p=128)
        sCf3 = scr["sCf"].rearrange("(g p) t -> p g t", p=128)
        sout3 = scr["sout"].rearrange("(c p) t -> p c t", p=128)
        for d in range(2):
            order = scan_order(d == 1)
            mask = C["tri_le"] if d == 0 else C["tri_ge"]
            maskb = C["tri_le_b"] if d == 0 else C["tri_ge_b"]
            if d == 1:
                P.barrier()
            P.op("dve", lambda e: e.memset(Hf[:], 0.0), writes=[Hf])
            P.op("pool", lambda e: e.memset(Hb[:], 0.0), writes=[Hb])
            for oi, c in enumerate(order):
                if SSD_DEBUG_LIMIT is not None and (d * NCH + oi) >= SSD_DEBUG_LIMIT:
                    break
                i2 = oi % 2
                x_, bt_, bf_, cf_, X_, cb_, xw_, y_ = xt[i2], Bt[i2], Bf[i2], Cf[i2], X[i2], cbm[i2], xw[i2], ych[i2]
                dma(P, "sp", x_[:], scr["sx"][c * 128:(c + 1) * 128, :], writes=[x_])
                dma(P, "sp", bt_[:], scr["sBt"][c * 128:(c + 1) * 128, :], writes=[bt_])
                dma(P, "sp", bf_[:], sBf3[:, :, c * 128:(c + 1) * 128], writes=[bf_])
                dma(P, "sp", cf_[:], sCf3[:, :, c * 128:(c + 1) * 128], writes=[cf_])
                if d == 1:
                    yp_, sz_ = ypv[i2], szt[i2]
                    dma(P, "sp", yp_[:], scr["yacc"][c * 128:(c + 1) * 128, :], writes=[yp_])
                    dma(P, "sp", sz_[:], scr["sz"][c * 128:(c + 1) * 128, :], writes=[sz_])
                go = d * 16
                for g in range(4):
                    P.op("pe", lambda e, g=g, bf_=bf_, cf_=cf_: e.matmul(ps_cb[:, g * 128:(g + 1) * 128], lhsT=bf_[:, g, :], rhs=cf_[:, g, :], start=True, stop=True),
                         reads=[bf_, cf_], writes=[ps_cb], acc=True)
                P.op("dve", lambda e, cb_=cb_, mask=mask: e.tensor_tensor(out=cb_[:], in0=ps_cb[:].rearrange("p (g t) -> p g t", g=4), in1=mask[:].unsqueeze(1).to_broadcast([128, 4, 128]), op=ALU.mult),
                     reads=[ps_cb], writes=[cb_])
                for si in range(2):
                    P.op("dve", lambda e, X_=X_, c=c, d=d, maskb=maskb, si=si: e.tensor_tensor(out=X_[:, si], in0=maskb[:].unsqueeze(1).to_broadcast([128, 16, 128]),
                                                                                        in1=dsp[si][:, d, c, :].unsqueeze(2).to_broadcast([128, 16, 128]), op=ALU.mult), writes=[(X_, si)])
                for hf in range(2):
                    pA = ps_A.next()
                    E_, M_ = Eh[hf], MT[hf]
                    for j in range(2):
                        gg = hf * 2 + j
                        for si in range(2):
                            P.op("pe", lambda e, pA=pA, j=j, gg=gg, X_=X_, si=si: e.matmul(pA[j][:, :], lhsT=C["ones"][:], rhs=X_[:, si, gg * 4:(gg + 1) * 4, :].rearrange("p h t -> p (h t)"), start=(si == 0), stop=False),
                                 reads=[(X_, si)], writes=[pA[j]], acc=True)
                        P.op("pe", lambda e, pA=pA, j=j, d=d: e.matmul(pA[j][:, :], lhsT=C["ident"][:], rhs=negm[d][:].rearrange("p h t -> p (h t)"), start=False, stop=True),
                             reads=[negm[d]], writes=[pA[j]], acc=True)
                    for hh in range(8):
                        h = hf * 8 + hh
                        P.op("act", lambda e, pA=pA, hh=hh, h=h, E_=E_, c=c, d=d: e.activation(out=E_[:, hh, :], in_=pA[hh // 4][:, (hh % 4) * 128:(hh % 4 + 1) * 128], func=AF.Exp,
                                                                                         bias=nain[:, d, c, h:h + 1], scale=1.0), reads=[pA[hh // 4]], writes=[(E_, hh)])
                        P.op("dve", lambda e, hh=hh, h=h, E_=E_, M_=M_, cb_=cb_, c=c, d=d: e.scalar_tensor_tensor(out=M_[:, hh, :], in0=E_[:, hh, :], scalar=dtt[:, d, c, h:h + 1],
                                                                                                        in1=cb_[:, h // 4, :], op0=ALU.mult, op1=ALU.mult),
                             reads=[(E_, hh), cb_], writes=[(M_, hh)])
                    for hh in range(8):
                        h = hf * 8 + hh
                        P.op("pe", lambda e, hh=hh, h=h, M_=M_, x_=x_: e.matmul(ps_y[:, hh * 64:(hh + 1) * 64], lhsT=M_[:, hh, :], rhs=x_[:, h * 64:(h + 1) * 64], start=True, stop=True),
                             reads=[(M_, hh), x_], writes=[ps_y], acc=True)
                    for j in range(2):
                        gg = hf * 2 + j
                        P.op("pe", lambda e, j=j, gg=gg, cf_=cf_: e.matmul(ps_i[:, j * 256:(j + 1) * 256], lhsT=cf_[:, gg, :], rhs=Hb[:, gg * 256:(gg + 1) * 256], start=True, stop=True),
                             reads=[cf_, Hb], writes=[ps_i], acc=True)
                    ysl = y_[:, hf * 512:(hf + 1) * 512]
                    P.op("dve", lambda e, ysl=ysl, c=c, d=d, hf=hf: e.tensor_tensor(out=ysl.rearrange("p (h q) -> p h q", h=8), in0=ps_i[:].rearrange("p (h q) -> p h q", h=8),
                                                                                 in1=eain[:, d, c, hf * 8:hf * 8 + 8].unsqueeze(2).to_broadcast([128, 8, 64]), op=ALU.mult),
                         reads=[ps_i], writes=[(y_, hf)])
                    P.op("dve", lambda e, ysl=ysl: e.tensor_tensor(out=ysl, in0=ps_y[:], in1=ysl, op=ALU.add), reads=[ps_y, (y_, hf)], writes=[(y_, hf)])
                    if d == 1:
                        P.op("pool", lambda e, ysl=ysl, yp_=yp_, hf=hf: e.tensor_tensor(out=ysl, in0=ysl, in1=yp_[:, hf * 512:(hf + 1) * 512], op=ALU.add),
                             reads=[(y_, hf), yp_], writes=[(y_, hf)])
                if oi + 1 < len(order):
                    P.op("dve", lambda e, xw_=xw_, x_=x_, c=c, d=d: e.tensor_tensor(out=xw_[:].rearrange("p (h q) -> p h q", h=16), in0=x_[:].rearrange("p (h q) -> p h q", h=16),
                                                                                  in1=wsrc[:, d, c, :].unsqueeze(2).to_broadcast([128, 16, 64]), op=ALU.mult),
                         reads=[x_], writes=[xw_])
                    P.op("dve", lambda e, c=c, d=d: e.tensor_tensor(out=ht[:].rearrange("p (h q) -> p h q", h=16), in0=Hf[:].rearrange("p (h q) -> p h q", h=16),
                                                                      in1=etot[:, d, c, :].unsqueeze(2).to_broadcast([128, 16, 64]), op=ALU.mult),
                         reads=[Hf], writes=[ht])
                    for hf in range(2):
                        for j in range(2):
                            gg = hf * 2 + j
                            P.op("pe", lambda e, j=j, gg=gg, bt_=bt_, xw_=xw_: e.matmul(ps_h[:, j * 256:(j + 1) * 256], lhsT=bt_[:, gg * 128:(gg + 1) * 128], rhs=xw_[:, gg * 256:(gg + 1) * 256], start=True, stop=True),
                                 reads=[bt_, xw_], writes=[ps_h], acc=True)
                        P.op("dve", lambda e, hf=hf: e.tensor_tensor(out=Hf[:, hf * 512:(hf + 1) * 512], in0=ps_h[:], in1=ht[:, hf * 512:(hf + 1) * 512], op=ALU.add),
                             reads=[ps_h, ht], writes=[Hf])
                    P.op("act", lambda e: e.copy(Hb[:], Hf[:]), reads=[Hf], writes=[Hb])
                ykeys = [(y_, 0), (y_, 1)]
                if d == 0:
                    dma(P, "sp", scr["yacc"][c * 128:(c + 1) * 128, :], y_[:], reads=ykeys, writes=["yacc_o"])
                else:
                    P.op("dve", lambda e, x_=x_: e.tensor_tensor(out=y2[:].rearrange("p (h q) -> p h q", h=16), in0=x_[:].rearrange("p (h q) -> p h q", h=16),
                                                                  in1=dbc[:].unsqueeze(2).to_broadcast([128, 16, 64]), op=ALU.mult), reads=[x_], writes=[y2])
                    P.op("dve", lambda e, y_=y_: e.tensor_tensor(out=y2[:], in0=y2[:], in1=y_[:], op=ALU.add), reads=[y2] + ykeys, writes=[y2])
                    P.op("dve", lambda e, sz_=sz_: e.tensor_tensor(out=y2[:], in0=y2[:], in1=sz_[:], op=ALU.mult), reads=[y2, sz_], writes=[y2])
                    P.op("act", lambda e: e.activation(out=sq[:], in_=y2[:], func=AF.Square), reads=[y2], writes=[sq])
                    P.op("dve", lambda e: e.reduce_sum(out=ss[:], in_=sq[:].rearrange("p (g v) -> p g v", g=4), axis=AX.X), reads=[sq], writes=[ss])
                    P.op("act", lambda e: e.activation(out=ss[:], in_=ss[:], func=AF.Sqrt, bias=C["eps"][:, 0:1], scale=1.0 / 256), reads=[ss], writes=[ss])
                    P.op("dve", lambda e: e.reciprocal(ss[:], ss[:]), reads=[ss], writes=[ss])
                    P.op("dve", lambda e: e.tensor_tensor(out=sq[:].rearrange("p (g v) -> p g v", g=4), in0=y2[:].rearrange("p (g v) -> p g v", g=4),
                                                          in1=ss[:].unsqueeze(2).to_broadcast([128, 4, 256]), op=ALU.mult), reads=[y2, ss], writes=[sq])
                    P.op("pool", lambda e: e.tensor_tensor(out=stm[:], in0=sq[:], in1=sngb[:], op=ALU.mult), reads=[sq], writes=[stm])
                    sf = sfm[i2]
                    for j in range(8):
                        P.op("pe", lambda e, j=j: e.transpose(pst[:, j * 128:(j + 1) * 128], stm[:, j * 128:(j + 1) * 128], C["ident"][:]), reads=[stm], writes=[pst])
                    P.op("act", lambda e, sf=sf: e.copy(sf[:], pst[:].rearrange("p (a b) -> p a b", a=8)), reads=[pst], writes=[sf])
                    dma(P, "sp", sout3[:, :, c * 128:(c + 1) * 128], sf[:], reads=[sf], writes=["sout_o"])


def phaseOUT(k, l):
    P, C, din, scr = k.P, k.C, k.din, k.scr
    with ExitStack() as st:
        k.alloc_psum(st, 8)
        wa = P.sb("O_wa", [64, 8, 1024], BF16, st)
        wm = P.sb("O_wm", [128, 4, 1024], BF16, st)
        ws = P.sb("O_ws", [128, 8, 1024], BF16, st)
        wo = P.sb("O_wo", [128, 8, 1024], BF16, st)
        wrf = P.sb("O_wrf", [128, 8, 32], F32, st)
        wrh = P.sb("O_wrh", [128, 8, 32], BF16, st)
        wrl = P.sb("O_wrl", [128, 8, 32], BF16, st)
        brt = P.sb("O_brt", [128, 32], F32, st)
        dma(P, "pool", wa[:], din["w_bra"][l].rearrange("(h p) n -> p h n", p=64), writes=[wa])
        dma(P, "pool", wm[:], din["w_brm"][l].rearrange("(c p) n -> p c n", p=128), writes=[wm])
        dma(P, "pool", ws[:], din["w_brs"][l].rearrange("(c p) n -> p c n", p=128), writes=[ws])
        dma(P, "pool", wo[:], din["w_out"][l].rearrange("(c p) n -> p c n", p=128), writes=[wo])
        dma(P, "sp", wrf[:], din["w_rt"][l].rearrange("(c p) n -> p c n", p=128), writes=[wrf])
        dma(P, "sp", brt[:], din["b_rt"][l, 0:1, :].partition_broadcast(128), writes=[brt])
        P.op("dve", lambda e: e.tensor_copy(wrh[:], wrf[:]), reads=[wrf], writes=[wrh])
        P.op("dve", lambda e: e.tensor_tensor(out=wrl[:], in0=wrf[:], in1=wrh[:], op=ALU.subtract), reads=[wrf, wrh], writes=[wrl])
        att = P.sb("O_att", [64, 8, 512], BF16, st)
        mo_ = P.sb("O_mo", [128, 4, 512], BF16, st)
        so_ = P.sb("O_so", [128, 8, 512], BF16, st)
        gg_ = P.sb("O_gg", [128, 24, 512], BF16, st)
        xr = P.sb("O_xr", [128, 8, 512], F32, st)
        mg = P.sb("O_mg", [128, 8, 512], BF16, st)
        tt = [[P.sb("O_t%d%d" % (i, j), [128, 512], F32, st) for j in range(3)] for i in range(2)]
        sq = P.sb("O_sq", [128, 8, 512], BF16, st)
        rs = P.sb("O_rs", [128, 512], F32, st)
        hfc = [P.sb("O_hf%d" % i, [128, 512], F32, st) for i in range(2)]
        h2b = P.sb("O_h2b", [128, 8, 512], BF16, st)
        hlo = P.sb("O_hlo", [128, 8, 512], BF16, st)
        lg = P.sb("O_lg", [128, 4, 32], F32, st)
        mk = P.sb("O_mk", [128, 4, 32], F32, st)
        ex = P.sb("O_ex", [128, 4, 32], F32, st)
        mx8 = P.sb("O_mx8", [128, 4, 8], F32, st)
        nmx = P.sb("O_nmx", [128, 4], F32, st)
        sm = P.sb("O_sm", [128, 4], F32, st)
        gfs = P.sb("O_gfs", [32, 512], F32, st)
        att3 = scr["att"].rearrange("(h p) t -> p h t", p=64)
        mo3 = scr["mout"].rearrange("(c p) t -> p c t", p=128)
        so3 = scr["sout"].rearrange("(c p) t -> p c t", p=128)
        gg3 = scr["gg"].rearrange("(c p) t -> p c t", p=128)
        xr3 = scr["xres"].rearrange("(c p) t -> p c t", p=128)
        h23 = scr["hn2"].rearrange("(c p) t -> p c t", p=128)
        modv, a2 = k.modv, k.a2
        for ti, (t0, n) in enumerate(TILES):
            mc = 1 if t0 < CTX else 0
            nj = n // 128
            dma(P, "sp", att[:, :, :n], att3[:, :, t0:t0 + n], writes=[att])
            dma(P, "sp", mo_[:, :, :n], mo3[:, :, t0:t0 + n], writes=[mo_])
            dma(P, "sp", so_[:, :, :n], so3[:, :, t0:t0 + n], writes=[so_])
            for g3 in range(3):
                dma(P, "sp", gg_[:, g3 * 8:(g3 + 1) * 8, :n], gg3[:, g3 * 8:(g3 + 1) * 8, t0:t0 + n], writes=[(gg_, g3)])
            dma(P, "sp", xr[:, :, :n], xr3[:, :, t0:t0 + n], writes=[xr] + [(xr, "n", oc) for oc in range(8)])
            for oc in range(8):
                pp = k.psum[(oc % 2) * 3:(oc % 2) * 3 + 3]
                tq = tt[oc % 2]
                for h in range(8):
                    P.op("pe", lambda e, p0=pp[0], h=h, oc=oc, n=n: e.matmul(p0[:, :n], lhsT=wa[:, h, oc * 128:(oc + 1) * 128], rhs=att[:, h, :n], start=(h == 0), stop=(h == 7)),
                         reads=[wa, att], writes=[pp[0]], acc=True)
                for c in range(4):
                    P.op("pe", lambda e, p1=pp[1], c=c, oc=oc, n=n: e.matmul(p1[:, :n], lhsT=wm[:, c, oc * 128:(oc + 1) * 128], rhs=mo_[:, c, :n], start=(c == 0), stop=(c == 3)),
                         reads=[wm, mo_], writes=[pp[1]], acc=True)
                for c in range(8):
                    P.op("pe", lambda e, p2=pp[2], c=c, oc=oc, n=n: e.matmul(p2[:, :n], lhsT=ws[:, c, oc * 128:(oc + 1) * 128], rhs=so_[:, c, :n], start=(c == 0), stop=(c == 7)),
                         reads=[ws, so_], writes=[pp[2]], acc=True)
                for b in range(3):
                    P.op("dve", lambda e, b=b, pb=pp[b], tq=tq, oc=oc, n=n: e.tensor_tensor(out=tq[b][:, :n], in0=pb[:, :n], in1=gg_[:, b * 8 + oc, :n], op=ALU.mult),
                         reads=[pp[b], (gg_, b)], writes=[tq[b]])
                P.op("pool", lambda e, tq=tq, n=n: e.tensor_tensor(out=tq[0][:, :n], in0=tq[0][:, :n], in1=tq[1][:, :n], op=ALU.add), reads=[tq[0], tq[1]], writes=[tq[0]])
                P.op("pool", lambda e, tq=tq, n=n, oc=oc: e.tensor_tensor(out=mg[:, oc, :n], in0=tq[0][:, :n], in1=tq[2][:, :n], op=ALU.add), reads=[tq[0], tq[2]], writes=[(mg, oc)])
            for oc in range(8):
                ps = k.psum[6 + oc % 2]
                for c in range(8):
                    P.op("pe", lambda e, ps=ps, c=c, oc=oc, n=n: e.matmul(ps[:, :n], lhsT=wo[:, c, oc * 128:(oc + 1) * 128], rhs=mg[:, c, :n], start=(c == 0), stop=(c == 7)),
                         reads=[wo] + [(mg, c2) for c2 in range(8)], writes=[ps], acc=True)
                P.op("dve", lambda e, ps=ps, oc=oc, n=n, mc=mc: e.scalar_tensor_tensor(out=xr[:, oc, :n], in0=ps[:, :n], scalar=modv[:, 16 + oc, mc:mc + 1], in1=xr[:, oc, :n], op0=ALU.mult, op1=ALU.add),
                     reads=[ps, xr], writes=[(xr, "n", oc)])
            xkeys = [(xr, "n", oc) for oc in range(8)]
            dma(P, "sp", xr3[:, :, t0:t0 + n], xr[:, :, :n], reads=xkeys, writes=["xres_o"])
            P.op("act", lambda e, n=n: e.activation(out=sq[:, :, :n], in_=xr[:, :, :n], func=AF.Square), reads=xkeys, writes=[sq])
            ps = k.psum[6]
            for c in range(8):
                P.op("pe", lambda e, ps=ps, c=c, n=n: e.matmul(ps[:, :n], lhsT=C["ones"][:], rhs=sq[:, c, :n], start=(c == 0), stop=(c == 7)), reads=[sq], writes=[ps], acc=True)
            P.op("act", lambda e, ps=ps, n=n: e.activation(out=rs[:, :n], in_=ps[:, :n], func=AF.Sqrt, bias=C["eps"][:, 0:1], scale=1.0 / D), reads=[ps], writes=[rs])
            P.op("dve", lambda e, n=n: e.reciprocal(rs[:, :n], rs[:, :n]), reads=[rs], writes=[rs])
            for c in range(8):
                hf = hfc[c % 2]
                P.op("dve", lambda e, hf=hf, c=c, n=n: e.tensor_tensor(out=hf[:, :n], in0=xr[:, c, :n], in1=rs[:, :n], op=ALU.mult), reads=xkeys + [rs, "xres_o"], writes=[hf])
                P.op("act", lambda e, hf=hf, c=c, n=n, mc=mc: e.activation(out=hf[:, :n], in_=hf[:, :n], func=AF.Identity, bias=modv[:, 24 + c, mc:mc + 1], scale=a2[:, c, mc:mc + 1]),
                     reads=[hf], writes=[hf])
                P.op("dve", lambda e, hf=hf, c=c, n=n: e.tensor_copy(h2b[:, c, :n], hf[:, :n]), reads=[hf], writes=[(h2b, c)])
                P.op("dve", lambda e, hf=hf, c=c, n=n: e.tensor_tensor(out=hlo[:, c, :n], in0=hf[:, :n], in1=h2b[:, c, :n], op=ALU.subtract), reads=[hf, (h2b, c)], writes=[(hlo, c)])
            hkeys = [(h2b, c) for c in range(8)]
            dma(P, "sp", h23[:, :, t0:t0 + n], h2b[:, :, :n], reads=hkeys, writes=["hn2_o"])
            pr = k.psum[7]
            for j in range(nj):
                cnt = 0
                for (ha, wb_) in ((h2b, wrh), (h2b, wrl), (hlo, wrh)):
                    for c in range(8):
                        P.op("pe", lambda e, j=j, c=c, ha=ha, wb_=wb_, cnt=cnt: e.matmul(pr[:, j * 32:(j + 1) * 32], lhsT=ha[:, c, j * 128:(j + 1) * 128], rhs=wb_[:, c, :], start=(cnt == 0), stop=(cnt == 23)),
                             reads=hkeys + [(hlo, c2) for c2 in range(8)] + [wrh, wrl], writes=[pr], acc=True)
                        cnt += 1
            P.op("dve", lambda e, nj=nj: e.tensor_tensor(out=lg[:, :nj, :], in0=pr[:, :nj * 32].rearrange("p (j x) -> p j x", x=32), in1=brt[:].unsqueeze(1).to_broadcast([128, nj, 32]), op=ALU.add),
                 reads=[pr, brt], writes=[lg])
            for j in range(nj):
                P.op("dve", lambda e, j=j: e.max(out=mx8[:, j, :], in_=lg[:, j, :]), reads=[lg], writes=[(mx8, j)])
            mkeys = [(mx8, j) for j in range(nj)]
            P.op("dve", lambda e, nj=nj: e.tensor_scalar(out=nmx[:, :nj], in0=mx8[:, :nj, 0], scalar1=-1.0, scalar2=None, op0=ALU.mult), reads=mkeys, writes=[nmx])
            for j in range(nj):
                P.op("dve", lambda e, j=j: e.tensor_scalar(out=mk[:, j, :], in0=lg[:, j, :], scalar1=mx8[:, j, 3:4], scalar2=C["big"][:, 0:1], op0=ALU.subtract, op1=ALU.mult), reads=[lg] + mkeys, writes=[(mk, j)])
                P.op("dve", lambda e, j=j: e.tensor_scalar(out=mk[:, j, :], in0=mk[:, j, :], scalar1=1.0, scalar2=0.0, op0=ALU.add, op1=ALU.max), reads=[(mk, j)], writes=[(mk, j)])
                P.op("dve", lambda e, j=j: e.tensor_scalar(out=mk[:, j, :], in0=mk[:, j, :], scalar1=1.0, scalar2=None, op0=ALU.min), reads=[(mk, j)], writes=[(mk, j)])
                P.op("act", lambda e, j=j: e.activation(out=ex[:, j, :], in_=lg[:, j, :], func=AF.Exp, bias=nmx[:, j:j + 1], scale=1.0), reads=[lg, nmx], writes=[(ex, j)])
                P.op("dve", lambda e, j=j: e.tensor_tensor(out=ex[:, j, :], in0=ex[:, j, :], in1=mk[:, j, :], op=ALU.mult), reads=[(ex, j), (mk, j)], writes=[(ex, j)])
            ekeys = [(ex, j) for j in range(nj)]
            P.op("dve", lambda e, nj=nj: e.reduce_sum(out=sm[:, :nj], in_=ex[:, :nj, :], axis=AX.X), reads=ekeys, writes=[sm])
            P.op("dve", lambda e, nj=nj: e.reciprocal(sm[:, :nj], sm[:, :nj]), reads=[sm], writes=[sm])
            P.op("dve", lambda e, nj=nj: e.tensor_tensor(out=ex[:, :nj, :], in0=ex[:, :nj, :], in1=sm[:, :nj].unsqueeze(2).to_broadcast([128, nj, 32]), op=ALU.mult), reads=ekeys + [sm], writes=ekeys)
            pg = k.psum[6]
            for j in range(nj):
                P.op("pe", lambda e, j=j: e.transpose(pg[0:32, j * 128:(j + 1) * 128], ex[:, j, :], C["ident_f"][:]), reads=ekeys, writes=[pg], acc=True)
            P.op("act", lambda e, n=n: e.copy(gfs[:, :n], pg[0:32, :n]), reads=[pg], writes=[gfs])
            dma(P, "sp", scr["gfm"][:, t0:t0 + n], gfs[:, :n], reads=[gfs], writes=["gfm_o"])


MOE_BLOCKS = [
    [(0, 256), (256, 384), (640, 512)],
    [(1152, 384), (1536, 384), (1920, 384)],
    [(2304, 512), (2816, 512)],
    [(3328, 512), (3840, 512)],
]
MOE_EXPERTS = NE


def phaseMOE(k, l):
    P, C, din, scr = k.P, k.C, k.din, k.scr
    with ExitStack() as st:
        k.alloc_psum(st, 8)
        NB = 1152
        h2 = P.sb("E_h2", [128, 8, NB], BF16, st)
        Gt = P.sb("E_G", [32, NB], F32, st)
        yacc = P.sb("E_y", [128, 8, NB], F32, st)
        wup = [P.sb("E_wu%d" % i, [128, 8, 2048], BF16, st) for i in range(2)]
        wdn = [P.sb("E_wd%d" % i, [128, 8, 1024], BF16, st) for i in range(2)]
        bup = [P.sb("E_bu%d" % i, [128, 16], F32, st) for i in range(2)]
        bdn = P.sb("E_bdn", [32, 1024], F32, st)
        gb = [P.sb("E_gb%d" % i, [128, 512], F32, st) for i in range(2)]
        glu = [P.sb("E_gl%d" % i, [128, 512], BF16, st) for i in range(2)]
        sig = [P.sb("E_sg%d" % i, [128, 512], BF16, st) for i in range(2)]
        lin = [P.sb("E_ln%d" % i, [128, 512], BF16, st) for i in range(2)]
        avs = [P.sb("E_a%d" % i, [128, 8, 512], BF16, st) for i in range(2)]
        avi = [0]
        xc = [P.sb("E_xc%d" % i, [128, 512], F32, st) for i in range(2)]
        h23 = scr["hn2"].rearrange("(c p) t -> p c t", p=128)
        xr3 = scr["xres"].rearrange("(c p) t -> p c t", p=128)
        dma(P, "sp", bdn[:], din["b_dn"][l], writes=[bdn])
        ps_gl = [k.psum[0], k.psum[1]]
        ps_ln = [k.psum[2], k.psum[3]]
        ps_y = [k.psum[4], k.psum[5]]
        ps_b = k.psum[6]
        wi = 0
        for blk in MOE_BLOCKS:
            b0 = blk[0][0]
            nb = sum(n for _, n in blk)
            dma(P, "sp", h2[:, :, :nb], h23[:, :, b0:b0 + nb], writes=[h2])
            dma(P, "sp", Gt[:, :nb], scr["gfm"][:, b0:b0 + nb], writes=[Gt])
            for (t0, n) in blk:
                o = t0 - b0
                for oc in range(8):
                    P.op("pe", lambda e, oc=oc, o=o, n=n: e.matmul(ps_b[:, :n], lhsT=bdn[:, oc * 128:(oc + 1) * 128], rhs=Gt[:, o:o + n], start=True, stop=True),
                         reads=[bdn, Gt], writes=[ps_b])
                    P.op("act", lambda e, oc=oc, o=o, n=n: e.copy(yacc[:, oc, o:o + n], ps_b[:, :n]), reads=[ps_b], writes=[(yacc, oc, t0)])
            for ex in range(MOE_EXPERTS):
                wu, wd, bu = wup[wi % 2], wdn[wi % 2], bup[wi % 2]
                wi += 1
                wu3 = din["w_up"][l, ex].rearrange("(kc p) n -> p kc n", p=128)
                wd3 = din["w_dn"][l, ex].rearrange("(kc p) n -> p kc n", p=128)
                for q4 in range(4):
                    dma(P, "pool", wu[:, q4 * 2:q4 * 2 + 2, :], wu3[:, q4 * 2:q4 * 2 + 2, :], writes=[wu] if q4 == 0 else [(wu, q4)])
                for q2 in range(2):
                    dma(P, "pool", wd[:, q2 * 4:q2 * 4 + 4, :], wd3[:, q2 * 4:q2 * 4 + 4, :], writes=[wd] if q2 == 0 else [(wd, q2)])
                dma(P, "sp", bu[:], din["b_up_fm"][l, ex], writes=[bu])
                wukeys = [wu] + [(wu, q) for q in range(1, 4)]
                wdkeys = [wd, (wd, 1)]
                for ti, (t0, n) in enumerate(blk):
                    o = t0 - b0
                    g_ = gb[ti % 2]
                    av = avs[avi[0] % 2]
                    avi[0] += 1
                    dma(P, "sp", g_[:, :n], scr["gfm"][ex:ex + 1, t0:t0 + n].partition_broadcast(128), writes=[g_])
                    for fc in range(8):
                        i2 = fc % 2
                        pg, pl = ps_gl[i2], ps_ln[i2]
                        for kc in range(8):
                            P.op("pe", lambda e, pg=pg, kc=kc, fc=fc, o=o, n=n, wu=wu: e.matmul(pg[:, :n], lhsT=wu[:, kc, fc * 128:(fc + 1) * 128], rhs=h2[:, kc, o:o + n], start=(kc == 0), stop=(kc == 7)),
                                 reads=wukeys + [h2], writes=[pg], acc=True)
                        for kc in range(8):
                            P.op("pe", lambda e, pl=pl, kc=kc, fc=fc, o=o, n=n, wu=wu: e.matmul(pl[:, :n], lhsT=wu[:, kc, 1024 + fc * 128:1024 + (fc + 1) * 128], rhs=h2[:, kc, o:o + n], start=(kc == 0), stop=(kc == 7)),
                                 reads=wukeys + [h2], writes=[pl], acc=True)
                        gl, sg, ln = glu[i2], sig[i2], lin[i2]
                        P.op("dve", lambda e, gl=gl, pg=pg, fc=fc, n=n, bu=bu: e.tensor_scalar(out=gl[:, :n], in0=pg[:, :n], scalar1=bu[:, fc:fc + 1], scalar2=C["seven"][:, 0:1], op0=ALU.add, op1=ALU.min),
                             reads=[pg, bu], writes=[gl])
                        P.op("act", lambda e, gl=gl, sg=sg, n=n: e.activation(out=sg[:, :n], in_=gl[:, :n], func=AF.Sigmoid, scale=1.702), reads=[gl], writes=[sg])
                        P.op("dve", lambda e, ln=ln, pl=pl, fc=fc, n=n, bu=bu: e.tensor_scalar(out=ln[:, :n], in0=pl[:, :n], scalar1=bu[:, 8 + fc:9 + fc], scalar2=C["seven"][:, 0:1], op0=ALU.add, op1=ALU.min),
                             reads=[pl, bu], writes=[ln])
                        P.op("dve", lambda e, ln=ln, n=n: e.tensor_scalar(out=ln[:, :n], in0=ln[:, :n], scalar1=-7.0, scalar2=1.0, op0=ALU.max, op1=ALU.add), reads=[ln], writes=[ln])
                        P.op("dve", lambda e, gl=gl, sg=sg, n=n: e.tensor_tensor(out=sg[:, :n], in0=gl[:, :n], in1=sg[:, :n], op=ALU.mult), reads=[gl, sg], writes=[sg])
                        P.op("dve", lambda e, ln=ln, sg=sg, n=n: e.tensor_tensor(out=sg[:, :n], in0=sg[:, :n], in1=ln[:, :n], op=ALU.mult), reads=[ln, sg], writes=[sg])
                        P.op("dve", lambda e, sg=sg, g_=g_, fc=fc, n=n, av=av: e.tensor_tensor(out=av[:, fc, :n], in0=sg[:, :n], in1=g_[:, :n], op=ALU.mult), reads=[sg, g_], writes=[(av, fc)])
                    akeys = [(av, fc) for fc in range(8)]
                    for oc in range(8):
                        py = ps_y[oc % 2]
                        for fc in range(8):
                            P.op("pe", lambda e, py=py, fc=fc, oc=oc, n=n, wd=wd, av=av: e.matmul(py[:, :n], lhsT=wd[:, fc, oc * 128:(oc + 1) * 128], rhs=av[:, fc, :n], start=(fc == 0), stop=(fc == 7)),
                                 reads=wdkeys + akeys, writes=[py], acc=True)
                        P.op("dve", lambda e, py=py, oc=oc, o=o, n=n: e.tensor_tensor(out=yacc[:, oc, o:o + n], in0=py[:, :n], in1=yacc[:, oc, o:o + n], op=ALU.add),
                             reads=[py, (yacc, oc, t0)], writes=[(yacc, oc, t0)])
            for (t0, n) in blk:
                o = t0 - b0
                mc = 1 if t0 < CTX else 0
                for oc in range(8):
                    x_ = xc[oc % 2]
                    dma(P, "sp", x_[:, :n], xr3[:, oc, t0:t0 + n], writes=[x_])
                    P.op("dve", lambda e, x_=x_, oc=oc, o=o, n=n, mc=mc: e.scalar_tensor_tensor(out=x_[:, :n], in0=yacc[:, oc, o:o + n], scalar=k.modv[:, 40 + oc, mc:mc + 1], in1=x_[:, :n], op0=ALU.mult, op1=ALU.add),
                         reads=[x_, (yacc, oc, t0)], writes=[x_])
                    dma(P, "sp", xr3[:, oc, t0:t0 + n], x_[:, :n], reads=[x_], writes=["xres_o"])


def phaseFIN(k):
    P, C, din, scr = k.P, k.C, k.din, k.scr
    with ExitStack() as st:
        k.alloc_psum(st, 8)
        fg = P.sb("F_g", [128, 8], F32, st)
        dma(P, "sp", fg[:], din["fing_fm"][:, :], writes=[fg])
        xt = [P.sb("F_x%d" % i, [128, 8, 512], F32, st) for i in range(2)]
        sq = [P.sb("F_sq%d" % i, [128, 8, 512], BF16, st) for i in range(2)]
        rs = [P.sb("F_rs%d" % i, [128, 512], F32, st) for i in range(2)]
        ot = [P.sb("F_o%d" % i, [128, 1024], F32, st) for i in range(2)]
        xr3 = scr["xres"].rearrange("(c p) t -> p c t", p=128)
        oi = 0
        for ti, (t0, n) in enumerate(TILES):
            if t0 < CTX:
                continue
            x, q, r = xt[ti % 2], sq[ti % 2], rs[ti % 2]
            ps = k.psum[ti % 2]
            dma(P, "sp", x[:, :, :n], xr3[:, :, t0:t0 + n], writes=[x])
            P.op("act", lambda e, x=x, q=q, n=n: e.activation(out=q[:, :, :n], in_=x[:, :, :n], func=AF.Square), reads=[x], writes=[q])
            for c in range(8):
                P.op("pe", lambda e, ps=ps, q=q, c=c, n=n: e.matmul(ps[:, :n], lhsT=C["ones"][:], rhs=q[:, c, :n], start=(c == 0), stop=(c == 7)), reads=[q], writes=[ps], acc=True)
            P.op("act", lambda e, ps=ps, r=r, n=n: e.activation(out=r[:, :n], in_=ps[:, :n], func=AF.Sqrt, bias=C["eps"][:, 0:1], scale=1.0 / D), reads=[ps], writes=[r])
            P.op("dve", lambda e, r=r, n=n: e.reciprocal(r[:, :n], r[:, :n]), reads=[r], writes=[r])
            P.op("dve", lambda e, x=x, r=r, n=n: e.tensor_tensor(out=x[:, :, :n], in0=x[:, :, :n], in1=r[:, :n].unsqueeze(1).to_broadcast([128, 8, n]), op=ALU.mult), reads=[x, r], writes=[x])
            P.op("dve", lambda e, x=x, n=n: e.tensor_tensor(out=x[:, :, :n], in0=x[:, :, :n], in1=fg[:].unsqueeze(2).to_broadcast([128, 8, n]), op=ALU.mult), reads=[x, fg], writes=[x])
            for j in range(n // 128):
                o_ = ot[oi % 2]
                oi += 1
                for half in range(2):
                    pt = k.psum[2 + (oi * 2 + half) % 4]
                    for f4 in range(4):
                        fc = half * 4 + f4
                        P.op("pe", lambda e, pt=pt, f4=f4, fc=fc, j=j, x=x: e.transpose(pt[:, f4 * 128:(f4 + 1) * 128], x[:, fc, j * 128:(j + 1) * 128], C["ident_f"][:]), reads=[x], writes=[pt], acc=True)
                    if half == 0:
                        P.op("act", lambda e, pt=pt, o_=o_: e.copy(o_[:, 0:512], pt[:]), reads=[pt], writes=[(o_, 0)])
                    else:
                        P.op("dve", lambda e, pt=pt, o_=o_: e.tensor_copy(o_[:, 512:1024], pt[:]), reads=[pt], writes=[(o_, 1)])
                r0 = t0 - CTX + j * 128
                dma(P, "sp", k.out[r0:r0 + 128, :], o_[:], reads=[(o_, 0), (o_, 1)], writes=["out_o"])


_PROG_CACHE = {}


def kernel(**inputs):
    inp = {k_: np.asarray(v) for k_, v in inputs.items()}
    sh = host_prepare(inp)
    sh = {k_: np.ascontiguousarray(v, dtype=np.float32) for k_, v in sh.items()}
    if "nc" not in _PROG_CACHE:
        _PROG_CACHE["nc"] = build_program({k_: v.shape for k_, v in sh.items()})
    nc = _PROG_CACHE["nc"]
    in_maps = []
    for b in range(8):
        m = dict(sh)
        m.update(core_inputs(inp, b))
        in_maps.append(m)
    res = run_bass_kernel_spmd(nc, in_maps, core_ids=list(range(8)))
    out = np.stack([np.asarray(r["out"], dtype=np.float32) for r in res.results], axis=0)
    return out
```
